# Optimizing a Trainium2 kernel written in Bass

```python
import jax, jax.numpy as jnp
from jax import lax
import numpy as np

D_MODEL = 1024
BATCH = 4
SEQ = 4096
DEPTH = 4

N_HEADS_MLA = 8
QK_NOPE = 64
QK_ROPE = 32
V_HEAD = 64
Q_RANK = 384
KV_RANK = 256
ROPE_THETA = 10000.0
Q_BLOCK = 128
MLA_WIDTH = N_HEADS_MLA * V_HEAD
POOL_WINDOWS = (2, 4, 8, 16)
POOL_GROUPS = 4
POOL_GROUP = 128
POOL_WIDTH = POOL_GROUPS * POOL_GROUP
POOL_OUT_GROUP = D_MODEL // POOL_GROUPS
CONV_HEADS = 8
CONV_HEAD_DIM = 64
CONV_WIDTH = CONV_HEADS * CONV_HEAD_DIM
CONV_K = 3
N_BRANCH = 3
N_EXPERTS = 16
EXPERT_FF = 1024
EC_FACTOR = 2
EPS = 1e-6

IN_SPLITS = (Q_RANK, KV_RANK, QK_ROPE, POOL_WIDTH, CONV_WIDTH, CONV_WIDTH, CONV_WIDTH, N_BRANCH * D_MODEL)
IN_COLS = sum(IN_SPLITS)
IN_OFFSETS = tuple(int(v) for v in np.cumsum(IN_SPLITS)[:-1])

kernel_name = "hybrid_mla_pool_conv_ec_moe_encoder"


def rmsnorm(x, g):
    xf = x.astype(jnp.float32)
    y = xf * lax.rsqrt(jnp.mean(xf * xf, axis=-1, keepdims=True) + EPS)
    return (y * g.astype(jnp.float32)).astype(x.dtype)


def rope_tables(positions, dim):
    freqs = ROPE_THETA ** (-jnp.arange(0, dim, 2, dtype=jnp.float32) / dim)
    ang = positions.astype(jnp.float32)[..., None] * freqs
    return jnp.cos(ang), jnp.sin(ang)


def apply_rope(x, cos, sin):
    half = x.shape[-1] // 2
    xf = x.astype(jnp.float32)
    x1, x2 = xf[..., :half], xf[..., half:]
    return jnp.concatenate([x1 * cos - x2 * sin, x1 * sin + x2 * cos], axis=-1).astype(x.dtype)


def mla_branch(cq, ckv, kr, cos, sin, q_norm_g, w_uq, kv_norm_g, w_ukv, w_oa):
    B, S, _ = cq.shape
    H, DQK = N_HEADS_MLA, QK_NOPE + QK_ROPE
    q = (rmsnorm(cq, q_norm_g) @ w_uq).reshape(B, S, H, DQK)
    q_rope = apply_rope(q[..., QK_NOPE:], cos[:, :, None], sin[:, :, None])
    kv = (rmsnorm(ckv, kv_norm_g) @ w_ukv).reshape(B, S, H, QK_NOPE + V_HEAD)
    k_nope, v = kv[..., :QK_NOPE], kv[..., QK_NOPE:]
    k_rope = apply_rope(kr, cos, sin)
    q = jnp.concatenate([q[..., :QK_NOPE], q_rope], axis=-1) * (DQK ** -0.5)
    k = jnp.concatenate([k_nope, jnp.broadcast_to(k_rope[:, :, None], (B, S, H, QK_ROPE))], axis=-1)
    nb = S // Q_BLOCK
    qb = q.reshape(B, nb, Q_BLOCK, H, DQK).transpose(1, 0, 2, 3, 4)

    def attend(q_blk):
        s = jnp.einsum('bqhd,bkhd->bhqk', q_blk, k).astype(jnp.float32)
        p = jax.nn.softmax(s, axis=-1).astype(v.dtype)
        return jnp.einsum('bhqk,bkhd->bqhd', p, v)

    o = lax.map(attend, qb)
    o = o.transpose(1, 0, 2, 3, 4).reshape(B, S, MLA_WIDTH)
    return o @ w_oa


def pool_branch(u, w_pool, pool_scale):
    B, S, _ = u.shape
    ug = u.reshape(B, S, POOL_GROUPS, POOL_GROUP).astype(jnp.float32)
    cs = jnp.concatenate([jnp.zeros((B, 1, POOL_GROUPS, POOL_GROUP), jnp.float32),
                          jnp.cumsum(ug, axis=1)], axis=1)
    t = jnp.arange(S)
    outs = []
    for gi, w in enumerate(POOL_WINDOWS):
        lo = jnp.clip(t - w // 2, 0, S - 1)
        hi = jnp.clip(t + w // 2 - 1, 0, S - 1)
        cnt = (hi - lo + 1).astype(jnp.float32)[:, None]
        csg = cs[:, :, gi]
        mean = (csg[:, hi + 1] - csg[:, lo]) / cnt
        outs.append(mean - ug[:, :, gi])
    mixed = jnp.stack(outs, axis=2).astype(u.dtype)
    y = jnp.einsum('bsgc,gcd->bsgd', mixed, w_pool).reshape(B, S, D_MODEL)
    return y * pool_scale


def conv_branch(xin, b_gate, c_gate, conv_w, w_oc):
    S = xin.shape[1]
    z = c_gate * xin
    pad = CONV_K // 2
    zp = jnp.pad(z, ((0, 0), (pad, pad), (0, 0)))
    y = sum(conv_w[k] * zp[:, k:k + S] for k in range(CONV_K))
    return (b_gate * y) @ w_oc


def ec_moe(h, w_router, w_gate, w_up, w_down):
    B, S, D = h.shape
    cap = EC_FACTOR * S // N_EXPERTS
    logits = jnp.einsum('bsd,de->bse', h, w_router).astype(jnp.float32)
    aff = jax.nn.softmax(logits, axis=-1)
    top_val, top_idx = lax.top_k(aff.transpose(0, 2, 1), cap)
    xg = jax.vmap(lambda hb, ib: hb[ib])(h, top_idx)
    g = jnp.einsum('becd,edf->becf', xg, w_gate)
    u = jnp.einsum('becd,edf->becf', xg, w_up)
    y = jnp.einsum('becf,efd->becd', jax.nn.silu(g) * u, w_down) * top_val[..., None].astype(h.dtype)
    return jax.vmap(lambda yb, ib: jnp.zeros((S, D), yb.dtype).at[ib.reshape(-1)].add(yb.reshape(-1, D)))(y, top_idx)


def setup_inputs(seed: int = 0) -> dict:
    key = jax.random.key(seed)
    ks = jax.random.split(key, 24)
    f32 = jnp.float32
    D, L = D_MODEL, DEPTH

    def nrm(k, shape, scale):
        return jax.random.normal(k, shape, f32) * scale

    steps = jax.random.randint(ks[2], (BATCH, SEQ), 1, 3)
    positions = (jnp.cumsum(steps, axis=1) - steps[:, :1]).astype(jnp.int32)
    return {
        "x": nrm(ks[0], (BATCH, SEQ, D), 1.0),
        "c": nrm(ks[1], (BATCH, D), 1.0),
        "positions": positions,
        "w_mod": nrm(ks[3], (L, D, 6 * D), 0.5 * D ** -0.5),
        "b_mod": nrm(ks[4], (L, 6 * D), 0.01),
        "norm1_g": 1.0 + nrm(ks[5], (L, D), 0.05),
        "w_in": nrm(ks[6], (L, D, IN_COLS), D ** -0.5),
        "b_gate": nrm(ks[7], (L, N_BRANCH * D), 0.01),
        "q_norm_g": 1.0 + nrm(ks[8], (L, Q_RANK), 0.05),
        "w_uq": nrm(ks[9], (L, Q_RANK, N_HEADS_MLA * (QK_NOPE + QK_ROPE)), Q_RANK ** -0.5),
        "kv_norm_g": 1.0 + nrm(ks[10], (L, KV_RANK), 0.05),
        "w_ukv": nrm(ks[11], (L, KV_RANK, N_HEADS_MLA * (QK_NOPE + V_HEAD)), KV_RANK ** -0.5),
        "w_oa": nrm(ks[12], (L, MLA_WIDTH, D), MLA_WIDTH ** -0.5),
        "w_pool": nrm(ks[13], (L, POOL_GROUPS, POOL_GROUP, POOL_OUT_GROUP), POOL_GROUP ** -0.5),
        "pool_scale": 1.0 + nrm(ks[14], (L, D), 0.05),
        "conv_w": nrm(ks[15], (L, CONV_K, CONV_WIDTH), CONV_K ** -0.5),
        "w_oc": nrm(ks[16], (L, CONV_WIDTH, D), CONV_WIDTH ** -0.5),
        "w_out": nrm(ks[17], (L, D, D), D ** -0.5),
        "norm2_g": 1.0 + nrm(ks[18], (L, D), 0.05),
        "w_router": nrm(ks[19], (L, D, N_EXPERTS), D ** -0.5),
        "w_gate": nrm(ks[20], (L, N_EXPERTS, D, EXPERT_FF), D ** -0.5),
        "w_up": nrm(ks[21], (L, N_EXPERTS, D, EXPERT_FF), D ** -0.5),
        "w_down": nrm(ks[22], (L, N_EXPERTS, EXPERT_FF, D), EXPERT_FF ** -0.5),
        "final_g": 1.0 + nrm(ks[23], (D,), 0.05),
    }


def reference(x, c, positions, w_mod, b_mod, norm1_g, w_in, b_gate, q_norm_g, w_uq, kv_norm_g, w_ukv, w_oa,
              w_pool, pool_scale, conv_w, w_oc, w_out, norm2_g, w_router, w_gate, w_up, w_down, final_g):
    B, S, D = x.shape
    cos, sin = rope_tables(positions, QK_ROPE)
    c_act = jax.nn.silu(c)
    for l in range(DEPTH):
        mod = (c_act @ w_mod[l] + b_mod[l])[:, None, :]
        sh1, sc1, g1, sh2, sc2, g2 = jnp.split(mod, 6, axis=-1)
        h = rmsnorm(x, norm1_g[l]) * (1.0 + sc1) + sh1
        proj = h @ w_in[l]
        cq, ckv, kr, pu, cx, cb, cc, gl = jnp.split(proj, IN_OFFSETS, axis=-1)
        ya = mla_branch(cq, ckv, kr, cos, sin, q_norm_g[l], w_uq[l], kv_norm_g[l], w_ukv[l], w_oa[l])
        yb = pool_branch(pu, w_pool[l], pool_scale[l])
        yc = conv_branch(cx, cb, cc, conv_w[l], w_oc[l])
        gates = jax.nn.sigmoid((gl + b_gate[l]).astype(jnp.float32)).astype(x.dtype).reshape(B, S, N_BRANCH, D)
        merged = gates[:, :, 0] * ya + gates[:, :, 1] * yb + gates[:, :, 2] * yc
        x = x + g1 * (merged @ w_out[l])
        h2 = rmsnorm(x, norm2_g[l]) * (1.0 + sc2) + sh2
        x = x + g2 * ec_moe(h2, w_router[l], w_gate[l], w_up[l], w_down[l])
    return rmsnorm(x, final_g)
```

```python
import math
import os as _os
from contextlib import ExitStack

import numpy as np
import concourse.bass as bass
import concourse.mybir as mybir
from concourse.bass_utils import run_bass_kernel_spmd

F32 = mybir.dt.float32
BF16 = mybir.dt.bfloat16
I32 = mybir.dt.int32
ALU = mybir.AluOpType
AF = mybir.ActivationFunctionType
AX = mybir.AxisListType

D = 1024
KC = 8
H = 8
NE = 16
EPS = 1e-6
SCALE = 96 ** -0.5
WIN_EXT = 5888


class Buf:
    __slots__ = ("last_w", "readers", "dma_readers")

    def __init__(self):
        self.last_w = None
        self.readers = {}
        self.dma_readers = []


class V:
    __slots__ = ("ap", "bufs")

    def __init__(self, ap, bufs):
        self.ap = ap
        self.bufs = bufs


class Tile:
    def __init__(self, handle):
        self.h = handle
        self.bufs = {}

    def buf(self, key):
        b = self.bufs.get(key)
        if b is None:
            b = self.bufs[key] = Buf()
        return b

    def __getitem__(self, idx):
        return V(self.h[idx], [self.buf(None)])

    def p(self, key, idx):
        keys = key if isinstance(key, (list, tuple)) else [key]
        return V(self.h[idx], [self.buf(k) for k in keys])


class Op:
    __slots__ = ("eng", "meth", "kw", "deps", "is_dma", "has_dep", "sig")


WRITE_KEYS = ("out", "accum_out", "ap")
ENGS = ("pe", "act", "dve", "pool", "sp")


class StopBuild(Exception):
    pass


class Prog:
    def __init__(self, nc):
        self.nc = nc
        self.ops = []
        self.last = {}
        self.recent_dma = {e: [] for e in ENGS}
        self.R = 8

    limit = None

    def op(self, eng, meth, reads=(), writes=(), dma=False, deps_extra=(), **kw):
        if self.limit is not None and len(self.ops) >= self.limit:
            self.limit = None
            raise StopBuild()
        rd, wr = [], []
        for k, v in list(kw.items()):
            if isinstance(v, V):
                (wr if k in WRITE_KEYS else rd).extend(v.bufs)
                kw[k] = v.ap
        for v in reads:
            rd.extend(v.bufs)
        for v in writes:
            wr.extend(v.bufs)
        o = Op()
        o.eng, o.meth, o.kw, o.is_dma, o.has_dep, o.sig = eng, meth, kw, dma, False, None
        deps = {}

        def add(d):
            if d is None or d is o:
                return
            if (not dma) and eng == "pe" and d.eng == "pe" and not d.is_dma:
                return
            deps[id(d)] = d

        for d_ in deps_extra:
            add(d_)
        for b in rd:
            add(b.last_w)
        for b in wr:
            add(b.last_w)
            for r in b.readers.values():
                add(r)
            for r in b.dma_readers:
                add(r)
        o.deps = list(deps.values())
        for d in o.deps:
            d.has_dep = True
        for b in rd:
            if dma:
                b.dma_readers.append(o)
                if len(b.dma_readers) > 64:
                    b.dma_readers = b.dma_readers[-64:]
            else:
                b.readers[eng] = o
        for b in wr:
            b.last_w = o
            b.readers = {}
            b.dma_readers = []
        self.ops.append(o)
        if dma:
            lst = self.recent_dma[eng]
            lst.append(o)
            if len(lst) > self.R:
                lst.pop(0)
        else:
            self.last[eng] = o
        return o

    def barrier(self):
        alld = [o for o in self.last.values()]
        for lst in self.recent_dma.values():
            alld.extend(lst)
        for d in alld:
            d.has_dep = True
        for e in ENGS:
            o = Op()
            o.eng, o.meth, o.kw, o.is_dma, o.has_dep, o.sig = e, None, {}, False, False, None
            o.deps = list(alld)
            self.ops.append(o)

    def emit(self, stack):
        nc = self.nc
        engobj = {"pe": nc.tensor, "act": nc.scalar, "dve": nc.vector, "pool": nc.gpsimd, "sp": nc.sync}
        nsem = [0]

        def new_sem(tag):
            nsem[0] += 1
            return stack.enter_context(nc.semaphore(f"s_{tag}_{nsem[0]}"))

        sem_state = {}
        waited = {e: {} for e in ENGS}
        dma_cnt = {e: 0 for e in ENGS}
        dma_sems = {}

        def wait(eng, sem, val):
            w = waited[eng]
            k = id(sem)
            if w.get(k, 0) >= val:
                return
            engobj[eng].wait_ge(sem, val)
            w[k] = val

        keep = []
        for o in self.ops:
            e = engobj[o.eng]
            for d in o.deps:
                wait(o.eng, d.sig[0], d.sig[1])
            if o.meth is None:
                continue
            if o.is_dma:
                if o.eng not in dma_sems:
                    dma_sems[o.eng] = [new_sem("d" + o.eng) for _ in range(self.R)]
                j = dma_cnt[o.eng]
                dma_cnt[o.eng] += 1
                sem = dma_sems[o.eng][j % self.R]
                val = 16 * (j // self.R + 1)
                if j >= self.R:
                    wait(o.eng, sem, val - 16)
                ins = getattr(e, o.meth)(**o.kw)
                ins.then_inc(sem, 16)
                o.sig = (sem, val)
            else:
                ins = getattr(e, o.meth)(**o.kw)
                if o.has_dep:
                    st = sem_state.get(o.eng)
                    if st is None or st[1] >= 30000:
                        st = sem_state[o.eng] = [new_sem(o.eng), 0]
                    st[1] += 1
                    ins.then_inc(st[0], 1)
                    o.sig = (st[0], st[1])
            o.kw = None
        for eng in ENGS:
            for q, sems in dma_sems.items():
                n = dma_cnt[q]
                for sl, sem in enumerate(sems):
                    cnt = (n - sl + self.R - 1) // self.R if n > sl else 0
                    if cnt > 0:
                        wait(eng, sem, 16 * cnt)
            for e2, st in sem_state.items():
                if st[1] > 0:
                    wait(eng, st[0], st[1])


class Ring:
    def __init__(self, tiles):
        self.t = tiles
        self.i = 0

    def next(self):
        t = self.t[self.i % len(self.t)]
        self.i += 1
        return t


def build(S, L, debug=False, stop=99):
    NT = S // 512
    NB = S // 128
    CAP = 2 * S // NE
    SCH = CAP // 128
    nc = bass.Bass("TRN2", target_bir_lowering=False)
    P = Prog(nc)
    if _os.environ.get('KLIMIT'):
        P.limit = int(_os.environ['KLIMIT'])
    top = ExitStack()

    def dram(name, shape, dt, kind):
        return Tile(nc.dram_tensor(name, list(shape), dt, kind=kind))

    uniq = [0]

    def sb(stack, name, shape, dt):
        uniq[0] += 1
        return Tile(stack.enter_context(nc.sbuf_tensor(f"sb{uniq[0]}_{name}", list(shape), dt)))

    def sbring(stack, name, shape, dt, n):
        return Ring([sb(stack, f"{name}{i}", shape, dt) for i in range(n)])

    EI = "ExternalInput"
    x_in = dram("x", [S, D], F32, EI)
    cT_in = dram("cT", [128, KC], F32, EI)
    pos_in = dram("pos", [1, S], I32, EI)
    w_mod = dram("w_mod", [L, D, 6 * D], F32, EI)
    b_mod = dram("b_mod", [L, 1, 6 * D], F32, EI)
    n1g_in = dram("n1g", [L, 128, KC], F32, EI)
    n2g_in = dram("n2g", [L, 1, D], F32, EI)
    w_in = dram("w_in", [L, D, WIN_EXT], F32, EI)
    bgate_in = dram("bgate", [L, 128, 24], F32, EI)
    qng_in = dram("qng", [L, 128, 3], F32, EI)
    kvng_in = dram("kvng", [L, 128, 2], F32, EI)
    wuqA_in = dram("wuqA", [L, 384, 768], F32, EI)
    wuqB_in = dram("wuqB", [L, 384, 768], F32, EI)
    wk_in = dram("wk", [L, 256, 512], F32, EI)
    wv_in = dram("wv", [L, 256, 512], F32, EI)
    woa_in = dram("woa", [L, 64, H, D], F32, EI)
    wpool_in = dram("wpool", [L, 128, 4, 256], F32, EI)
    pscale_in = dram("pscale", [L, 128, KC], F32, EI)
    convw_in = dram("convw", [L, 128, 4, 3], F32, EI)
    woc_in = dram("woc", [L, 512, D], F32, EI)
    wout_in = dram("wout", [L, D, D], F32, EI)
    wr_in = dram("wr", [L, 128, KC, NE], F32, EI)
    wg_in = dram("w_gate", [L, NE, D, D], F32, EI)
    wu_in = dram("w_up", [L, NE, D, D], F32, EI)
    wd_in = dram("w_down", [L, NE, D, D], F32, EI)
    fg_in = dram("fg", [1, D], F32, EI)
    ident_in = dram("ident", [128, 128], F32, EI)
    ltri_in = dram("ltri", [128, 128], F32, EI)
    iota_in = dram("iota", [128, 512], F32, EI)
    tokhl_in = dram("tokhl", [128, NB, 2], F32, EI)
    invc_in = dram("invc", [128, 4, 16], F32, EI)
    ropec_in = dram("ropec", [96, 4], F32, EI)
    out_d = dram("out", [S, D], F32, "ExternalOutput")

    IN = "Internal"
    xm_d = dram("xm_d", [S, D], F32, IN)
    cc_d = dram("cc_d", [32, S], F32, IN)
    ss_d = dram("ss_d", [32, S], F32, IN)
    mod_d = dram("mod_d", [1, 6 * D], F32, IN)
    wa1_d = dram("wa1_d", [128, KC, 1984], BF16, IN)
    wa2_d = dram("wa2_d", [128, KC, 896], BF16, IN)
    wgt_d = dram("wgt_d", [128, 8, KC, 384], BF16, IN)
    woa_d = dram("woa_d", [64, 8, H, 128], BF16, IN)
    woc_d = dram("woc_d", [128, 8, 4, 128], BF16, IN)
    wout_d = dram("wout_d", [128, KC, D], BF16, IN)
    kt_d = dram("kt_d", [96, H, S], BF16, IN)
    v_d = dram("v_d", [128, NB, H, 65], BF16, IN)
    ot_d = dram("ot_d", [64, H, S], BF16, IN)
    pu_d = dram("pu_d", [128, 4, S + 16], F32, IN)
    z_d = dram("z_d", [128, 4, S + 2], F32, IN)
    h2_d = dram("h2_d", [S, D], BF16, IN)
    dbg = {}
    if debug:
        dbg["dbg_x1"] = dram("dbg_x1", [S, D], F32, "ExternalOutput")

    ident_f = sb(top, "ident_f", [128, 128], F32)
    ident_b = sb(top, "ident_b", [128, 128], BF16)
    ones_b = sb(top, "ones_b", [128, 128], BF16)
    ones_f = sb(top, "ones_f", [128, 128], F32)
    zeros_b = sb(top, "zeros_b", [128, 128], BF16)
    sel_f = sb(top, "sel_f", [65, 64], F32)
    ltri_b = sb(top, "ltri_b", [128, 128], BF16)
    iota_f = sb(top, "iota_f", [128, 512], F32)
    tokhl_b = sb(top, "tokhl_b", [128, NB, 2], BF16)
    invc = sb(top, "invc", [128, 4, 16], F32)
    eps_t = sb(top, "eps_t", [128, 1], F32)
    chalf = sb(top, "chalf", [128, 8], F32)
    phalf = sb(top, "phalf", [128, 8], F32)
    cact = sb(top, "cact", [128, KC], F32)
    kmax = sb(top, "kmax", [128, H], F32)
    gm1T = sb(top, "gm1T", [128, KC], F32)
    sh1T = sb(top, "sh1T", [128, KC], F32)
    small = sb(top, "small", [128, 64], F32)

    mmall = Tile(top.enter_context(nc.psum_tensor("pmmall", [128, 2048], F32)))

    class Sub:
        def __init__(self, k0, nb):
            self.h = mmall.h[:, k0 * 512:(k0 + nb) * 512]
            self.bl = [mmall.buf(k0 + j) for j in range(nb)]

        def __getitem__(self, idx):
            return V(self.h[idx], self.bl)

    mmring = Ring([Sub(i, 1) for i in range(4)])
    mm2ring = Ring([Sub(0, 2), Sub(2, 2)])
    accring = Ring([Tile(top.enter_context(nc.psum_tensor(f"pacc{i}", [128, 512], F32))) for i in range(2)])
    auxring = Ring([Tile(top.enter_context(nc.psum_tensor(f"paux{i}", [128, 512], F32))) for i in range(2)])

    def dma(q, out, in_, **kw):
        return P.op(q, "dma_start", dma=True, out=out, in_=in_, **kw)

    def mm(out, lhsT, rhs, start, stop, **kw):
        return P.op("pe", "matmul", out=out, lhsT=lhsT, rhs=rhs, start=start, stop=stop, **kw)

    def act(out, in_, func, **kw):
        return P.op("act", "activation", out=out, in_=in_, func=func, **kw)

    def tt(eng, out, in0, in1, op):
        return P.op(eng, "tensor_tensor", out=out, in0=in0, in1=in1, op=op)

    def ts(eng, out, in0, s1, op0, s2=None, op1=None, **kw):
        if op1 is None:
            return P.op(eng, "tensor_scalar", out=out, in0=in0, scalar1=s1, scalar2=None, op0=op0, **kw)
        return P.op(eng, "tensor_scalar", out=out, in0=in0, scalar1=s1, scalar2=s2, op0=op0, op1=op1, **kw)

    def stt(eng, out, in0, scalar, in1, op0, op1):
        return P.op(eng, "scalar_tensor_tensor", out=out, in0=in0, scalar=scalar, in1=in1, op0=op0, op1=op1)

    def cp(eng, out, in_):
        if eng == "act":
            return act(out, in_, AF.Copy)
        return P.op(eng, "tensor_copy", out=out, in_=in_)

    def rsqrt_from_ss(dst, ss, n):
        np_ = dst.ap.shape[0]
        w = dst.ap.shape[1]
        if w <= 8:
            ts("dve", dst, ss, 1.0 / n, ALU.mult, EPS, ALU.add)
            P.op("pool", "tensor_tensor", out=dst, in0=dst, in1=chalf[0:np_, 0:w], op=ALU.pow)
        else:
            act(dst, ss, AF.Ln, scale=1.0 / n, bias=eps_t[0:np_, 0:1])
            act(dst, dst, AF.Exp, scale=-0.5)

    def bview(v, shape_mid):
        return V(v.ap.unsqueeze(2).to_broadcast(list(shape_mid)), v.bufs)

    with ExitStack() as st:
        stg = sb(st, "su_stg", [128, 1024], F32)
        dma("sp", ident_f[:, :], ident_in[:, :])
        cp("dve", ident_b[:, :], ident_f[:, :])
        dma("sp", stg[:, 0:128], ltri_in[:, :])
        cp("dve", ltri_b[:, :], stg[:, 0:128])
        dma("sp", iota_f[:, :], iota_in[:, :])
        dma("sp", invc[:, :, :], invc_in[:, :, :])
        stg2 = sb(st, "su_stg2", [128, NB, 2], F32)
        dma("sp", stg2[:, :, :], tokhl_in[:, :, :])
        cp("dve", tokhl_b[:, :, :], stg2[:, :, :])
        P.op("dve", "memset", ap=ones_b[:, :], constant=1.0)
        P.op("dve", "memset", ap=ones_f[:, :], constant=1.0)
        P.op("dve", "memset", ap=zeros_b[:, :], constant=0.0)
        P.op("dve", "memset", ap=sel_f[:, :], constant=0.0)
        P.op("dve", "memset", ap=sel_f[64:65, :], constant=1.0)
        P.op("dve", "memset", ap=eps_t[:, :], constant=EPS)
        P.op("dve", "memset", ap=chalf[:, :], constant=-0.5)
        P.op("dve", "memset", ap=phalf[:, :], constant=0.5)
        dma("sp", cact[:, :], cT_in[:, :])
        act(cact[:, :], cact[:, :], AF.Silu)
        P.op("dve", "memset", ap=stg[:, 0:64], constant=0.0)
        zv = V(stg.h[:, 0:32].rearrange("p (g c) -> p g c", g=4), stg[:, :].bufs)
        dma("pool", pu_d.p("pad", (slice(None), slice(None), slice(0, 8))), zv)
        dma("pool", pu_d.p("pad", (slice(None), slice(None), slice(S + 8, S + 16))), zv)
        zv1 = V(stg.h[:, 0:4].rearrange("p (g c) -> p g c", g=4), stg[:, :].bufs)
        dma("pool", z_d.p("pad", (slice(None), slice(None), slice(0, 1))), zv1, allow_slow_non_contiguous=True)
        dma("pool", z_d.p("pad", (slice(None), slice(None), slice(S + 1, S + 2))), zv1, allow_slow_non_contiguous=True)
        rc = sb(st, "su_rc", [96, 4], F32)
        dma("sp", rc[:, :], ropec_in[:, :])
        CH = min(S, 2048)
        posi = sb(st, "su_posi", [96, CH], I32)
        posf = sb(st, "su_posf", [96, CH], F32)
        a2 = sb(st, "su_a2", [96, CH], F32)
        ki = sb(st, "su_ki", [96, CH], I32)
        kf = sb(st, "su_kf", [96, CH], F32)
        r_ = sb(st, "su_r", [96, CH], F32)
        m_ = sb(st, "su_m", [96, CH], F32)
        PR = slice(64, 96)
        C1 = 6.28125
        C2 = 2.0 * math.pi - 6.28125
        for c0 in range(0, S, CH):
            dma("sp", posi[PR, :], V(pos_in.h[0:1, c0:c0 + CH].broadcast_to([32, CH]), pos_in[:, :].bufs))
            cp("dve", posf[PR, :], posi[PR, :])
            for which, dst in ((1, cc_d), (2, ss_d)):
                ts("dve", a2[PR, :], posf[PR, :], rc[PR, 0:1], ALU.mult, rc[PR, which:which + 1], ALU.add)
                ts("dve", m_[PR, :], a2[PR, :], 1.0 / (2.0 * math.pi), ALU.mult)
                cp("dve", ki[PR, :], m_[PR, :])
                cp("dve", kf[PR, :], ki[PR, :])
                stt("dve", r_[PR, :], kf[PR, :], -C1, a2[PR, :], ALU.mult, ALU.add)
                stt("dve", r_[PR, :], kf[PR, :], -C2, r_[PR, :], ALU.mult, ALU.add)
                ts("dve", m_[PR, :], r_[PR, :], math.pi, ALU.is_gt, -2.0 * math.pi, ALU.mult)
                tt("dve", r_[PR, :], r_[PR, :], m_[PR, :], ALU.add)
                ts("dve", m_[PR, :], r_[PR, :], -math.pi, ALU.is_lt, 2.0 * math.pi, ALU.mult)
                tt("dve", r_[PR, :], r_[PR, :], m_[PR, :], ALU.add)
                ts("dve", r_[PR, :], r_[PR, :], -3.141592, ALU.max, 3.141592, ALU.min)
                act(m_[PR, :], r_[PR, :], AF.Sin)
                dma("pool", dst.p("all", (slice(None), slice(c0, c0 + CH))), m_[PR, :])
    P.barrier()

    def make_norm(st, tag):
        xring = sbring(st, f"{tag}_x", [128, D], F32, 2)
        sqj = sb(st, f"{tag}_sqj", [128, D], BF16)
        xnring = sbring(st, f"{tag}_xn", [128, D], BF16, 2)
        rs = sbring(st, f"{tag}_rs", [128, 2], F32, 2)

        def norm_tile(xsrc, i, hT):
            for j in range(4):
                blk = i * 4 + j
                xt = xring.next()
                r = rs.next()
                dma("sp", xt[:, :], xsrc.p(blk, (slice(blk * 128, blk * 128 + 128), slice(None))))
                act(sqj[:, :], xt[:, :], AF.Square, accum_out=r[:, 0:1])
                rsqrt_from_ss(r[:, 1:2], r[:, 0:1], D)
                xn = xnring.next()
                act(xn[:, :], xt[:, :], AF.Copy, scale=r[:, 1:2])
                pt = auxring.next()
                ptv = pt.h[:, :].bitcast(BF16)
                for kc in range(KC):
                    P.op("pe", "transpose", out=V(ptv[:, kc * 128:(kc + 1) * 128], pt[:, :].bufs),
                         in_=xn[:, kc * 128:(kc + 1) * 128], identity=ident_b[:, :])
                pv = V(ptv.rearrange("p (k t) -> p k t", k=KC), pt[:, :].bufs)
                hv = V(hT.h[:, :, j * 128:(j + 1) * 128], hT[:, :, :].bufs)
                tt("dve", hv, pv, bview(gm1T[:, :], [128, KC, 128]), ALU.mult)
                tt("dve", hv, hv, bview(sh1T[:, :], [128, KC, 128]), ALU.add)

        return norm_tile

    def load_mod_T(l):
        with ExitStack() as st:
            row = sb(st, "lm_row", [1, 2 * D], F32)
            n1 = sb(st, "lm_n1", [128, KC], F32)
            dma("sp", row[:, :], mod_d.p("all", (slice(0, 1), slice(0, 2 * D))))
            dma("sp", n1[:, :], n1g_in.p(l, (l, slice(None), slice(None))))
            ps = auxring.next()
            for j in range(2 * KC):
                mm(ps[:, j:j + 1], row[0:1, j * 128:(j + 1) * 128], ones_f[0:1, 0:1], True, True)
            cp("dve", sh1T[:, :], ps[:, 0:KC])
            ts("dve", gm1T[:, :], ps[:, KC:2 * KC], 1.0, ALU.add)
            tt("dve", gm1T[:, :], gm1T[:, :], n1[:, :], ALU.mult)
        P.barrier()

    def bc_row(dst, off, stq, scratch_row):
        dma("sp", scratch_row[:, :], mod_d.p("all", (slice(0, 1), slice(off, off + D))))
        for hf in range(2):
            ps = auxring.next()
            mm(ps[:, :], ones_f[0:1, :], scratch_row[0:1, hf * 512:(hf + 1) * 512], True, True)
            cp("dve", dst[:, hf * 512:(hf + 1) * 512], ps[:, :])

    class _Stop(Exception):
        pass

    def chk(n):
        if _os.environ.get('KVERB'):
            print('chk', n, 'ops', len(P.ops))
        if stop <= n:
            raise _Stop()

    try:
      for l in range(L):
        chk(0)
        xsrc = x_in if l == 0 else xm_d

        with ExitStack() as st:
            wst = sbring(st, "m_w", [128, KC, 512], F32, 2)
            mrow = sb(st, "m_row", [1, 6 * D], F32)
            brow = sb(st, "m_brow", [1, 6 * D], F32)
            dma("sp", brow[:, :], b_mod.p(l, (l, slice(None), slice(None))))
            for g in range(12):
                w = wst.next()
                dma("sp", w[:, :, :], V(w_mod.h[l].rearrange("(kc p) c -> p kc c", p=128)[:, :, g * 512:(g + 1) * 512],
                                        w_mod.p(l, (l,)).bufs))
                ps = mmring.next()
                for kc in range(KC):
                    mm(ps[0:1, :], cact[:, kc:kc + 1], w[:, kc, :], kc == 0, kc == KC - 1)
                tt("dve", mrow[0:1, g * 512:(g + 1) * 512], ps[0:1, :], brow[0:1, g * 512:(g + 1) * 512], ALU.add)
            dma("pool", mod_d.p("all", (slice(None), slice(None))), mrow[:, :])
        P.barrier()
        chk(1)
        load_mod_T(l)

        with ExitStack() as st:
            stg = sbring(st, "c_stg", [128, 8192], F32, 2)
            ob = sbring(st, "c_ob", [128, 8192], BF16, 2)
            g1bc = sb(st, "c_g1bc", [128, D], F32)
            srow = sb(st, "c_srow", [1, D], F32)
            bc_row(g1bc, 2 * D, st, srow)
            engs = ["dve", "act", "pool"]
            ei = [0]

            def ce():
                ei[0] += 1
                return engs[ei[0] % 3]

            for kc in range(KC):
                s_ = stg.next()
                o_ = ob.next()
                dma("sp", s_[:, 0:WIN_EXT], w_in.p(l, (l, slice(kc * 128, kc * 128 + 128), slice(None))))
                for (a, b, o0) in ((384, 640, 0), (576, 672, 256), (5792, 5888, 352), (672, 1696, 448), (2208, 2720, 1472)):
                    cp(ce(), o_[:, o0:o0 + (b - a)], s_[:, a:b])
                cp(ce(), o_[:, 2048:2048 + 384], s_[:, 0:384])
                cp(ce(), o_[:, 2432:2432 + 512], s_[:, 1696:2208])
                gin = V(s_.h[:, 2720:5792].rearrange("p (j c i) -> p j c i", j=3, c=8), s_[:, :].bufs)
                gout = V(o_.h[:, 3072:3072 + 3072].rearrange("p (c j i) -> p j c i", j=3, c=8), o_[:, :].bufs)
                for j in range(3):
                    cp(ce(), V(gout.ap[:, j], gout.bufs), V(gin.ap[:, j], gin.bufs))
                dma("pool", wa1_d.p("all", (slice(None), kc, slice(None))), o_[:, 0:1984])
                dma("pool", wa2_d.p("all", (slice(None), kc, slice(None))), o_[:, 2048:2048 + 896])
                dma("pool", wgt_d.p("all", (slice(None), slice(None), kc, slice(None))),
                    V(o_.h[:, 3072:6144].rearrange("p (c x) -> p c x", c=8), o_[:, :].bufs))
            s_ = stg.next(); o_ = ob.next()
            dma("sp", V(s_.h[0:64, 0:8192].rearrange("p (h x) -> p h x", h=H), s_[:, :].bufs), woa_in.p(l, (l,)))
            for h in range(H):
                cp(ce(), V(o_.h[0:64, 0:8192].rearrange("p (c h i) -> p h c i", c=8, h=H)[:, h], o_[:, :].bufs),
                   V(s_.h[0:64, h * 1024:(h + 1) * 1024].rearrange("p (c i) -> p c i", c=8), s_[:, :].bufs))
            dma("pool", woa_d.p("all", (slice(None),)), V(o_.h[0:64, 0:8192].rearrange("p (c h i) -> p c h i", c=8, h=H), o_[:, :].bufs))
            s_ = stg.next(); o_ = ob.next()
            dma("sp", V(s_.h[:, 0:4096].rearrange("p (k x) -> p k x", k=4), s_[:, :].bufs),
                V(woc_in.h[l].rearrange("(k p) x -> p k x", p=128), woc_in.p(l, (l,)).bufs))
            for k in range(4):
                cp(ce(), V(o_.h[:, 0:4096].rearrange("p (c k i) -> p k c i", c=8, k=4)[:, k], o_[:, :].bufs),
                   V(s_.h[:, k * 1024:(k + 1) * 1024].rearrange("p (c i) -> p c i", c=8), s_[:, :].bufs))
            dma("pool", woc_d.p("all", (slice(None),)), V(o_.h[:, 0:4096].rearrange("p (c k i) -> p c k i", c=8, k=4), o_[:, :].bufs))
            s_ = stg.next(); o_ = ob.next()
            dma("sp", V(s_.h[:, 0:8192].rearrange("p (k x) -> p k x", k=KC), s_[:, :].bufs),
                V(wout_in.h[l].rearrange("(k p) x -> p k x", p=128), wout_in.p(l, (l,)).bufs))
            for k in range(KC):
                tt("dve" if k % 2 else "pool", o_[:, k * 1024:(k + 1) * 1024], s_[:, k * 1024:(k + 1) * 1024], g1bc[:, :], ALU.mult)
            dma("pool", wout_d.p("all", (slice(None),)), V(o_.h[:, 0:8192].rearrange("p (k x) -> p k x", k=KC), o_[:, :].bufs))
        P.barrier()
        chk(2)

        with ExitStack() as st:
            wa1 = sb(st, "a1_w", [128, KC, 1984], BF16)
            wk = sb(st, "a1_wk", [128, 2, 512], BF16)
            wv = sb(st, "a1_wv", [128, 2, 512], BF16)
            kvng = sb(st, "a1_kvng", [128, 2], F32)
            stg = sb(st, "a1_stg", [128, 2, 512], F32)
            dma("sp", wa1[:, :, :], wa1_d.p("all", (slice(None),)))
            dma("sp", kvng[:, :], kvng_in.p(l, (l,)))
            for src, dst in ((wk_in, wk), (wv_in, wv)):
                dma("sp", stg[:, :, :], V(src.h[l].rearrange("(k p) x -> p k x", p=128), src.p(l, (l,)).bufs))
                cp("dve", dst[:, :, :], stg[:, :, :])
            P.op("dve", "memset", ap=kmax[:, :], constant=0.0)
            norm_tile = make_norm(st, "a1n")
            hTr = sbring(st, "a1_hT", [128, KC, 512], BF16, 2)
            sq = sb(st, "a1_sq", [128, 2, 512], BF16)
            ckvg = sb(st, "a1_ckvg", [128, 2, 512], BF16)
            rstd = sb(st, "a1_rstd", [128, 512], F32)
            rtok = sb(st, "a1_rtok", [128, 4], F32)
            cct = sbring(st, "a1_cct", [96, 512], F32, 2)
            sst = sbring(st, "a1_sst", [96, 512], F32, 2)
            t1 = sb(st, "a1_t1", [96, 512], F32)
            t2 = sb(st, "a1_t2", [96, 512], F32)
            ktt = sbring(st, "a1_kt", [96, H, 512], BF16, 2)
            sqk = sb(st, "a1_sqk", [96, H, 512], BF16)
            vt = sbring(st, "a1_vt", [128, 4, H, 65], BF16, 2)
            f32r = sbring(st, "a1_f", [128, 512], F32, 4)
            kmt = sb(st, "a1_kmt", [128, H], F32)
            for i in range(NT):
                ts_ = slice(i * 512, (i + 1) * 512)
                hT = hTr.next()
                norm_tile(xsrc, i, hT)
                CC = cct.next(); SS = sst.next()
                dma("sp", CC[PR, :], cc_d.p("all", (slice(None), ts_)))
                dma("sp", SS[PR, :], ss_d.p("all", (slice(None), ts_)))
                chk(2.1)
                for m in range(2):
                    ps = mmring.next()
                    for kc in range(KC):
                        mm(ps[:, :], wa1[:, kc, m * 128:(m + 1) * 128], hT[:, kc, :], kc == 0, kc == KC - 1)
                    act(sq[:, m, :], ps[:, :], AF.Square)
                    act(ckvg[:, m, :], ps[:, :], AF.Copy, scale=kvng[:, m:m + 1])
                ps = auxring.next()
                for m in range(2):
                    mm(ps[:, :], ones_b[:, :], sq[:, m, :], m == 0, m == 1)
                rsqrt_from_ss(rstd[:, :], ps[:, :], 256)
                chk(2.2)
                ps = auxring.next()
                for j in range(4):
                    for m in range(2):
                        mm(ps[:, j:j + 1], sq[:, m, j * 128:(j + 1) * 128], ones_b[:, 0:1], m == 0, m == 1)
                rsqrt_from_ss(rtok[:, :], ps[:, 0:4], 256)
                chk(2.3)
                KT = ktt.next()
                for h in range(H):
                    ps = mmring.next()
                    for m in range(2):
                        mm(ps[0:64, :], wk[:, m, h * 64:(h + 1) * 64], ckvg[:, m, :], m == 0, m == 1)
                    tt("dve", KT[0:64, h, :], ps[0:64, :], rstd[0:64, :], ALU.mult)
                chk(2.4)
                psa = mmring.next()
                for kc in range(KC):
                    mm(psa[0:96, :], wa1[:, kc, 256:352], hT[:, kc, :], kc == 0, kc == KC - 1)
                psb = mmring.next()
                for kc in range(KC):
                    mm(psb[0:96, :], wa1[:, kc, 352:448], hT[:, kc, :], kc == 0, kc == KC - 1)
                tt("dve", t1[PR, :], psa[PR, :], CC[PR, :], ALU.mult)
                tt("dve", t2[PR, :], psb[PR, :], SS[PR, :], ALU.mult)
                for h in range(H):
                    tt("pool" if h % 2 else "dve", KT[PR, h, :], t1[PR, :], t2[PR, :], ALU.add)
                dma("pool", kt_d.p(i, (slice(None), slice(None), ts_)), KT[:, :, :])
                chk(2.5)
                act(sqk[:, :, :], KT[:, :, :], AF.Square)
                for h in range(H):
                    ps = auxring.next()
                    mm(ps[:, :], ones_b[0:96, :], sqk[0:96, h, :], True, True)
                    P.op("dve", "tensor_reduce", out=kmt[:, h:h + 1], in_=ps[:, :], axis=AX.X, op=ALU.max)
                tt("dve", kmax[:, :], kmax[:, :], kmt[:, :], ALU.max)
                chk(2.6)
                VT = vt.next()
                P.op("pool", "memset", ap=VT[:, :, :, 64:65], constant=1.0)
                for j in range(4):
                    ps = mmring.next()
                    for m in range(2):
                        mm(ps[:, :], ckvg[:, m, j * 128:(j + 1) * 128], wv[:, m, :], m == 0, m == 1)
                    act(VT[:, j, :, 0:64], V(ps.h[:, :].rearrange("p (h d) -> p h d", h=H), ps[:, :].bufs),
                        AF.Copy, scale=rtok[:, j:j + 1])
                dma("pool", v_d.p(i, (slice(None), slice(i * 4, i * 4 + 4))), VT[:, :, :, :])
                chk(2.7)
                for g in range(4):
                    ps = mmring.next()
                    for kc in range(KC):
                        mm(ps[:, :], wa1[:, kc, 448 + g * 128:448 + (g + 1) * 128], hT[:, kc, :], kc == 0, kc == KC - 1)
                    f = f32r.next()
                    cp("act", f[:, :], ps[:, :])
                    dma("pool", pu_d.p(i, (slice(None), g, slice(8 + i * 512, 8 + (i + 1) * 512))), f[:, :])
                chk(2.8)
                for g in range(4):
                    ps = mmring.next()
                    for kc in range(KC):
                        mm(ps[:, :], wa1[:, kc, 960 + g * 128:960 + (g + 1) * 128], hT[:, kc, :], kc == 0, kc == KC - 1)
                    f = f32r.next()
                    cp("act", f[:, :], ps[:, :])
                    ps2 = mmring.next()
                    for kc in range(KC):
                        mm(ps2[:, :], wa1[:, kc, 1472 + g * 128:1472 + (g + 1) * 128], hT[:, kc, :], kc == 0, kc == KC - 1)
                    f2 = f32r.next()
                    tt("dve", f2[:, :], ps2[:, :], f[:, :], ALU.mult)
                    dma("pool", z_d.p(i, (slice(None), g, slice(1 + i * 512, 1 + (i + 1) * 512))), f2[:, :])
        P.barrier()
        chk(3)

        with ExitStack() as st:
            KTs = sb(st, "at_KT", [96, H, S], BF16)
            Vs = sb(st, "at_V", [128, NB, H, 65], BF16)
            for i in range(NT):
                dma("sp", KTs.p(i, (slice(None), slice(None), slice(i * 512, (i + 1) * 512))),
                    kt_d.p(i, (slice(None), slice(None), slice(i * 512, (i + 1) * 512))))
                dma("sp", Vs.p(i, (slice(None), slice(i * 4, i * 4 + 4))), v_d.p(i, (slice(None), slice(i * 4, i * 4 + 4))))
            wcq = sb(st, "at_wcq", [128, KC, 384], BF16)
            dma("sp", wcq[:, :, :], wa2_d.p("all", (slice(None), slice(None), slice(0, 384))))
            wA = sb(st, "at_wA", [128, 3, 768], BF16)
            wB = sb(st, "at_wB", [128, 3, 768], BF16)
            qng = sb(st, "at_qng", [128, 3], F32)
            dma("sp", qng[:, :], qng_in.p(l, (l,)))
            with ExitStack() as stw:
                stg = sb(stw, "at_stg", [128, 3, 768], F32)
                for src, dst in ((wuqA_in, wA), (wuqB_in, wB)):
                    dma("sp", stg[:, :, :], V(src.h[l].rearrange("(k p) x -> p k x", p=128), src.p(l, (l,)).bufs))
                    cp("dve", dst[:, :, :], stg[:, :, :])
            P.barrier()
            norm_tile = make_norm(st, "atn")
            hTr = sbring(st, "at_hT", [128, KC, 512], BF16, 1)
            sq = sb(st, "at_sq", [128, 3, 512], BF16)
            cqg = sb(st, "at_cqg", [128, 3, 512], BF16)
            rstd = sb(st, "at_rstd", [128, 512], F32)
            cct = sbring(st, "at_cct", [96, 512], F32, 2)
            sst = sbring(st, "at_sst", [96, 512], F32, 2)
            ta = sbring(st, "at_ta", [96, 512], F32, 1)
            tb = sbring(st, "at_tb", [96, 512], F32, 1)
            qTr = sbring(st, "at_qT", [96, H, 512], BF16, 2)
            sqq = sbring(st, "at_sqq", [96, 512], BF16, 2)
            negb = sbring(st, "at_negb", [128, H], F32, 2)
            qm = sbring(st, "at_qm", [128, 2], F32, 4)
            ptr_ = sbring(st, "at_pt", [128, 1024], BF16, 3)
            osb = sbring(st, "at_osb", [65, 512], F32, 2)
            rec = sbring(st, "at_rec", [64, 512], F32, 2)
            oTr = sbring(st, "at_oT", [64, H, 512], BF16, 1)
            allk = list(range(NT))
            pending = [None]

            def prep(i):
                ts_ = slice(i * 512, (i + 1) * 512)
                hT = hTr.next()
                norm_tile(xsrc, i, hT)
                CC = cct.next(); SS = sst.next()
                dma("sp", CC[PR, :], cc_d.p("all", (slice(None), ts_)))
                dma("sp", SS[PR, :], ss_d.p("all", (slice(None), ts_)))
                for m in range(3):
                    ps = auxring.next()
                    for kc in range(KC):
                        mm(ps[:, :], wcq[:, kc, m * 128:(m + 1) * 128], hT[:, kc, :], kc == 0, kc == KC - 1)
                    act(sq[:, m, :], ps[:, :], AF.Square)
                    act(cqg[:, m, :], ps[:, :], AF.Copy, scale=qng[:, m:m + 1])
                ps = auxring.next()
                for m in range(3):
                    mm(ps[:, :], ones_b[:, :], sq[:, m, :], m == 0, m == 2)
                rsqrt_from_ss(rstd[:, :], ps[:, :], 384)
                tt("dve", CC[PR, :], CC[PR, :], rstd[PR, :], ALU.mult)
                tt("pool", SS[PR, :], SS[PR, :], rstd[PR, :], ALU.mult)
                return dict(CC=CC, SS=SS, qT=qTr.next(), nb=negb.next(), s2={})

            def q1(stt_, h):
                CC, SS, qT = stt_["CC"], stt_["SS"], stt_["qT"]
                psA = auxring.next()
                for m in range(3):
                    mm(psA[0:96, :], wA[:, m, h * 96:(h + 1) * 96], cqg[:, m, :], m == 0, m == 2)
                psB = auxring.next()
                for m in range(3):
                    mm(psB[0:96, :], wB[:, m, h * 96:(h + 1) * 96], cqg[:, m, :], m == 0, m == 2)
                tt("dve", qT[0:64, h, :], psA[0:64, :], rstd[0:64, :], ALU.mult)
                a_ = ta.next(); b_ = tb.next()
                tt("dve", a_[PR, :], psA[PR, :], CC[PR, :], ALU.mult)
                tt("dve", b_[PR, :], psB[PR, :], SS[PR, :], ALU.mult)
                tt("pool", qT[PR, h, :], a_[PR, :], b_[PR, :], ALU.add)
                s2 = sqq.next()
                tt("pool", s2[0:96, :], qT[0:96, h, :], qT[0:96, h, :], ALU.mult)
                stt_["s2"][h] = s2

            def q2(stt_, h):
                s2 = stt_["s2"].pop(h)
                nb_ = stt_["nb"]
                ps = auxring.next()
                mm(ps[:, :], ones_b[0:96, :], s2[0:96, :], True, True)
                q_ = qm.next()
                P.op("dve", "tensor_reduce", out=q_[:, 0:1], in_=ps[:, :], axis=AX.X, op=ALU.max)
                tt("dve", q_[:, 1:2], q_[:, 0:1], kmax[:, h:h + 1], ALU.mult)
                P.op("pool", "tensor_tensor", out=q_[:, 1:2], in0=q_[:, 1:2], in1=phalf[:, 0:1], op=ALU.pow)
                ts("dve", nb_[:, h:h + 1], q_[:, 1:2], -SCALE * 1.03, ALU.mult)

            def qorder(stt_):
                items = []
                for h in range(H):
                    items.append(lambda h=h: q1(stt_, h))
                    if h >= 1:
                        items.append(lambda h=h: q2(stt_, h - 1))
                items.append(lambda: q2(stt_, H - 1))
                return items

            cur = prep(0)
            for f_ in qorder(cur):
                f_()
            for i in range(NT):
                ts_ = slice(i * 512, (i + 1) * 512)
                qT = cur["qT"]
                nb_ = cur["nb"]
                nxt_state = None
                todo = []
                oT = oTr.next()
                for h in range(H):
                    acc = accring.next()
                    NP2 = NB // 2
                    pss = [None] * NP2

                    def qk2(kp_):
                        pss[kp_] = mm2ring.next()
                        for u in range(2):
                            kb_ = 2 * kp_ + u
                            mm(pss[kp_][:, u * 512:(u + 1) * 512], KTs.p(allk, (slice(0, 96), h, slice(kb_ * 128, (kb_ + 1) * 128))),
                               qT[0:96, h, :], True, True)

                    qk2(0)
                    for kp in range(NP2):
                        if kp + 1 < NP2:
                            qk2(kp + 1)
                        if kp == min(1, NP2 - 1) and pending[0] is not None:
                            pending[0]()
                            pending[0] = None
                        if i + 1 < NT:
                            if h == 0 and kp == min(2, NP2 - 1):
                                nxt_state = prep(i + 1)
                                todo = qorder(nxt_state)
                            elif h >= 1 and kp in (3, 8, 13) and todo:
                                todo.pop(0)()
                        pt = ptr_.next()
                        act(pt[:, :], pss[kp][:, :], AF.Exp, scale=SCALE, bias=nb_[:, h:h + 1])
                        for u in range(2):
                            kb = 2 * kp + u
                            mm(acc[0:65, :], Vs.p(allk, (slice(None), kb, h, slice(None))), pt[:, u * 512:(u + 1) * 512], kb == 0, kb == NB - 1)
                        pss[kp] = None

                    def fin(h=h, acc=acc, oT=oT):
                        o_ = osb.next()
                        cp("act", o_[0:65, :], acc[0:65, :])
                        den = auxring.next()
                        mm(den[0:64, :], sel_f[0:65, 0:64], o_[0:65, :], True, True)
                        rc_ = rec.next()
                        P.op("dve", "reciprocal", out=rc_[:, :], in_=den[0:64, :])
                        tt("pool", oT[0:64, h, :], o_[0:64, :], rc_[:, :], ALU.mult)

                    pending[0] = fin
                while todo:
                    todo.pop(0)()
                pending[0]()
                pending[0] = None
                dma("pool", ot_d.p(i, (slice(None), slice(None), ts_)), oT[:, :, :])
                cur = nxt_state
        P.barrier()
        chk(4)

        with ExitStack() as st:
            wcb = sb(st, "a2_wcb", [128, KC, 512], BF16)
            dma("sp", wcb[:, :, :], wa2_d.p("all", (slice(None), slice(None), slice(384, 896))))
            wout = sb(st, "a2_wout", [128, KC, D], BF16)
            dma("sp", wout[:, :, :], wout_d.p("all", (slice(None),)))
            wpool = sb(st, "a2_wpool", [128, 4, 256], BF16)
            stg = sb(st, "a2_stg", [128, 4, 256], F32)
            dma("sp", stg[:, :, :], wpool_in.p(l, (l,)))
            cp("dve", wpool[:, :, :], stg[:, :, :])
            bgate = sb(st, "a2_bgate", [128, 24], F32)
            pscale = sb(st, "a2_pscale", [128, KC], F32)
            convw = sb(st, "a2_convw", [128, 4, 3], F32)
            dma("sp", bgate[:, :], bgate_in.p(l, (l,)))
            dma("sp", pscale[:, :], pscale_in.p(l, (l,)))
            dma("sp", convw[:, :, :], convw_in.p(l, (l,)))
            norm_tile = make_norm(st, "a2n")
            hTr = sbring(st, "a2_hT", [128, KC, 512], BF16, 2)
            oTr = sbring(st, "a2_oT", [64, H, 512], BF16, 2)
            Ur = sbring(st, "a2_U", [128, 4, 528], F32, 2)
            Zr = sbring(st, "a2_Z", [128, 4, 514], F32, 2)
            pa = sb(st, "a2_pa", [128, 528], F32)
            pb = sb(st, "a2_pb", [128, 528], F32)
            t8 = sb(st, "a2_t8", [128, 8], F32)
            mixed = sb(st, "a2_mixed", [128, 4, 512], BF16)
            yc = sbring(st, "a2_yc", [128, 512], F32, 2)
            convo = sb(st, "a2_convo", [128, 4, 512], BF16)
            wgr = sbring(st, "a2_wg", [128, KC, 384], BF16, 2)
            woar = sbring(st, "a2_woa", [64, H, 128], BF16, 2)
            wocr = sbring(st, "a2_woc", [128, 4, 128], BF16, 2)
            gt = sbring(st, "a2_gt", [128, 3, 512], F32, 2)
            tmp = sbring(st, "a2_tmp", [128, 512], F32, 6)
            merged = sb(st, "a2_merged", [128, KC, 512], BF16)
            xr = sbring(st, "a2_xr", [128, D], F32, 2)
            xn_ = sbring(st, "a2_xnew", [128, D], F32, 2)
            for i in range(NT):
                ts_ = slice(i * 512, (i + 1) * 512)
                hT = hTr.next()
                norm_tile(xsrc, i, hT)
                oT = oTr.next()
                dma("sp", oT[:, :, :], ot_d.p(i, (slice(None), slice(None), ts_)))
                U = Ur.next(); Z = Zr.next()
                nbr = [k for k in (i - 1, i, i + 1) if 0 <= k < NT] + ["pad"]
                dma("sp", U[:, :, :], pu_d.p(nbr, (slice(None), slice(None), slice(i * 512, i * 512 + 528))))
                dma("sp", Z[:, :, :], z_d.p(nbr, (slice(None), slice(None), slice(i * 512, i * 512 + 514))))
                for g in range(4):
                    w = 2 << g
                    half = w // 2
                    cur, n = None, 528
                    bufs2 = [pa, pb]
                    src = V(U.h[:, g, :], U[:, :, :].bufs)
                    k = 1
                    bi = 0
                    while k < w:
                        dst = bufs2[bi % 2]; bi += 1
                        n2 = n - k
                        tt("dve" if g >= 2 else "pool", dst[:, 0:n2], V(src.ap[:, 0:n2], src.bufs), V(src.ap[:, k:k + n2], src.bufs), ALU.add)
                        src = V(dst.h[:, :], dst[:, :].bufs)
                        n = n2
                        k *= 2
                    sw = src
                    stt("dve", mixed[:, g, :], V(sw.ap[:, 8 - half:8 - half + 512], sw.bufs), 1.0 / w,
                        V(U.h[:, g, 8:520], U[:, :, :].bufs), ALU.mult, ALU.subtract)
                    if i == 0:
                        tt("dve", t8[:, :], V(sw.ap[:, 8 - half:16 - half], sw.bufs), invc[:, g, 0:8], ALU.mult)
                        tt("dve", mixed[:, g, 0:8], t8[:, :], V(U.h[:, g, 8:16], U[:, :, :].bufs), ALU.subtract)
                    if i == NT - 1:
                        tt("dve", t8[:, :], V(sw.ap[:, 512 - half:520 - half], sw.bufs), invc[:, g, 8:16], ALU.mult)
                        tt("dve", mixed[:, g, 504:512], t8[:, :], V(U.h[:, g, 512:520], U[:, :, :].bufs), ALU.subtract)
                for k4 in range(4):
                    y = yc.next()
                    ts("dve", y[:, :], Z[:, k4, 0:512], convw[:, k4, 0:1], ALU.mult)
                    stt("dve", y[:, :], Z[:, k4, 1:513], convw[:, k4, 1:2], y[:, :], ALU.mult, ALU.add)
                    stt("dve", y[:, :], Z[:, k4, 2:514], convw[:, k4, 2:3], y[:, :], ALU.mult, ALU.add)
                    ps = mmring.next()
                    for kc in range(KC):
                        mm(ps[:, :], wcb[:, kc, k4 * 128:(k4 + 1) * 128], hT[:, kc, :], kc == 0, kc == KC - 1)
                    tt("dve", convo[:, k4, :], ps[:, :], y[:, :], ALU.mult)
                for c in range(8):
                    wg = wgr.next(); woa = woar.next(); woc = wocr.next()
                    dma("sp", wg[:, :, :], wgt_d.p("all", (slice(None), c)))
                    dma("sp", woa[:, :, :], woa_d.p("all", (slice(None), c)))
                    dma("sp", woc[:, :, :], woc_d.p("all", (slice(None), c)))
                    G = gt.next()
                    for j in range(3):
                        ps = mmring.next()
                        for kc in range(KC):
                            mm(ps[:, :], wg[:, kc, j * 128:(j + 1) * 128], hT[:, kc, :], kc == 0, kc == KC - 1)
                        act(G[:, j, :], ps[:, :], AF.Sigmoid, bias=bgate[:, j * 8 + c:j * 8 + c + 1])
                    psa = accring.next()
                    for h in range(H):
                        mm(psa[:, :], woa[0:64, h, :], oT[0:64, h, :], h == 0, h == H - 1)
                    t0 = tmp.next()
                    tt("dve", t0[:, :], psa[:, :], G[:, 0, :], ALU.mult)
                    psb = accring.next()
                    mm(psb[:, :], wpool[:, c // 2, (c % 2) * 128:(c % 2) * 128 + 128], mixed[:, c // 2, :], True, True)
                    t1_ = tmp.next()
                    stt("dve", t1_[:, :], psb[:, :], pscale[:, c:c + 1], G[:, 1, :], ALU.mult, ALU.mult)
                    psc = auxring.next()
                    for k4 in range(4):
                        mm(psc[:, :], woc[:, k4, :], convo[:, k4, :], k4 == 0, k4 == 3)
                    t2_ = tmp.next()
                    tt("dve", t2_[:, :], psc[:, :], G[:, 2, :], ALU.mult)
                    tt("pool", t0[:, :], t0[:, :], t1_[:, :], ALU.add)
                    tt("pool", merged[:, c, :], t0[:, :], t2_[:, :], ALU.add)
                for j in range(4):
                    blk = i * 4 + j
                    xt = xr.next(); xo = xn_.next()
                    rows = slice(blk * 128, blk * 128 + 128)
                    dma("sp", xt[:, :], xsrc.p(blk, (rows, slice(None))))
                    for hf in range(2):
                        ps = mmring.next()
                        for kc in range(KC):
                            mm(ps[:, :], merged[:, kc, j * 128:(j + 1) * 128], wout[:, kc, hf * 512:(hf + 1) * 512], kc == 0, kc == KC - 1)
                        tt("dve", xo[:, hf * 512:(hf + 1) * 512], ps[:, :], xt[:, hf * 512:(hf + 1) * 512], ALU.add)
                    dma("pool", xm_d.p(blk, (rows, slice(None))), xo[:, :])
                    if debug and l == 0:
                        dma("pool", dbg["dbg_x1"].p(blk, (rows, slice(None))), xo[:, :])
        P.barrier()
        chk(5)

        with ExitStack() as st:
            g2 = sb(st, "b_g2", [128, D], F32)
            aff = sb(st, "b_aff", [128, NB, NE], F32)
            slotm = sb(st, "b_slotm", [128, NB, NE], F32)
            tinfo = sb(st, "b_tinfo", [128, NB, NE, 5], BF16)
            allb = list(range(NB))
            with ExitStack() as st1:
                gm2 = sb(st1, "b_gm2", [128, D], F32)
                sh2 = sb(st1, "b_sh2", [128, D], F32)
                srow = sb(st1, "b1_srow", [1, D], F32)
                n2row = sb(st1, "b1_n2row", [1, D], F32)
                n2bc = sb(st1, "b1_n2bc", [128, D], F32)
                bc_row(sh2, 3 * D, st1, srow)
                bc_row(gm2, 4 * D, st1, srow)
                bc_row(g2, 5 * D, st1, srow)
                dma("sp", n2row[:, :], n2g_in.p(l, (l,)))
                for hf in range(2):
                    ps = auxring.next()
                    mm(ps[:, :], ones_f[0:1, :], n2row[0:1, hf * 512:(hf + 1) * 512], True, True)
                    cp("dve", n2bc[:, hf * 512:(hf + 1) * 512], ps[:, :])
                ts("dve", gm2[:, :], gm2[:, :], 1.0, ALU.add)
                tt("dve", gm2[:, :], gm2[:, :], n2bc[:, :], ALU.mult)
                wr = sb(st1, "b1_wr", [128, KC, NE], F32)
                dma("sp", wr[:, :, :], wr_in.p(l, (l,)))
                xr = sbring(st1, "b1_x", [128, D], F32, 2)
                sqj = sb(st1, "b1_sqj", [128, D], BF16)
                rs = sbring(st1, "b1_rs", [128, 8], F32, 2)
                h2r = sbring(st1, "b1_h2", [128, D], F32, 2)
                h2br = sbring(st1, "b1_h2b", [128, D], BF16, 2)
                h2Tr = sbring(st1, "b1_h2T", [128, KC, 128], F32, 2)
                ex = sbring(st1, "b1_ex", [128, NE], F32, 2)
                for b in range(NB):
                    rows = slice(b * 128, b * 128 + 128)
                    xt = xr.next(); r = rs.next()
                    dma("sp", xt[:, :], xm_d.p(b, (rows, slice(None))))
                    act(sqj[:, :], xt[:, :], AF.Square, accum_out=r[:, 0:1])
                    rsqrt_from_ss(r[:, 1:2], r[:, 0:1], D)
                    h2 = h2r.next()
                    stt("dve", h2[:, :], xt[:, :], r[:, 1:2], gm2[:, :], ALU.mult, ALU.mult)
                    tt("pool", h2[:, :], h2[:, :], sh2[:, :], ALU.add)
                    h2b = h2br.next()
                    cp("act", h2b[:, :], h2[:, :])
                    dma("pool", h2_d.p(b, (rows, slice(None))), h2b[:, :])
                    h2T = h2Tr.next()
                    for hf in range(2):
                        ps = mmring.next()
                        for k4 in range(4):
                            kc = hf * 4 + k4
                            P.op("pe", "transpose", out=ps[:, k4 * 128:(k4 + 1) * 128], in_=h2[:, kc * 128:(kc + 1) * 128],
                                 identity=ident_f[:, :])
                        cp("dve" if hf else "act", V(h2T.h[:, hf * 4:(hf + 1) * 4, :], h2T[:, :, :].bufs),
                           V(ps.h[:, :].rearrange("p (k t) -> p k t", k=4), ps[:, :].bufs))
                    ps = auxring.next()
                    for kc in range(KC):
                        mm(ps[:, 0:NE], h2T[:, kc, :], wr[:, kc, :], kc == 0, kc == KC - 1)
                    P.op("dve", "tensor_reduce", out=r[:, 2:3], in_=ps[:, 0:NE], axis=AX.X, op=ALU.max)
                    ts("dve", r[:, 3:4], r[:, 2:3], -1.0, ALU.mult)
                    e_ = ex.next()
                    act(e_[:, :], ps[:, 0:NE], AF.Exp, bias=r[:, 3:4], accum_out=r[:, 4:5])
                    P.op("dve", "reciprocal", out=r[:, 5:6], in_=r[:, 4:5])
                    ts("dve", aff.p(b, (slice(None), b, slice(None))), e_[:, :], r[:, 5:6], ALU.mult)
            P.barrier()
            chk(6)
            with ExitStack() as st2:
                W = NB * NE
                lo = sb(st2, "b2_lo", [128, NE], F32)
                hi = sb(st2, "b2_hi", [128, NE], F32)
                mid = sb(st2, "b2_mid", [128, NE], F32)
                c16 = sb(st2, "b2_c16", [128, NE], F32)
                ge = sb(st2, "b2_ge", [128, NE], F32)
                t16 = sb(st2, "b2_t16", [128, NE], F32)
                cmpb = sb(st2, "b2_cmp", [128, NB, NE], BF16)
                maskf = sb(st2, "b2_maskf", [128, NB, NE], F32)
                offs = sb(st2, "b2_offs", [128, NB, NE], F32)
                r1 = sb(st2, "b2_r1", [128, NB, NE], F32)
                r2 = sb(st2, "b2_r2", [128, NB, NE], F32)
                affv = aff.p(allb, (slice(None),))
                P.op("dve", "memset", ap=lo[:, :], constant=0.0)

                def bcast_e(t):
                    return V(t.h[:, :].unsqueeze(1).to_broadcast([128, NB, NE]), t[:, :].bufs)

                for it in range(30):
                    hstep = 2.0 ** -(it + 1)
                    ts("dve", mid[:, :], lo[:, :], hstep, ALU.add)
                    tt("dve", cmpb[:, :, :], affv, bcast_e(mid), ALU.is_ge)
                    ps = auxring.next()
                    mm(ps[:, 0:W], ones_b[:, :], V(cmpb.h[:, :, :].rearrange("p b e -> p (b e)"), cmpb[:, :, :].bufs), True, True)
                    P.op("dve", "tensor_reduce", out=c16[:, :], in_=V(ps.h[:, 0:W].rearrange("p (b e) -> p e b", e=NE), ps[:, :].bufs),
                         axis=AX.X, op=ALU.add)
                    ts("dve", ge[:, :], c16[:, :], float(CAP) - 0.5, ALU.is_ge)
                    tt("dve", t16[:, :], mid[:, :], ge[:, :], ALU.mult)
                    tt("dve", lo[:, :], lo[:, :], t16[:, :], ALU.max)
                tt("dve", maskf[:, :, :], affv, bcast_e(lo), ALU.is_ge)
                cp("dve", cmpb[:, :, :], maskf[:, :, :])
                cmpflat = V(cmpb.h[:, :, :].rearrange("p b e -> p (b e)"), cmpb[:, :, :].bufs)
                psw = auxring.next()
                mm(psw[:, 0:W], ltri_b[:, :], cmpflat, True, True)
                pst = auxring.next()
                mm(pst[:, 0:W], ones_b[:, :], cmpflat, True, True)
                cp("dve", V(r1.h[:, :, :].rearrange("p b e -> p (b e)"), r1[:, :, :].bufs), pst[:, 0:W])
                P.op("dve", "memset", ap=offs[:, 0, :], constant=0.0)
                for b in range(1, NB):
                    tt("dve", offs[:, b, :], offs[:, b - 1, :], r1[:, b - 1, :], ALU.add)
                tt("dve", V(r2.h[:, :, :].rearrange("p b e -> p (b e)"), r2[:, :, :].bufs), psw[:, 0:W],
                   V(offs.h[:, :, :].rearrange("p b e -> p (b e)"), offs[:, :, :].bufs), ALU.add)
                stt("dve", r2[:, :, :], r2[:, :, :], 1.0, maskf[:, :, :], ALU.add, ALU.mult)
                ts("dve", slotm[:, :, :], r2[:, :, :], -1.0, ALU.add)
                for q in range(2):
                    cp("dve", V(tinfo.h[:, :, :, q], tinfo[:, :, :, :].bufs),
                       V(tokhl_b.h[:, :, q:q + 1].to_broadcast([128, NB, NE]), tokhl_b[:, :, :].bufs))
                a1v = V(tinfo.h[:, :, :, 2], tinfo[:, :, :, :].bufs)
                a2v = V(tinfo.h[:, :, :, 3], tinfo[:, :, :, :].bufs)
                a3v = V(tinfo.h[:, :, :, 4], tinfo[:, :, :, :].bufs)
                cp("dve", a1v, affv)
                tt("dve", r1[:, :, :], affv, a1v, ALU.subtract)
                cp("dve", a2v, r1[:, :, :])
                tt("dve", r2[:, :, :], r1[:, :, :], a2v, ALU.subtract)
                cp("dve", a3v, r2[:, :, :])
            P.barrier()
            chk(7)
            with ExitStack() as st3:
                stgr = sbring(st3, "b3_stg", [128, 4, D], F32, 4)
                wbr = sbring(st3, "b3_w", [128, KC, D], BF16, 4)
                ohr = sbring(st3, "b3_oh", [128, CAP], BF16, 8)
                idxf = sbring(st3, "b3_idxf", [128, SCH], F32, 2)
                pcsr = sbring(st3, "b3_pcs", [128, SCH * 8], F32, 2)
                idxi = sbring(st3, "b3_idxi", [128, SCH], I32, 2)
                val = sbring(st3, "b3_val", [128, SCH], F32, 2)
                xgr = sbring(st3, "b3_xg", [128, SCH, D], BF16, 2)
                sgr = sbring(st3, "b3_sg", [128, CAP], F32, 1)
                aT = sb(st3, "b3_aT", [128, KC, CAP], BF16)
                ysr = sbring(st3, "b3_ys", [128, D], F32, 2)
                cengs = ["dve", "act"]
                ci = [0]

                def loadw_issue(src, e):
                    wt = wbr.next()
                    stgs = []
                    for hf in range(2):
                        s_ = stgr.next()
                        dma("sp", s_[:, :, :], V(src.h[l, e, hf * 512:(hf + 1) * 512, :].rearrange("(k p) x -> p k x", p=128),
                                                 src.p((l, e), (l, e)).bufs))
                        stgs.append(s_)
                    return (wt, stgs)

                def loadw_cast(lw):
                    wt, stgs = lw
                    for hf, s_ in enumerate(stgs):
                        ci[0] += 1
                        cp(cengs[ci[0] % 2], V(wt.h[:, hf * 4:(hf + 1) * 4, :], wt[:, :, :].bufs), s_[:, :, :])
                    return wt

                def loadw(src, e):
                    return loadw_cast(loadw_issue(src, e))

                xgTr = sbring(st3, "b3_xgTr", [128, KC, CAP], BF16, 2)

                def A1_init(e):
                    pidx = accring.next()
                    mm(pidx[:, 0:SCH * 8], zeros_b[:, :], zeros_b[:, 0:SCH * 8], True, False, skip_group_check=True)
                    return dict(e=e, pidx=pidx, ohs=[])

                def A1_gen(sa_, b):
                    oh = ohr.next()
                    ts("dve", oh[:, :], iota_f[:, 0:CAP], slotm[:, b, sa_["e"]:sa_["e"] + 1], ALU.is_equal)
                    sa_["ohs"].append((b, oh))

                def A1_mm(sa_):
                    for b, oh in sa_["ohs"]:
                        for sc in range(SCH):
                            mm(sa_["pidx"][:, sc * 8:sc * 8 + 5], oh[:, sc * 128:(sc + 1) * 128], tinfo[:, b, sa_["e"], :], False, b == NB - 1,
                               skip_group_check=True)
                    sa_["ohs"] = []

                def A1_fin(sa_):
                    pidx = sa_["pidx"]
                    pcs = pcsr.next()
                    cp("act", pcs[:, :], pidx[:, 0:SCH * 8])
                    pv = V(pcs.h[:, :].rearrange("p (s c) -> p s c", c=8), pcs[:, :].bufs)
                    xf = idxf.next(); xi = idxi.next(); vl = val.next()
                    stt("dve", xf[:, :], V(pv.ap[:, :, 0], pv.bufs), 64.0, V(pv.ap[:, :, 1], pv.bufs), ALU.mult, ALU.add)
                    cp("dve", xi[:, :], xf[:, :])
                    tt("dve", vl[:, :], V(pv.ap[:, :, 2], pv.bufs), V(pv.ap[:, :, 3], pv.bufs), ALU.add)
                    tt("dve", vl[:, :], vl[:, :], V(pv.ap[:, :, 4], pv.bufs), ALU.add)
                    xg = xgr.next()
                    for sc in range(SCH):
                        P.op("pool", "indirect_dma_start", dma=True, reads=[xi[:, :], h2_d.p(allb, (slice(None),))],
                             out=xg[:, sc, :], out_offset=None, in_=h2_d.h[:, :],
                             in_offset=bass.IndirectOffsetOnAxis(ap=xi.h[:, sc:sc + 1], axis=0))
                    sa_.update(xi=xi, vl=vl, xg=xg)

                def stageA1(e):
                    sa_ = A1_init(e)
                    for b in range(NB):
                        A1_gen(sa_, b)
                        A1_mm(sa_)
                    A1_fin(sa_)
                    return sa_

                def stageA2(sa):
                    xg = sa["xg"]
                    xgT_ = xgTr.next()
                    for sc in range(SCH):
                        pt = auxring.next()
                        ptv = pt.h[:, :].bitcast(BF16)
                        for kc in range(KC):
                            P.op("pe", "transpose", out=V(ptv[:, kc * 128:(kc + 1) * 128], pt[:, :].bufs),
                                 in_=xg[:, sc, kc * 128:(kc + 1) * 128], identity=ident_b[:, :])
                        cp("act" if sc % 2 else "dve", V(xgT_.h[:, :, sc * 128:(sc + 1) * 128], xgT_[:, :, :].bufs),
                           V(ptv.rearrange("p (k t) -> p k t", k=KC), pt[:, :].bufs))
                    sa["xgT"] = xgT_

                def stage_up(sa, Wg, Wu, mid=None, nxt_sa=None):
                    xgT_ = sa["xgT"]
                    for fc in range(KC):
                        if fc == 4 and mid is not None:
                            mid()
                        if nxt_sa is not None:
                            A1_mm(nxt_sa)
                            for b in range(fc * NB // KC, (fc + 1) * NB // KC):
                                A1_gen(nxt_sa, b)
                        psg = mmring.next()
                        for kc in range(KC):
                            mm(psg[:, 0:CAP], Wg[:, kc, fc * 128:(fc + 1) * 128], xgT_[:, kc, :], kc == 0, kc == KC - 1)
                        psu = mmring.next()
                        for kc in range(KC):
                            mm(psu[:, 0:CAP], Wu[:, kc, fc * 128:(fc + 1) * 128], xgT_[:, kc, :], kc == 0, kc == KC - 1)
                        sg = sgr.next()
                        act(sg[:, :], psg[:, 0:CAP], AF.Silu)
                        tt("dve", aT[:, fc, :], psu[:, 0:CAP], sg[:, :], ALU.mult)
                    if nxt_sa is not None:
                        A1_mm(nxt_sa)
                        A1_fin(nxt_sa)

                prev_sc = [[]]

                def stage_down(sa, Wd, mid=None):
                    xi, vl = sa["xi"], sa["vl"]
                    mine = []
                    for sc in range(SCH):
                        if sc == (SCH + 1) // 2 and mid is not None:
                            mid()
                            mid = None
                        ys = ysr.next()
                        for hf in range(2):
                            ps = mmring.next()
                            for fc in range(KC):
                                mm(ps[:, :], aT[:, fc, sc * 128:(sc + 1) * 128], Wd[:, fc, hf * 512:(hf + 1) * 512], fc == 0, fc == KC - 1)
                            stt("dve", ys[:, hf * 512:(hf + 1) * 512], ps[:, :], vl[:, sc:sc + 1], g2[:, hf * 512:(hf + 1) * 512],
                                ALU.mult, ALU.mult)
                        mine.append(P.op("pool", "indirect_dma_start", dma=True, reads=[xi[:, :], ys[:, :]], deps_extra=prev_sc[0],
                                         out=xm_d.h[:, :], out_offset=bass.IndirectOffsetOnAxis(ap=xi.h[:, sc:sc + 1], axis=0),
                                         in_=ys.h[:, :], in_offset=None, compute_op=ALU.add))
                    if mid is not None:
                        mid()
                    prev_sc[0] = mine

                sa = stageA1(0)
                Wg = loadw(wg_in, 0)
                Wu = loadw(wu_in, 0)
                Wd = loadw(wd_in, 0)
                stageA2(sa)
                for e in range(NE):
                    nxt = e + 1 < NE
                    if nxt:
                        sb_ = A1_init(e + 1)
                        lg = loadw_issue(wg_in, e + 1)
                        stage_up(sa, Wg, Wu, mid=lambda: loadw_cast(lg), nxt_sa=sb_)
                        lu = loadw_issue(wu_in, e + 1)
                        stage_down(sa, Wd, mid=lambda: loadw_cast(lu))
                        Wd2 = loadw(wd_in, e + 1)
                        stageA2(sb_)
                        sa, Wg, Wu, Wd = sb_, lg[0], lu[0], Wd2
                    else:
                        stage_up(sa, Wg, Wu)
                        stage_down(sa, Wd)
        P.barrier()
        chk(8)

    except (_Stop, StopBuild):
        P.limit = None
    with ExitStack() as st:
        frow = sb(st, "f_row", [1, D], F32)
        fbc = sb(st, "f_bc", [128, D], F32)
        dma("sp", frow[:, :], fg_in[:, :])
        for hf in range(2):
            ps = auxring.next()
            mm(ps[:, :], ones_f[0:1, :], frow[0:1, hf * 512:(hf + 1) * 512], True, True)
            cp("dve", fbc[:, hf * 512:(hf + 1) * 512], ps[:, :])
        xr = sbring(st, "f_x", [128, D], F32, 3)
        sqj = sb(st, "f_sqj", [128, D], BF16)
        rs = sbring(st, "f_rs", [128, 2], F32, 3)
        yo = sbring(st, "f_y", [128, D], F32, 3)
        for b in range(NB):
            rows = slice(b * 128, b * 128 + 128)
            xt = xr.next(); r = rs.next(); y = yo.next()
            dma("sp", xt[:, :], xm_d.p(b, (rows, slice(None))))
            act(sqj[:, :], xt[:, :], AF.Square, accum_out=r[:, 0:1])
            rsqrt_from_ss(r[:, 1:2], r[:, 0:1], D)
            stt("dve", y[:, :], xt[:, :], r[:, 1:2], fbc[:, :], ALU.mult, ALU.mult)
            dma("pool", out_d.p(b, (rows, slice(None))), y[:, :])

    P.emit(top)
    return nc, top


def host_consts(S):
    NB = S // 128
    f = np.float32
    ident = np.eye(128, dtype=f)
    ltri = (np.arange(128)[:, None] < np.arange(128)[None, :]).astype(f)
    iota = np.tile(np.arange(512, dtype=f)[None, :], (128, 1))
    t = (np.arange(NB)[None, :] * 128 + np.arange(128)[:, None])
    tokhl = np.stack([(t // 64).astype(f), (t % 64).astype(f)], axis=-1)
    invc = np.zeros((128, 4, 16), f)
    for g in range(4):
        half = 1 << g
        for n in range(8):
            invc[:, g, n] = 1.0 / (min(n, half) + half)
            r = 7 - n
            invc[:, g, 8 + n] = 1.0 / (min(half - 1, r) + half + 1)
    ropec = np.zeros((96, 4), f)
    freqs = (10000.0 ** (-np.arange(0, 32, 2, dtype=np.float32) / np.float32(32))).astype(f)
    for i in range(16):
        for base in (64, 80):
            ropec[base + i, 0] = freqs[i]
            ropec[base + i, 1] = np.pi / 2
        ropec[64 + i, 2] = np.pi
        ropec[80 + i, 2] = 0.0
    return dict(ident=ident, ltri=ltri, iota=iota, tokhl=np.ascontiguousarray(tokhl), invc=invc, ropec=ropec)


def host_layout(inp, L):
    f = np.float32
    A = lambda a: np.ascontiguousarray(np.asarray(a), dtype=f)
    w_in = A(inp["w_in"])[:L]
    w_in_ext = np.concatenate([w_in, w_in[:, :, 576:640], w_in[:, :, 656:672], w_in[:, :, 640:656]], axis=2)
    w_uq = A(inp["w_uq"])[:L].reshape(L, 384, H, 96)
    wuqB = np.concatenate([w_uq[..., 0:64], w_uq[..., 80:96], w_uq[..., 64:80]], axis=-1)
    w_ukv = A(inp["w_ukv"])[:L].reshape(L, 256, H, 128)
    pp = lambda a, k: np.ascontiguousarray(A(a)[:L].reshape(L, k, 128).transpose(0, 2, 1))
    d = dict(
        w_mod=A(inp["w_mod"])[:L], b_mod=A(inp["b_mod"])[:L].reshape(L, 1, 6 * D),
        n1g=pp(inp["norm1_g"], KC), n2g=A(inp["norm2_g"])[:L].reshape(L, 1, D),
        w_in=np.ascontiguousarray(w_in_ext),
        bgate=pp(inp["b_gate"], 24), qng=pp(inp["q_norm_g"], 3), kvng=pp(inp["kv_norm_g"], 2),
        wuqA=np.ascontiguousarray(w_uq.reshape(L, 384, 768)), wuqB=np.ascontiguousarray(wuqB.reshape(L, 384, 768)),
        wk=np.ascontiguousarray(w_ukv[..., 0:64].reshape(L, 256, 512)),
        wv=np.ascontiguousarray(w_ukv[..., 64:128].reshape(L, 256, 512)),
        woa=np.ascontiguousarray(A(inp["w_oa"])[:L].reshape(L, H, 64, D).transpose(0, 2, 1, 3)),
        wpool=np.ascontiguousarray(A(inp["w_pool"])[:L].transpose(0, 2, 1, 3)),
        pscale=pp(inp["pool_scale"], KC),
        convw=np.ascontiguousarray(A(inp["conv_w"])[:L].reshape(L, 3, 4, 128).transpose(0, 3, 2, 1)),
        woc=A(inp["w_oc"])[:L], wout=A(inp["w_out"])[:L],
        wr=np.ascontiguousarray(A(inp["w_router"])[:L].reshape(L, KC, 128, NE).transpose(0, 2, 1, 3)),
        w_gate=A(inp["w_gate"])[:L], w_up=A(inp["w_up"])[:L], w_down=A(inp["w_down"])[:L],
        fg=A(inp["final_g"]).reshape(1, D),
    )
    return d


_CACHE = {}


def run(inp, S, L, debug=False, trace=False, stop=99):
    B = np.asarray(inp["x"]).shape[0]
    key = (S, L, debug, stop)
    if key not in _CACHE:
        _CACHE[key] = build(S, L, debug, stop)
    nc, _ = _CACHE[key]
    shared = host_layout(inp, L)
    shared.update(host_consts(S))
    x = np.ascontiguousarray(np.asarray(inp["x"]), dtype=np.float32)
    c = np.asarray(inp["c"], dtype=np.float32)
    pos = np.asarray(inp["positions"]).astype(np.int32)
    in_maps = []
    ncores = int(_os.environ.get('KCORES', '8'))
    for core in range(ncores):
        b = core % B
        m = dict(shared)
        m["x"] = x[b]
        m["cT"] = np.ascontiguousarray(c[b].reshape(KC, 128).T)
        m["pos"] = np.ascontiguousarray(pos[b].reshape(1, S))
        in_maps.append(m)
    res = run_bass_kernel_spmd(nc, in_maps, core_ids=list(range(ncores)), **({"trace": True} if trace else {}))
    return res


def kernel(**inputs):
    S = np.asarray(inputs["x"]).shape[1]
    B = np.asarray(inputs["x"]).shape[0]
    L = np.asarray(inputs["w_mod"]).shape[0]
    res = run(inputs, S, L)
    return np.stack([np.asarray(res.results[b]["out"], dtype=np.float32) for b in range(B)], axis=0)
```

```python
import math
import os as _os
from contextlib import ExitStack

import numpy as np
import concourse.bass as bass
import concourse.mybir as mybir
from concourse.bass_utils import run_bass_kernel_spmd

F32 = mybir.dt.float32
BF16 = mybir.dt.bfloat16
I32 = mybir.dt.int32
ALU = mybir.AluOpType
AF = mybir.ActivationFunctionType
AX = mybir.AxisListType

D = 1024
KC = 8
H = 8
NE = 16
EPS = 1e-6
SCALE = 96 ** -0.5
WIN_EXT = 5888


class Buf:
    __slots__ = ("last_w", "readers", "dma_readers")

    def __init__(self):
        self.last_w = None
        self.readers = {}
        self.dma_readers = []


class V:
    __slots__ = ("ap", "bufs")

    def __init__(self, ap, bufs):
        self.ap = ap
        self.bufs = bufs


class Tile:
    def __init__(self, handle):
        self.h = handle
        self.bufs = {}

    def buf(self, key):
        b = self.bufs.get(key)
        if b is None:
            b = self.bufs[key] = Buf()
        return b

    def __getitem__(self, idx):
        return V(self.h[idx], [self.buf(None)])

    def p(self, key, idx):
        keys = key if isinstance(key, (list, tuple)) else [key]
        return V(self.h[idx], [self.buf(k) for k in keys])


class Op:
    __slots__ = ("eng", "meth", "kw", "deps", "is_dma", "has_dep", "sig")


WRITE_KEYS = ("out", "accum_out", "ap")
ENGS = ("pe", "act", "dve", "pool", "sp")


class StopBuild(Exception):
    pass


class Prog:
    def __init__(self, nc):
        self.nc = nc
        self.ops = []
        self.last = {}
        self.recent_dma = {e: [] for e in ENGS}
        self.R = 8

    limit = None

    def op(self, eng, meth, reads=(), writes=(), dma=False, deps_extra=(), **kw):
        if self.limit is not None and len(self.ops) >= self.limit:
            self.limit = None
            raise StopBuild()
        rd, wr = [], []
        for k, v in list(kw.items()):
            if isinstance(v, V):
                (wr if k in WRITE_KEYS else rd).extend(v.bufs)
                kw[k] = v.ap
        for v in reads:
            rd.extend(v.bufs)
        for v in writes:
            wr.extend(v.bufs)
        o = Op()
        o.eng, o.meth, o.kw, o.is_dma, o.has_dep, o.sig = eng, meth, kw, dma, False, None
        deps = {}

        def add(d):
            if d is None or d is o:
                return
            if (not dma) and eng == "pe" and d.eng == "pe" and not d.is_dma:
                return
            deps[id(d)] = d

        for d_ in deps_extra:
            add(d_)
        for b in rd:
            add(b.last_w)
        for b in wr:
            add(b.last_w)
            for r in b.readers.values():
                add(r)
            for r in b.dma_readers:
                add(r)
        o.deps = list(deps.values())
        for d in o.deps:
            d.has_dep = True
        for b in rd:
            if dma:
                b.dma_readers.append(o)
                if len(b.dma_readers) > 64:
                    b.dma_readers = b.dma_readers[-64:]
            else:
                b.readers[eng] = o
        for b in wr:
            b.last_w = o
            b.readers = {}
            b.dma_readers = []
        self.ops.append(o)
        if dma:
            lst = self.recent_dma[eng]
            lst.append(o)
            if len(lst) > self.R:
                lst.pop(0)
        else:
            self.last[eng] = o
        return o

    def barrier(self):
        alld = [o for o in self.last.values()]
        for lst in self.recent_dma.values():
            alld.extend(lst)
        for d in alld:
            d.has_dep = True
        for e in ENGS:
            o = Op()
            o.eng, o.meth, o.kw, o.is_dma, o.has_dep, o.sig = e, None, {}, False, False, None
            o.deps = list(alld)
            self.ops.append(o)

    def emit(self, stack):
        nc = self.nc
        engobj = {"pe": nc.tensor, "act": nc.scalar, "dve": nc.vector, "pool": nc.gpsimd, "sp": nc.sync}
        nsem = [0]

        def new_sem(tag):
            nsem[0] += 1
            return stack.enter_context(nc.semaphore(f"s_{tag}_{nsem[0]}"))

        sem_state = {}
        waited = {e: {} for e in ENGS}
        dma_cnt = {e: 0 for e in ENGS}
        dma_sems = {}

        def wait(eng, sem, val):
            w = waited[eng]
            k = id(sem)
            if w.get(k, 0) >= val:
                return
            engobj[eng].wait_ge(sem, val)
            w[k] = val

        keep = []
        for o in self.ops:
            e = engobj[o.eng]
            for d in o.deps:
                wait(o.eng, d.sig[0], d.sig[1])
            if o.meth is None:
                continue
            if o.is_dma:
                if o.eng not in dma_sems:
                    dma_sems[o.eng] = [new_sem("d" + o.eng) for _ in range(self.R)]
                j = dma_cnt[o.eng]
                dma_cnt[o.eng] += 1
                sem = dma_sems[o.eng][j % self.R]
                val = 16 * (j // self.R + 1)
                if j >= self.R:
                    wait(o.eng, sem, val - 16)
                ins = getattr(e, o.meth)(**o.kw)
                ins.then_inc(sem, 16)
                o.sig = (sem, val)
            else:
                ins = getattr(e, o.meth)(**o.kw)
                if o.has_dep:
                    st = sem_state.get(o.eng)
                    if st is None or st[1] >= 30000:
                        st = sem_state[o.eng] = [new_sem(o.eng), 0]
                    st[1] += 1
                    ins.then_inc(st[0], 1)
                    o.sig = (st[0], st[1])
            o.kw = None
        for eng in ENGS:
            for q, sems in dma_sems.items():
                n = dma_cnt[q]
                for sl, sem in enumerate(sems):
                    cnt = (n - sl + self.R - 1) // self.R if n > sl else 0
                    if cnt > 0:
                        wait(eng, sem, 16 * cnt)
            for e2, st in sem_state.items():
                if st[1] > 0:
                    wait(eng, st[0], st[1])


class Ring:
    def __init__(self, tiles):
        self.t = tiles
        self.i = 0

    def next(self):
        t = self.t[self.i % len(self.t)]
        self.i += 1
        return t


def build(S, L, debug=False, stop=99):
    NT = S // 512
    NB = S // 128
    CAP = 2 * S // NE
    SCH = CAP // 128
    nc = bass.Bass("TRN2", target_bir_lowering=False)
    P = Prog(nc)
    if _os.environ.get('KLIMIT'):
        P.limit = int(_os.environ['KLIMIT'])
    top = ExitStack()

    def dram(name, shape, dt, kind):
        return Tile(nc.dram_tensor(name, list(shape), dt, kind=kind))

    uniq = [0]

    def sb(stack, name, shape, dt):
        uniq[0] += 1
        return Tile(stack.enter_context(nc.sbuf_tensor(f"sb{uniq[0]}_{name}", list(shape), dt)))

    def sbring(stack, name, shape, dt, n):
        return Ring([sb(stack, f"{name}{i}", shape, dt) for i in range(n)])

    EI = "ExternalInput"
    x_in = dram("x", [S, D], F32, EI)
    cT_in = dram("cT", [128, KC], F32, EI)
    pos_in = dram("pos", [1, S], I32, EI)
    w_mod = dram("w_mod", [L, D, 6 * D], F32, EI)
    b_mod = dram("b_mod", [L, 1, 6 * D], F32, EI)
    n1g_in = dram("n1g", [L, 128, KC], F32, EI)
    n2g_in = dram("n2g", [L, 1, D], F32, EI)
    w_in = dram("w_in", [L, D, WIN_EXT], F32, EI)
    bgate_in = dram("bgate", [L, 128, 24], F32, EI)
    qng_in = dram("qng", [L, 128, 3], F32, EI)
    kvng_in = dram("kvng", [L, 128, 2], F32, EI)
    wuqA_in = dram("wuqA", [L, 384, 768], F32, EI)
    wuqB_in = dram("wuqB", [L, 384, 768], F32, EI)
    wk_in = dram("wk", [L, 256, 512], F32, EI)
    wv_in = dram("wv", [L, 256, 512], F32, EI)
    woa_in = dram("woa", [L, 64, H, D], F32, EI)
    wpool_in = dram("wpool", [L, 128, 4, 256], F32, EI)
    pscale_in = dram("pscale", [L, 128, KC], F32, EI)
    convw_in = dram("convw", [L, 128, 4, 3], F32, EI)
    woc_in = dram("woc", [L, 512, D], F32, EI)
    wout_in = dram("wout", [L, D, D], F32, EI)
    wr_in = dram("wr", [L, 128, KC, NE], F32, EI)
    wg_in = dram("w_gate", [L, NE, D, D], F32, EI)
    wu_in = dram("w_up", [L, NE, D, D], F32, EI)
    wd_in = dram("w_down", [L, NE, D, D], F32, EI)
    fg_in = dram("fg", [1, D], F32, EI)
    ident_in = dram("ident", [128, 128], F32, EI)
    ltri_in = dram("ltri", [128, 128], F32, EI)
    iota_in = dram("iota", [128, 512], F32, EI)
    tokhl_in = dram("tokhl", [128, NB, 2], F32, EI)
    invc_in = dram("invc", [128, 4, 16], F32, EI)
    ropec_in = dram("ropec", [96, 4], F32, EI)
    out_d = dram("out", [S, D], F32, "ExternalOutput")

    IN = "Internal"
    xm_d = dram("xm_d", [S, D], F32, IN)
    cc_d = dram("cc_d", [32, S], F32, IN)
    ss_d = dram("ss_d", [32, S], F32, IN)
    mod_d = dram("mod_d", [1, 6 * D], F32, IN)
    wa1_d = dram("wa1_d", [128, KC, 1984], BF16, IN)
    wa2_d = dram("wa2_d", [128, KC, 896], BF16, IN)
    wgt_d = dram("wgt_d", [128, 8, KC, 384], BF16, IN)
    woa_d = dram("woa_d", [64, 8, H, 128], BF16, IN)
    woc_d = dram("woc_d", [128, 8, 4, 128], BF16, IN)
    wout_d = dram("wout_d", [128, KC, D], BF16, IN)
    kt_d = dram("kt_d", [96, H, S], BF16, IN)
    v_d = dram("v_d", [128, NB, H, 65], BF16, IN)
    ot_d = dram("ot_d", [64, H, S], BF16, IN)
    pu_d = dram("pu_d", [128, 4, S + 16], F32, IN)
    z_d = dram("z_d", [128, 4, S + 2], F32, IN)
    h2_d = dram("h2_d", [S, D], BF16, IN)
    dbg = {}
    if debug:
        dbg["dbg_x1"] = dram("dbg_x1", [S, D], F32, "ExternalOutput")

    ident_f = sb(top, "ident_f", [128, 128], F32)
    ident_b = sb(top, "ident_b", [128, 128], BF16)
    ones_b = sb(top, "ones_b", [128, 128], BF16)
    ones_f = sb(top, "ones_f", [128, 128], F32)
    zeros_b = sb(top, "zeros_b", [128, 128], BF16)
    sel_f = sb(top, "sel_f", [65, 64], F32)
    ltri_b = sb(top, "ltri_b", [128, 128], BF16)
    iota_f = sb(top, "iota_f", [128, 512], F32)
    tokhl_b = sb(top, "tokhl_b", [128, NB, 2], BF16)
    invc = sb(top, "invc", [128, 4, 16], F32)
    eps_t = sb(top, "eps_t", [128, 1], F32)
    chalf = sb(top, "chalf", [128, 8], F32)
    phalf = sb(top, "phalf", [128, 8], F32)
    cact = sb(top, "cact", [128, KC], F32)
    kmax = sb(top, "kmax", [128, H], F32)
    gm1T = sb(top, "gm1T", [128, KC], F32)
    sh1T = sb(top, "sh1T", [128, KC], F32)
    small = sb(top, "small", [128, 64], F32)

    mmall = Tile(top.enter_context(nc.psum_tensor("pmmall", [128, 2048], F32)))

    class Sub:
        def __init__(self, k0, nb):
            self.h = mmall.h[:, k0 * 512:(k0 + nb) * 512]
            self.bl = [mmall.buf(k0 + j) for j in range(nb)]

        def __getitem__(self, idx):
            return V(self.h[idx], self.bl)

    mmring = Ring([Sub(i, 1) for i in range(4)])
    mm2ring = Ring([Sub(0, 2), Sub(2, 2)])
    accring = Ring([Tile(top.enter_context(nc.psum_tensor(f"pacc{i}", [128, 512], F32))) for i in range(2)])
    auxring = Ring([Tile(top.enter_context(nc.psum_tensor(f"paux{i}", [128, 512], F32))) for i in range(2)])

    def dma(q, out, in_, **kw):
        return P.op(q, "dma_start", dma=True, out=out, in_=in_, **kw)

    def mm(out, lhsT, rhs, start, stop, **kw):
        return P.op("pe", "matmul", out=out, lhsT=lhsT, rhs=rhs, start=start, stop=stop, **kw)

    def act(out, in_, func, **kw):
        return P.op("act", "activation", out=out, in_=in_, func=func, **kw)

    def tt(eng, out, in0, in1, op):
        return P.op(eng, "tensor_tensor", out=out, in0=in0, in1=in1, op=op)

    def ts(eng, out, in0, s1, op0, s2=None, op1=None, **kw):
        if op1 is None:
            return P.op(eng, "tensor_scalar", out=out, in0=in0, scalar1=s1, scalar2=None, op0=op0, **kw)
        return P.op(eng, "tensor_scalar", out=out, in0=in0, scalar1=s1, scalar2=s2, op0=op0, op1=op1, **kw)

    def stt(eng, out, in0, scalar, in1, op0, op1):
        return P.op(eng, "scalar_tensor_tensor", out=out, in0=in0, scalar=scalar, in1=in1, op0=op0, op1=op1)

    def cp(eng, out, in_):
        if eng == "act":
            return act(out, in_, AF.Copy)
        return P.op(eng, "tensor_copy", out=out, in_=in_)

    def rsqrt_from_ss(dst, ss, n):
        np_ = dst.ap.shape[0]
        w = dst.ap.shape[1]
        if w <= 8:
            ts("dve", dst, ss, 1.0 / n, ALU.mult, EPS, ALU.add)
            P.op("pool", "tensor_tensor", out=dst, in0=dst, in1=chalf[0:np_, 0:w], op=ALU.pow)
        else:
            act(dst, ss, AF.Ln, scale=1.0 / n, bias=eps_t[0:np_, 0:1])
            act(dst, dst, AF.Exp, scale=-0.5)

    def bview(v, shape_mid):
        return V(v.ap.unsqueeze(2).to_broadcast(list(shape_mid)), v.bufs)

    with ExitStack() as st:
        stg = sb(st, "su_stg", [128, 1024], F32)
        dma("sp", ident_f[:, :], ident_in[:, :])
        cp("dve", ident_b[:, :], ident_f[:, :])
        dma("sp", stg[:, 0:128], ltri_in[:, :])
        cp("dve", ltri_b[:, :], stg[:, 0:128])
        dma("sp", iota_f[:, :], iota_in[:, :])
        dma("sp", invc[:, :, :], invc_in[:, :, :])
        stg2 = sb(st, "su_stg2", [128, NB, 2], F32)
        dma("sp", stg2[:, :, :], tokhl_in[:, :, :])
        cp("dve", tokhl_b[:, :, :], stg2[:, :, :])
        P.op("dve", "memset", ap=ones_b[:, :], constant=1.0)
        P.op("dve", "memset", ap=ones_f[:, :], constant=1.0)
        P.op("dve", "memset", ap=zeros_b[:, :], constant=0.0)
        P.op("dve", "memset", ap=sel_f[:, :], constant=0.0)
        P.op("dve", "memset", ap=sel_f[64:65, :], constant=1.0)
        P.op("dve", "memset", ap=eps_t[:, :], constant=EPS)
        P.op("dve", "memset", ap=chalf[:, :], constant=-0.5)
        P.op("dve", "memset", ap=phalf[:, :], constant=0.5)
        dma("sp", cact[:, :], cT_in[:, :])
        act(cact[:, :], cact[:, :], AF.Silu)
        P.op("dve", "memset", ap=stg[:, 0:64], constant=0.0)
        zv = V(stg.h[:, 0:32].rearrange("p (g c) -> p g c", g=4), stg[:, :].bufs)
        dma("pool", pu_d.p("pad", (slice(None), slice(None), slice(0, 8))), zv)
        dma("pool", pu_d.p("pad", (slice(None), slice(None), slice(S + 8, S + 16))), zv)
        zv1 = V(stg.h[:, 0:4].rearrange("p (g c) -> p g c", g=4), stg[:, :].bufs)
        dma("pool", z_d.p("pad", (slice(None), slice(None), slice(0, 1))), zv1, allow_slow_non_contiguous=True)
        dma("pool", z_d.p("pad", (slice(None), slice(None), slice(S + 1, S + 2))), zv1, allow_slow_non_contiguous=True)
        rc = sb(st, "su_rc", [96, 4], F32)
        dma("sp", rc[:, :], ropec_in[:, :])
        CH = min(S, 2048)
        posi = sb(st, "su_posi", [96, CH], I32)
        posf = sb(st, "su_posf", [96, CH], F32)
        a2 = sb(st, "su_a2", [96, CH], F32)
        ki = sb(st, "su_ki", [96, CH], I32)
        kf = sb(st, "su_kf", [96, CH], F32)
        r_ = sb(st, "su_r", [96, CH], F32)
        m_ = sb(st, "su_m", [96, CH], F32)
        PR = slice(64, 96)
        C1 = 6.28125
        C2 = 2.0 * math.pi - 6.28125
        for c0 in range(0, S, CH):
            dma("sp", posi[PR, :], V(pos_in.h[0:1, c0:c0 + CH].broadcast_to([32, CH]), pos_in[:, :].bufs))
            cp("dve", posf[PR, :], posi[PR, :])
            for which, dst in ((1, cc_d), (2, ss_d)):
                ts("dve", a2[PR, :], posf[PR, :], rc[PR, 0:1], ALU.mult, rc[PR, which:which + 1], ALU.add)
                ts("dve", m_[PR, :], a2[PR, :], 1.0 / (2.0 * math.pi), ALU.mult)
                cp("dve", ki[PR, :], m_[PR, :])
                cp("dve", kf[PR, :], ki[PR, :])
                stt("dve", r_[PR, :], kf[PR, :], -C1, a2[PR, :], ALU.mult, ALU.add)
                stt("dve", r_[PR, :], kf[PR, :], -C2, r_[PR, :], ALU.mult, ALU.add)
                ts("dve", m_[PR, :], r_[PR, :], math.pi, ALU.is_gt, -2.0 * math.pi, ALU.mult)
                tt("dve", r_[PR, :], r_[PR, :], m_[PR, :], ALU.add)
                ts("dve", m_[PR, :], r_[PR, :], -math.pi, ALU.is_lt, 2.0 * math.pi, ALU.mult)
                tt("dve", r_[PR, :], r_[PR, :], m_[PR, :], ALU.add)
                ts("dve", r_[PR, :], r_[PR, :], -3.141592, ALU.max, 3.141592, ALU.min)
                act(m_[PR, :], r_[PR, :], AF.Sin)
                dma("pool", dst.p("all", (slice(None), slice(c0, c0 + CH))), m_[PR, :])
    P.barrier()

    def make_norm(st, tag):
        xring = sbring(st, f"{tag}_x", [128, D], F32, 2)
        sqj = sb(st, f"{tag}_sqj", [128, D], BF16)
        xnring = sbring(st, f"{tag}_xn", [128, D], BF16, 2)
        rs = sbring(st, f"{tag}_rs", [128, 2], F32, 2)

        def norm_tile(xsrc, i, hT):
            for j in range(4):
                blk = i * 4 + j
                xt = xring.next()
                r = rs.next()
                dma("sp", xt[:, :], xsrc.p(blk, (slice(blk * 128, blk * 128 + 128), slice(None))))
                act(sqj[:, :], xt[:, :], AF.Square, accum_out=r[:, 0:1])
                rsqrt_from_ss(r[:, 1:2], r[:, 0:1], D)
                xn = xnring.next()
                act(xn[:, :], xt[:, :], AF.Copy, scale=r[:, 1:2])
                pt = auxring.next()
                ptv = pt.h[:, :].bitcast(BF16)
                for kc in range(KC):
                    P.op("pe", "transpose", out=V(ptv[:, kc * 128:(kc + 1) * 128], pt[:, :].bufs),
                         in_=xn[:, kc * 128:(kc + 1) * 128], identity=ident_b[:, :])
                pv = V(ptv.rearrange("p (k t) -> p k t", k=KC), pt[:, :].bufs)
                hv = V(hT.h[:, :, j * 128:(j + 1) * 128], hT[:, :, :].bufs)
                tt("dve", hv, pv, bview(gm1T[:, :], [128, KC, 128]), ALU.mult)
                tt("dve", hv, hv, bview(sh1T[:, :], [128, KC, 128]), ALU.add)

        return norm_tile

    def load_mod_T(l):
        with ExitStack() as st:
            row = sb(st, "lm_row", [1, 2 * D], F32)
            n1 = sb(st, "lm_n1", [128, KC], F32)
            dma("sp", row[:, :], mod_d.p("all", (slice(0, 1), slice(0, 2 * D))))
            dma("sp", n1[:, :], n1g_in.p(l, (l, slice(None), slice(None))))
            ps = auxring.next()
            for j in range(2 * KC):
                mm(ps[:, j:j + 1], row[0:1, j * 128:(j + 1) * 128], ones_f[0:1, 0:1], True, True)
            cp("dve", sh1T[:, :], ps[:, 0:KC])
            ts("dve", gm1T[:, :], ps[:, KC:2 * KC], 1.0, ALU.add)
            tt("dve", gm1T[:, :], gm1T[:, :], n1[:, :], ALU.mult)
        P.barrier()

    def bc_row(dst, off, stq, scratch_row):
        dma("sp", scratch_row[:, :], mod_d.p("all", (slice(0, 1), slice(off, off + D))))
        for hf in range(2):
            ps = auxring.next()
            mm(ps[:, :], ones_f[0:1, :], scratch_row[0:1, hf * 512:(hf + 1) * 512], True, True)
            cp("dve", dst[:, hf * 512:(hf + 1) * 512], ps[:, :])

    class _Stop(Exception):
        pass

    def chk(n):
        if _os.environ.get('KVERB'):
            print('chk', n, 'ops', len(P.ops))
        if stop <= n:
            raise _Stop()

    try:
      for l in range(L):
        chk(0)
        xsrc = x_in if l == 0 else xm_d

        with ExitStack() as st:
            wst = sbring(st, "m_w", [128, KC, 512], F32, 2)
            mrow = sb(st, "m_row", [1, 6 * D], F32)
            brow = sb(st, "m_brow", [1, 6 * D], F32)
            dma("sp", brow[:, :], b_mod.p(l, (l, slice(None), slice(None))))
            for g in range(12):
                w = wst.next()
                dma("sp", w[:, :, :], V(w_mod.h[l].rearrange("(kc p) c -> p kc c", p=128)[:, :, g * 512:(g + 1) * 512],
                                        w_mod.p(l, (l,)).bufs))
                ps = mmring.next()
                for kc in range(KC):
                    mm(ps[0:1, :], cact[:, kc:kc + 1], w[:, kc, :], kc == 0, kc == KC - 1)
                tt("dve", mrow[0:1, g * 512:(g + 1) * 512], ps[0:1, :], brow[0:1, g * 512:(g + 1) * 512], ALU.add)
            dma("pool", mod_d.p("all", (slice(None), slice(None))), mrow[:, :])
        P.barrier()
        chk(1)
        load_mod_T(l)

        with ExitStack() as st:
            stg = sbring(st, "c_stg", [128, 8192], F32, 2)
            ob = sbring(st, "c_ob", [128, 8192], BF16, 2)
            g1bc = sb(st, "c_g1bc", [128, D], F32)
            srow = sb(st, "c_srow", [1, D], F32)
            bc_row(g1bc, 2 * D, st, srow)
            engs = ["dve", "act", "pool"]
            ei = [0]

            def ce():
                ei[0] += 1
                return engs[ei[0] % 3]

            for kc in range(KC):
                s_ = stg.next()
                o_ = ob.next()
                dma("sp", s_[:, 0:WIN_EXT], w_in.p(l, (l, slice(kc * 128, kc * 128 + 128), slice(None))))
                for (a, b, o0) in ((384, 640, 0), (576, 672, 256), (5792, 5888, 352), (672, 1696, 448), (2208, 2720, 1472)):
                    cp(ce(), o_[:, o0:o0 + (b - a)], s_[:, a:b])
                cp(ce(), o_[:, 2048:2048 + 384], s_[:, 0:384])
                cp(ce(), o_[:, 2432:2432 + 512], s_[:, 1696:2208])
                gin = V(s_.h[:, 2720:5792].rearrange("p (j c i) -> p j c i", j=3, c=8), s_[:, :].bufs)
                gout = V(o_.h[:, 3072:3072 + 3072].rearrange("p (c j i) -> p j c i", j=3, c=8), o_[:, :].bufs)
                for j in range(3):
                    cp(ce(), V(gout.ap[:, j], gout.bufs), V(gin.ap[:, j], gin.bufs))
                dma("pool", wa1_d.p("all", (slice(None), kc, slice(None))), o_[:, 0:1984])
                dma("pool", wa2_d.p("all", (slice(None), kc, slice(None))), o_[:, 2048:2048 + 896])
                dma("pool", wgt_d.p("all", (slice(None), slice(None), kc, slice(None))),
                    V(o_.h[:, 3072:6144].rearrange("p (c x) -> p c x", c=8), o_[:, :].bufs))
            s_ = stg.next(); o_ = ob.next()
            dma("sp", V(s_.h[0:64, 0:8192].rearrange("p (h x) -> p h x", h=H), s_[:, :].bufs), woa_in.p(l, (l,)))
            for h in range(H):
                cp(ce(), V(o_.h[0:64, 0:8192].rearrange("p (c h i) -> p h c i", c=8, h=H)[:, h], o_[:, :].bufs),
                   V(s_.h[0:64, h * 1024:(h + 1) * 1024].rearrange("p (c i) -> p c i", c=8), s_[:, :].bufs))
            dma("pool", woa_d.p("all", (slice(None),)), V(o_.h[0:64, 0:8192].rearrange("p (c h i) -> p c h i", c=8, h=H), o_[:, :].bufs))
            s_ = stg.next(); o_ = ob.next()
            dma("sp", V(s_.h[:, 0:4096].rearrange("p (k x) -> p k x", k=4), s_[:, :].bufs),
                V(woc_in.h[l].rearrange("(k p) x -> p k x", p=128), woc_in.p(l, (l,)).bufs))
            for k in range(4):
                cp(ce(), V(o_.h[:, 0:4096].rearrange("p (c k i) -> p k c i", c=8, k=4)[:, k], o_[:, :].bufs),
                   V(s_.h[:, k * 1024:(k + 1) * 1024].rearrange("p (c i) -> p c i", c=8), s_[:, :].bufs))
            dma("pool", woc_d.p("all", (slice(None),)), V(o_.h[:, 0:4096].rearrange("p (c k i) -> p c k i", c=8, k=4), o_[:, :].bufs))
            s_ = stg.next(); o_ = ob.next()
            dma("sp", V(s_.h[:, 0:8192].rearrange("p (k x) -> p k x", k=KC), s_[:, :].bufs),
                V(wout_in.h[l].rearrange("(k p) x -> p k x", p=128), wout_in.p(l, (l,)).bufs))
            for k in range(KC):
                tt("dve" if k % 2 else "pool", o_[:, k * 1024:(k + 1) * 1024], s_[:, k * 1024:(k + 1) * 1024], g1bc[:, :], ALU.mult)
            dma("pool", wout_d.p("all", (slice(None),)), V(o_.h[:, 0:8192].rearrange("p (k x) -> p k x", k=KC), o_[:, :].bufs))
        P.barrier()
        chk(2)

        with ExitStack() as st:
            wa1 = sb(st, "a1_w", [128, KC, 1984], BF16)
            wk = sb(st, "a1_wk", [128, 2, 512], BF16)
            wv = sb(st, "a1_wv", [128, 2, 512], BF16)
            kvng = sb(st, "a1_kvng", [128, 2], F32)
            stg = sb(st, "a1_stg", [128, 2, 512], F32)
            dma("sp", wa1[:, :, :], wa1_d.p("all", (slice(None),)))
            dma("sp", kvng[:, :], kvng_in.p(l, (l,)))
            for src, dst in ((wk_in, wk), (wv_in, wv)):
                dma("sp", stg[:, :, :], V(src.h[l].rearrange("(k p) x -> p k x", p=128), src.p(l, (l,)).bufs))
                cp("dve", dst[:, :, :], stg[:, :, :])
            P.op("dve", "memset", ap=kmax[:, :], constant=0.0)
            norm_tile = make_norm(st, "a1n")
            hTr = sbring(st, "a1_hT", [128, KC, 512], BF16, 2)
            sq = sb(st, "a1_sq", [128, 2, 512], BF16)
            ckvg = sb(st, "a1_ckvg", [128, 2, 512], BF16)
            rstd = sb(st, "a1_rstd", [128, 512], F32)
            rtok = sb(st, "a1_rtok", [128, 4], F32)
            cct = sbring(st, "a1_cct", [96, 512], F32, 2)
            sst = sbring(st, "a1_sst", [96, 512], F32, 2)
            t1 = sb(st, "a1_t1", [96, 512], F32)
            t2 = sb(st, "a1_t2", [96, 512], F32)
            ktt = sbring(st, "a1_kt", [96, H, 512], BF16, 2)
            sqk = sb(st, "a1_sqk", [96, H, 512], BF16)
            vt = sbring(st, "a1_vt", [128, 4, H, 65], BF16, 2)
            f32r = sbring(st, "a1_f", [128, 512], F32, 4)
            kmt = sb(st, "a1_kmt", [128, H], F32)
            for i in range(NT):
                ts_ = slice(i * 512, (i + 1) * 512)
                hT = hTr.next()
                norm_tile(xsrc, i, hT)
                CC = cct.next(); SS = sst.next()
                dma("sp", CC[PR, :], cc_d.p("all", (slice(None), ts_)))
                dma("sp", SS[PR, :], ss_d.p("all", (slice(None), ts_)))
                chk(2.1)
                for m in range(2):
                    ps = mmring.next()
                    for kc in range(KC):
                        mm(ps[:, :], wa1[:, kc, m * 128:(m + 1) * 128], hT[:, kc, :], kc == 0, kc == KC - 1)
                    act(sq[:, m, :], ps[:, :], AF.Square)
                    act(ckvg[:, m, :], ps[:, :], AF.Copy, scale=kvng[:, m:m + 1])
                ps = auxring.next()
                for m in range(2):
                    mm(ps[:, :], ones_b[:, :], sq[:, m, :], m == 0, m == 1)
                rsqrt_from_ss(rstd[:, :], ps[:, :], 256)
                chk(2.2)
                ps = auxring.next()
                for j in range(4):
                    for m in range(2):
                        mm(ps[:, j:j + 1], sq[:, m, j * 128:(j + 1) * 128], ones_b[:, 0:1], m == 0, m == 1)
                rsqrt_from_ss(rtok[:, :], ps[:, 0:4], 256)
                chk(2.3)
                KT = ktt.next()
                for h in range(H):
                    ps = mmring.next()
                    for m in range(2):
                        mm(ps[0:64, :], wk[:, m, h * 64:(h + 1) * 64], ckvg[:, m, :], m == 0, m == 1)
                    tt("dve", KT[0:64, h, :], ps[0:64, :], rstd[0:64, :], ALU.mult)
                chk(2.4)
                psa = mmring.next()
                for kc in range(KC):
                    mm(psa[0:96, :], wa1[:, kc, 256:352], hT[:, kc, :], kc == 0, kc == KC - 1)
                psb = mmring.next()
                for kc in range(KC):
                    mm(psb[0:96, :], wa1[:, kc, 352:448], hT[:, kc, :], kc == 0, kc == KC - 1)
                tt("dve", t1[PR, :], psa[PR, :], CC[PR, :], ALU.mult)
                tt("dve", t2[PR, :], psb[PR, :], SS[PR, :], ALU.mult)
                for h in range(H):
                    tt("pool" if h % 2 else "dve", KT[PR, h, :], t1[PR, :], t2[PR, :], ALU.add)
                dma("pool", kt_d.p(i, (slice(None), slice(None), ts_)), KT[:, :, :])
                chk(2.6)
                VT = vt.next()
                P.op("pool", "memset", ap=VT[:, :, :, 64:65], constant=1.0)
                for j in range(4):
                    ps = mmring.next()
                    for m in range(2):
                        mm(ps[:, :], ckvg[:, m, j * 128:(j + 1) * 128], wv[:, m, :], m == 0, m == 1)
                    act(VT[:, j, :, 0:64], V(ps.h[:, :].rearrange("p (h d) -> p h d", h=H), ps[:, :].bufs),
                        AF.Copy, scale=rtok[:, j:j + 1])
                dma("pool", v_d.p(i, (slice(None), slice(i * 4, i * 4 + 4))), VT[:, :, :, :])
                chk(2.7)
                for g in range(4):
                    ps = mmring.next()
                    for kc in range(KC):
                        mm(ps[:, :], wa1[:, kc, 448 + g * 128:448 + (g + 1) * 128], hT[:, kc, :], kc == 0, kc == KC - 1)
                    f = f32r.next()
                    cp("act", f[:, :], ps[:, :])
                    dma("pool", pu_d.p(i, (slice(None), g, slice(8 + i * 512, 8 + (i + 1) * 512))), f[:, :])
                chk(2.8)
                for g in range(4):
                    ps = mmring.next()
                    for kc in range(KC):
                        mm(ps[:, :], wa1[:, kc, 960 + g * 128:960 + (g + 1) * 128], hT[:, kc, :], kc == 0, kc == KC - 1)
                    f = f32r.next()
                    cp("act", f[:, :], ps[:, :])
                    ps2 = mmring.next()
                    for kc in range(KC):
                        mm(ps2[:, :], wa1[:, kc, 1472 + g * 128:1472 + (g + 1) * 128], hT[:, kc, :], kc == 0, kc == KC - 1)
                    f2 = f32r.next()
                    tt("dve", f2[:, :], ps2[:, :], f[:, :], ALU.mult)
                    dma("pool", z_d.p(i, (slice(None), g, slice(1 + i * 512, 1 + (i + 1) * 512))), f2[:, :])
                chk(2.5)
                act(sqk[:, :, :], KT[:, :, :], AF.Square)
                for h in range(H):
                    ps = auxring.next()
                    mm(ps[:, :], ones_b[0:96, :], sqk[0:96, h, :], True, True)
                    P.op("dve", "tensor_reduce", out=kmt[:, h:h + 1], in_=ps[:, :], axis=AX.X, op=ALU.max)
                tt("dve", kmax[:, :], kmax[:, :], kmt[:, :], ALU.max)
        P.barrier()
        chk(3)

        with ExitStack() as st:
            KTs = sb(st, "at_KT", [96, H, S], BF16)
            Vs = sb(st, "at_V", [128, NB, H, 65], BF16)
            for i in range(NT):
                dma("sp", KTs.p(i, (slice(None), slice(None), slice(i * 512, (i + 1) * 512))),
                    kt_d.p(i, (slice(None), slice(None), slice(i * 512, (i + 1) * 512))))
                dma("sp", Vs.p(i, (slice(None), slice(i * 4, i * 4 + 4))), v_d.p(i, (slice(None), slice(i * 4, i * 4 + 4))))
            wcq = sb(st, "at_wcq", [128, KC, 384], BF16)
            dma("sp", wcq[:, :, :], wa2_d.p("all", (slice(None), slice(None), slice(0, 384))))
            wA = sb(st, "at_wA", [128, 3, 768], BF16)
            wB = sb(st, "at_wB", [128, 3, 768], BF16)
            qng = sb(st, "at_qng", [128, 3], F32)
            dma("sp", qng[:, :], qng_in.p(l, (l,)))
            with ExitStack() as stw:
                stg = sb(stw, "at_stg", [128, 3, 768], F32)
                for src, dst in ((wuqA_in, wA), (wuqB_in, wB)):
                    dma("sp", stg[:, :, :], V(src.h[l].rearrange("(k p) x -> p k x", p=128), src.p(l, (l,)).bufs))
                    cp("dve", dst[:, :, :], stg[:, :, :])
            P.barrier()
            norm_tile = make_norm(st, "atn")
            hTr = sbring(st, "at_hT", [128, KC, 512], BF16, 1)
            sq = sb(st, "at_sq", [128, 3, 512], BF16)
            cqg = sb(st, "at_cqg", [128, 3, 512], BF16)
            rstd = sb(st, "at_rstd", [128, 512], F32)
            cct = sbring(st, "at_cct", [96, 512], F32, 2)
            sst = sbring(st, "at_sst", [96, 512], F32, 2)
            ta = sbring(st, "at_ta", [96, 512], F32, 1)
            tb = sbring(st, "at_tb", [96, 512], F32, 1)
            qTr = sbring(st, "at_qT", [96, H, 512], BF16, 2)
            sqq = sbring(st, "at_sqq", [96, 512], BF16, 2)
            negb = sbring(st, "at_negb", [128, H], F32, 2)
            qm = sbring(st, "at_qm", [128, 2], F32, 4)
            ptr_ = sbring(st, "at_pt", [128, 1024], BF16, 3)
            osb = sbring(st, "at_osb", [65, 512], F32, 2)
            rec = sbring(st, "at_rec", [64, 512], F32, 2)
            oTr = sbring(st, "at_oT", [64, H, 512], BF16, 1)
            allk = list(range(NT))
            pending = [None]

            def prep(i):
                ts_ = slice(i * 512, (i + 1) * 512)
                hT = hTr.next()
                norm_tile(xsrc, i, hT)
                CC = cct.next(); SS = sst.next()
                dma("sp", CC[PR, :], cc_d.p("all", (slice(None), ts_)))
                dma("sp", SS[PR, :], ss_d.p("all", (slice(None), ts_)))
                for m in range(3):
                    ps = auxring.next()
                    for kc in range(KC):
                        mm(ps[:, :], wcq[:, kc, m * 128:(m + 1) * 128], hT[:, kc, :], kc == 0, kc == KC - 1)
                    act(sq[:, m, :], ps[:, :], AF.Square)
                    act(cqg[:, m, :], ps[:, :], AF.Copy, scale=qng[:, m:m + 1])
                ps = auxring.next()
                for m in range(3):
                    mm(ps[:, :], ones_b[:, :], sq[:, m, :], m == 0, m == 2)
                rsqrt_from_ss(rstd[:, :], ps[:, :], 384)
                tt("dve", CC[PR, :], CC[PR, :], rstd[PR, :], ALU.mult)
                tt("pool", SS[PR, :], SS[PR, :], rstd[PR, :], ALU.mult)
                return dict(CC=CC, SS=SS, qT=qTr.next(), nb=negb.next(), s2={})

            def q1(stt_, h):
                CC, SS, qT = stt_["CC"], stt_["SS"], stt_["qT"]
                psA = auxring.next()
                for m in range(3):
                    mm(psA[0:96, :], wA[:, m, h * 96:(h + 1) * 96], cqg[:, m, :], m == 0, m == 2)
                psB = auxring.next()
                for m in range(3):
                    mm(psB[0:96, :], wB[:, m, h * 96:(h + 1) * 96], cqg[:, m, :], m == 0, m == 2)
                tt("dve", qT[0:64, h, :], psA[0:64, :], rstd[0:64, :], ALU.mult)
                a_ = ta.next(); b_ = tb.next()
                tt("dve", a_[PR, :], psA[PR, :], CC[PR, :], ALU.mult)
                tt("dve", b_[PR, :], psB[PR, :], SS[PR, :], ALU.mult)
                tt("pool", qT[PR, h, :], a_[PR, :], b_[PR, :], ALU.add)
                s2 = sqq.next()
                tt("pool", s2[0:96, :], qT[0:96, h, :], qT[0:96, h, :], ALU.mult)
                stt_["s2"][h] = s2

            def q2(stt_, h):
                s2 = stt_["s2"].pop(h)
                nb_ = stt_["nb"]
                ps = auxring.next()
                mm(ps[:, :], ones_b[0:96, :], s2[0:96, :], True, True)
                q_ = qm.next()
                P.op("dve", "tensor_reduce", out=q_[:, 0:1], in_=ps[:, :], axis=AX.X, op=ALU.max)
                tt("dve", q_[:, 1:2], q_[:, 0:1], kmax[:, h:h + 1], ALU.mult)
                P.op("pool", "tensor_tensor", out=q_[:, 1:2], in0=q_[:, 1:2], in1=phalf[:, 0:1], op=ALU.pow)
                ts("dve", nb_[:, h:h + 1], q_[:, 1:2], -SCALE * 1.03, ALU.mult)

            def qorder(stt_):
                items = []
                for h in range(H):
                    items.append(lambda h=h: q1(stt_, h))
                    if h >= 1:
                        items.append(lambda h=h: q2(stt_, h - 1))
                items.append(lambda: q2(stt_, H - 1))
                return items

            cur = prep(0)
            for f_ in qorder(cur):
                f_()
            for i in range(NT):
                ts_ = slice(i * 512, (i + 1) * 512)
                qT = cur["qT"]
                nb_ = cur["nb"]
                nxt_state = None
                todo = []
                oT = oTr.next()
                for h in range(H):
                    acc = accring.next()
                    NP2 = NB // 2
                    pss = [None] * NP2

                    def qk2(kp_):
                        pss[kp_] = mm2ring.next()
                        for u in range(2):
                            kb_ = 2 * kp_ + u
                            mm(pss[kp_][:, u * 512:(u + 1) * 512], KTs.p(allk, (slice(0, 96), h, slice(kb_ * 128, (kb_ + 1) * 128))),
                               qT[0:96, h, :], True, True)

                    qk2(0)
                    for kp in range(NP2):
                        if kp + 1 < NP2:
                            qk2(kp + 1)
                        if kp == min(1, NP2 - 1) and pending[0] is not None:
                            pending[0]()
                            pending[0] = None
                        if i + 1 < NT:
                            if h == 0 and kp == min(2, NP2 - 1):
                                nxt_state = prep(i + 1)
                                todo = qorder(nxt_state)
                            elif h >= 1 and kp in (3, 8, 13) and todo:
                                todo.pop(0)()
                        pt = ptr_.next()
                        act(pt[:, :], pss[kp][:, :], AF.Exp, scale=SCALE, bias=nb_[:, h:h + 1])
                        for u in range(2):
                            kb = 2 * kp + u
                            mm(acc[0:65, :], Vs.p(allk, (slice(None), kb, h, slice(None))), pt[:, u * 512:(u + 1) * 512], kb == 0, kb == NB - 1)
                        pss[kp] = None

                    def fin(h=h, acc=acc, oT=oT):
                        o_ = osb.next()
                        cp("act", o_[0:65, :], acc[0:65, :])
                        den = auxring.next()
                        mm(den[0:64, :], sel_f[0:65, 0:64], o_[0:65, :], True, True)
                        rc_ = rec.next()
                        P.op("dve", "reciprocal", out=rc_[:, :], in_=den[0:64, :])
                        tt("pool", oT[0:64, h, :], o_[0:64, :], rc_[:, :], ALU.mult)

                    pending[0] = fin
                while todo:
                    todo.pop(0)()
                pending[0]()
                pending[0] = None
                dma("pool", ot_d.p(i, (slice(None), slice(None), ts_)), oT[:, :, :])
                cur = nxt_state
        P.barrier()
        chk(4)

        with ExitStack() as st:
            wcb = sb(st, "a2_wcb", [128, KC, 512], BF16)
            dma("sp", wcb[:, :, :], wa2_d.p("all", (slice(None), slice(None), slice(384, 896))))
            wout = sb(st, "a2_wout", [128, KC, D], BF16)
            dma("sp", wout[:, :, :], wout_d.p("all", (slice(None),)))
            wpool = sb(st, "a2_wpool", [128, 4, 256], BF16)
            stg = sb(st, "a2_stg", [128, 4, 256], F32)
            dma("sp", stg[:, :, :], wpool_in.p(l, (l,)))
            cp("dve", wpool[:, :, :], stg[:, :, :])
            bgate = sb(st, "a2_bgate", [128, 24], F32)
            pscale = sb(st, "a2_pscale", [128, KC], F32)
            convw = sb(st, "a2_convw", [128, 4, 3], F32)
            dma("sp", bgate[:, :], bgate_in.p(l, (l,)))
            dma("sp", pscale[:, :], pscale_in.p(l, (l,)))
            dma("sp", convw[:, :, :], convw_in.p(l, (l,)))
            norm_tile = make_norm(st, "a2n")
            hTr = sbring(st, "a2_hT", [128, KC, 512], BF16, 2)
            oTr = sbring(st, "a2_oT", [64, H, 512], BF16, 2)
            Ur = sbring(st, "a2_U", [128, 4, 528], F32, 2)
            Zr = sbring(st, "a2_Z", [128, 4, 514], F32, 2)
            pa = sb(st, "a2_pa", [128, 528], F32)
            pb = sb(st, "a2_pb", [128, 528], F32)
            t8 = sb(st, "a2_t8", [128, 8], F32)
            mixed = sb(st, "a2_mixed", [128, 4, 512], BF16)
            yc = sbring(st, "a2_yc", [128, 512], F32, 2)
            convo = sb(st, "a2_convo", [128, 4, 512], BF16)
            wgr = sbring(st, "a2_wg", [128, KC, 384], BF16, 2)
            woar = sbring(st, "a2_woa", [64, H, 128], BF16, 2)
            wocr = sbring(st, "a2_woc", [128, 4, 128], BF16, 2)
            gt = sbring(st, "a2_gt", [128, 3, 512], F32, 2)
            tmp = sbring(st, "a2_tmp", [128, 512], F32, 6)
            merged = sb(st, "a2_merged", [128, KC, 512], BF16)
            xr = sbring(st, "a2_xr", [128, D], F32, 2)
            xn_ = sbring(st, "a2_xnew", [128, D], F32, 2)
            for i in range(NT):
                ts_ = slice(i * 512, (i + 1) * 512)
                hT = hTr.next()
                norm_tile(xsrc, i, hT)
                oT = oTr.next()
                dma("sp", oT[:, :, :], ot_d.p(i, (slice(None), slice(None), ts_)))
                U = Ur.next(); Z = Zr.next()
                nbr = [k for k in (i - 1, i, i + 1) if 0 <= k < NT] + ["pad"]
                dma("sp", U[:, :, :], pu_d.p(nbr, (slice(None), slice(None), slice(i * 512, i * 512 + 528))))
                dma("sp", Z[:, :, :], z_d.p(nbr, (slice(None), slice(None), slice(i * 512, i * 512 + 514))))
                for g in range(4):
                    w = 2 << g
                    half = w // 2
                    cur, n = None, 528
                    bufs2 = [pa, pb]
                    src = V(U.h[:, g, :], U[:, :, :].bufs)
                    k = 1
                    bi = 0
                    while k < w:
                        dst = bufs2[bi % 2]; bi += 1
                        n2 = n - k
                        tt("dve" if g >= 2 else "pool", dst[:, 0:n2], V(src.ap[:, 0:n2], src.bufs), V(src.ap[:, k:k + n2], src.bufs), ALU.add)
                        src = V(dst.h[:, :], dst[:, :].bufs)
                        n = n2
                        k *= 2
                    sw = src
                    stt("dve", mixed[:, g, :], V(sw.ap[:, 8 - half:8 - half + 512], sw.bufs), 1.0 / w,
                        V(U.h[:, g, 8:520], U[:, :, :].bufs), ALU.mult, ALU.subtract)
                    if i == 0:
                        tt("dve", t8[:, :], V(sw.ap[:, 8 - half:16 - half], sw.bufs), invc[:, g, 0:8], ALU.mult)
                        tt("dve", mixed[:, g, 0:8], t8[:, :], V(U.h[:, g, 8:16], U[:, :, :].bufs), ALU.subtract)
                    if i == NT - 1:
                        tt("dve", t8[:, :], V(sw.ap[:, 512 - half:520 - half], sw.bufs), invc[:, g, 8:16], ALU.mult)
                        tt("dve", mixed[:, g, 504:512], t8[:, :], V(U.h[:, g, 512:520], U[:, :, :].bufs), ALU.subtract)
                for k4 in range(4):
                    y = yc.next()
                    ts("dve", y[:, :], Z[:, k4, 0:512], convw[:, k4, 0:1], ALU.mult)
                    stt("dve", y[:, :], Z[:, k4, 1:513], convw[:, k4, 1:2], y[:, :], ALU.mult, ALU.add)
                    stt("dve", y[:, :], Z[:, k4, 2:514], convw[:, k4, 2:3], y[:, :], ALU.mult, ALU.add)
                    ps = mmring.next()
                    for kc in range(KC):
                        mm(ps[:, :], wcb[:, kc, k4 * 128:(k4 + 1) * 128], hT[:, kc, :], kc == 0, kc == KC - 1)
                    tt("dve", convo[:, k4, :], ps[:, :], y[:, :], ALU.mult)
                for c in range(8):
                    wg = wgr.next(); woa = woar.next(); woc = wocr.next()
                    dma("sp", wg[:, :, :], wgt_d.p("all", (slice(None), c)))
                    dma("sp", woa[:, :, :], woa_d.p("all", (slice(None), c)))
                    dma("sp", woc[:, :, :], woc_d.p("all", (slice(None), c)))
                    G = gt.next()
                    for j in range(3):
                        ps = mmring.next()
                        for kc in range(KC):
                            mm(ps[:, :], wg[:, kc, j * 128:(j + 1) * 128], hT[:, kc, :], kc == 0, kc == KC - 1)
                        act(G[:, j, :], ps[:, :], AF.Sigmoid, bias=bgate[:, j * 8 + c:j * 8 + c + 1])
                    psa = accring.next()
                    for h in range(H):
                        mm(psa[:, :], woa[0:64, h, :], oT[0:64, h, :], h == 0, h == H - 1)
                    t0 = tmp.next()
                    tt("dve", t0[:, :], psa[:, :], G[:, 0, :], ALU.mult)
                    psb = accring.next()
                    mm(psb[:, :], wpool[:, c // 2, (c % 2) * 128:(c % 2) * 128 + 128], mixed[:, c // 2, :], True, True)
                    t1_ = tmp.next()
                    stt("dve", t1_[:, :], psb[:, :], pscale[:, c:c + 1], G[:, 1, :], ALU.mult, ALU.mult)
                    psc = auxring.next()
                    for k4 in range(4):
                        mm(psc[:, :], woc[:, k4, :], convo[:, k4, :], k4 == 0, k4 == 3)
                    t2_ = tmp.next()
                    tt("dve", t2_[:, :], psc[:, :], G[:, 2, :], ALU.mult)
                    tt("pool", t0[:, :], t0[:, :], t1_[:, :], ALU.add)
                    tt("pool", merged[:, c, :], t0[:, :], t2_[:, :], ALU.add)
                for j in range(4):
                    blk = i * 4 + j
                    xt = xr.next(); xo = xn_.next()
                    rows = slice(blk * 128, blk * 128 + 128)
                    dma("sp", xt[:, :], xsrc.p(blk, (rows, slice(None))))
                    for hf in range(2):
                        ps = mmring.next()
                        for kc in range(KC):
                            mm(ps[:, :], merged[:, kc, j * 128:(j + 1) * 128], wout[:, kc, hf * 512:(hf + 1) * 512], kc == 0, kc == KC - 1)
                        tt("dve", xo[:, hf * 512:(hf + 1) * 512], ps[:, :], xt[:, hf * 512:(hf + 1) * 512], ALU.add)
                    dma("pool", xm_d.p(blk, (rows, slice(None))), xo[:, :])
                    if debug and l == 0:
                        dma("pool", dbg["dbg_x1"].p(blk, (rows, slice(None))), xo[:, :])
        P.barrier()
        chk(5)

        with ExitStack() as st:
            gm2 = sb(st, "b_gm2", [128, D], F32)
            sh2 = sb(st, "b_sh2", [128, D], F32)
            g2 = sb(st, "b_g2", [128, D], F32)
            aff = sb(st, "b_aff", [128, NB, NE], F32)
            slotm = sb(st, "b_slotm", [128, NB, NE], F32)
            tinfo = sb(st, "b_tinfo", [128, NB, NE, 5], BF16)
            allb = list(range(NB))
            with ExitStack() as st1:
                srow = sb(st1, "b1_srow", [1, D], F32)
                n2row = sb(st1, "b1_n2row", [1, D], F32)
                n2bc = sb(st1, "b1_n2bc", [128, D], F32)
                bc_row(sh2, 3 * D, st1, srow)
                bc_row(gm2, 4 * D, st1, srow)
                bc_row(g2, 5 * D, st1, srow)
                dma("sp", n2row[:, :], n2g_in.p(l, (l,)))
                for hf in range(2):
                    ps = auxring.next()
                    mm(ps[:, :], ones_f[0:1, :], n2row[0:1, hf * 512:(hf + 1) * 512], True, True)
                    cp("dve", n2bc[:, hf * 512:(hf + 1) * 512], ps[:, :])
                ts("dve", gm2[:, :], gm2[:, :], 1.0, ALU.add)
                tt("dve", gm2[:, :], gm2[:, :], n2bc[:, :], ALU.mult)
                wr = sb(st1, "b1_wr", [128, KC, NE], F32)
                dma("sp", wr[:, :, :], wr_in.p(l, (l,)))
                xr = sbring(st1, "b1_x", [128, D], F32, 2)
                sqj = sb(st1, "b1_sqj", [128, D], BF16)
                rs = sbring(st1, "b1_rs", [128, 8], F32, 2)
                h2r = sbring(st1, "b1_h2", [128, D], F32, 2)
                h2br = sbring(st1, "b1_h2b", [128, D], BF16, 2)
                h2Tr = sbring(st1, "b1_h2T", [128, KC, 128], F32, 2)
                ex = sbring(st1, "b1_ex", [128, NE], F32, 2)
                for b in range(NB):
                    rows = slice(b * 128, b * 128 + 128)
                    xt = xr.next(); r = rs.next()
                    dma("sp", xt[:, :], xm_d.p(b, (rows, slice(None))))
                    act(sqj[:, :], xt[:, :], AF.Square, accum_out=r[:, 0:1])
                    rsqrt_from_ss(r[:, 1:2], r[:, 0:1], D)
                    h2 = h2r.next()
                    stt("dve", h2[:, :], xt[:, :], r[:, 1:2], gm2[:, :], ALU.mult, ALU.mult)
                    tt("pool", h2[:, :], h2[:, :], sh2[:, :], ALU.add)
                    h2b = h2br.next()
                    cp("act", h2b[:, :], h2[:, :])
                    dma("pool", h2_d.p(b, (rows, slice(None))), h2b[:, :])
                    h2T = h2Tr.next()
                    for hf in range(2):
                        ps = mmring.next()
                        for k4 in range(4):
                            kc = hf * 4 + k4
                            P.op("pe", "transpose", out=ps[:, k4 * 128:(k4 + 1) * 128], in_=h2[:, kc * 128:(kc + 1) * 128],
                                 identity=ident_f[:, :])
                        cp("dve" if hf else "act", V(h2T.h[:, hf * 4:(hf + 1) * 4, :], h2T[:, :, :].bufs),
                           V(ps.h[:, :].rearrange("p (k t) -> p k t", k=4), ps[:, :].bufs))
                    ps = auxring.next()
                    for kc in range(KC):
                        mm(ps[:, 0:NE], h2T[:, kc, :], wr[:, kc, :], kc == 0, kc == KC - 1)
                    P.op("dve", "tensor_reduce", out=r[:, 2:3], in_=ps[:, 0:NE], axis=AX.X, op=ALU.max)
                    ts("dve", r[:, 3:4], r[:, 2:3], -1.0, ALU.mult)
                    e_ = ex.next()
                    act(e_[:, :], ps[:, 0:NE], AF.Exp, bias=r[:, 3:4], accum_out=r[:, 4:5])
                    P.op("dve", "reciprocal", out=r[:, 5:6], in_=r[:, 4:5])
                    ts("dve", aff.p(b, (slice(None), b, slice(None))), e_[:, :], r[:, 5:6], ALU.mult)
            P.barrier()
            chk(6)
            with ExitStack() as st2:
                W = NB * NE
                lo = sb(st2, "b2_lo", [128, NE], F32)
                hi = sb(st2, "b2_hi", [128, NE], F32)
                mid = sb(st2, "b2_mid", [128, NE], F32)
                c16 = sb(st2, "b2_c16", [128, NE], F32)
                ge = sb(st2, "b2_ge", [128, NE], F32)
                t16 = sb(st2, "b2_t16", [128, NE], F32)
                cmpb = sb(st2, "b2_cmp", [128, NB, NE], BF16)
                maskf = sb(st2, "b2_maskf", [128, NB, NE], F32)
                offs = sb(st2, "b2_offs", [128, NB, NE], F32)
                r1 = sb(st2, "b2_r1", [128, NB, NE], F32)
                r2 = sb(st2, "b2_r2", [128, NB, NE], F32)
                affv = aff.p(allb, (slice(None),))
                P.op("dve", "memset", ap=lo[:, :], constant=0.0)

                def bcast_e(t):
                    return V(t.h[:, :].unsqueeze(1).to_broadcast([128, NB, NE]), t[:, :].bufs)

                for it in range(30):
                    hstep = 2.0 ** -(it + 1)
                    ts("dve", mid[:, :], lo[:, :], hstep, ALU.add)
                    tt("dve", cmpb[:, :, :], affv, bcast_e(mid), ALU.is_ge)
                    ps = auxring.next()
                    mm(ps[:, 0:W], ones_b[:, :], V(cmpb.h[:, :, :].rearrange("p b e -> p (b e)"), cmpb[:, :, :].bufs), True, True)
                    P.op("dve", "tensor_reduce", out=c16[:, :], in_=V(ps.h[:, 0:W].rearrange("p (b e) -> p e b", e=NE), ps[:, :].bufs),
                         axis=AX.X, op=ALU.add)
                    ts("dve", ge[:, :], c16[:, :], float(CAP) - 0.5, ALU.is_ge)
                    tt("dve", t16[:, :], mid[:, :], ge[:, :], ALU.mult)
                    tt("dve", lo[:, :], lo[:, :], t16[:, :], ALU.max)
                tt("dve", maskf[:, :, :], affv, bcast_e(lo), ALU.is_ge)
                cp("dve", cmpb[:, :, :], maskf[:, :, :])
                cmpflat = V(cmpb.h[:, :, :].rearrange("p b e -> p (b e)"), cmpb[:, :, :].bufs)
                psw = auxring.next()
                mm(psw[:, 0:W], ltri_b[:, :], cmpflat, True, True)
                pst = auxring.next()
                mm(pst[:, 0:W], ones_b[:, :], cmpflat, True, True)
                cp("dve", V(r1.h[:, :, :].rearrange("p b e -> p (b e)"), r1[:, :, :].bufs), pst[:, 0:W])
                P.op("dve", "memset", ap=offs[:, 0, :], constant=0.0)
                for b in range(1, NB):
                    tt("dve", offs[:, b, :], offs[:, b - 1, :], r1[:, b - 1, :], ALU.add)
                tt("dve", V(r2.h[:, :, :].rearrange("p b e -> p (b e)"), r2[:, :, :].bufs), psw[:, 0:W],
                   V(offs.h[:, :, :].rearrange("p b e -> p (b e)"), offs[:, :, :].bufs), ALU.add)
                stt("dve", r2[:, :, :], r2[:, :, :], 1.0, maskf[:, :, :], ALU.add, ALU.mult)
                ts("dve", slotm[:, :, :], r2[:, :, :], -1.0, ALU.add)
                for q in range(2):
                    cp("dve", V(tinfo.h[:, :, :, q], tinfo[:, :, :, :].bufs),
                       V(tokhl_b.h[:, :, q:q + 1].to_broadcast([128, NB, NE]), tokhl_b[:, :, :].bufs))
                a1v = V(tinfo.h[:, :, :, 2], tinfo[:, :, :, :].bufs)
                a2v = V(tinfo.h[:, :, :, 3], tinfo[:, :, :, :].bufs)
                a3v = V(tinfo.h[:, :, :, 4], tinfo[:, :, :, :].bufs)
                cp("dve", a1v, affv)
                tt("dve", r1[:, :, :], affv, a1v, ALU.subtract)
                cp("dve", a2v, r1[:, :, :])
                tt("dve", r2[:, :, :], r1[:, :, :], a2v, ALU.subtract)
                cp("dve", a3v, r2[:, :, :])
            P.barrier()
            chk(7)
            with ExitStack() as st3:
                stgr = sbring(st3, "b3_stg", [128, 4, D], F32, 4)
                wbr = sbring(st3, "b3_w", [128, KC, D], BF16, 4)
                ohr = sbring(st3, "b3_oh", [128, CAP], BF16, 3)
                idxf = sbring(st3, "b3_idxf", [128, SCH], F32, 2)
                pcsr = sbring(st3, "b3_pcs", [128, SCH * 8], F32, 2)
                idxi = sbring(st3, "b3_idxi", [128, SCH], I32, 2)
                val = sbring(st3, "b3_val", [128, SCH], F32, 2)
                xgr = sbring(st3, "b3_xg", [128, SCH, D], BF16, 2)
                sgr = sbring(st3, "b3_sg", [128, CAP], F32, 1)
                aT = sb(st3, "b3_aT", [128, KC, CAP], BF16)
                ysr = sbring(st3, "b3_ys", [128, D], F32, 2)
                cengs = ["dve", "act"]
                ci = [0]

                def loadw_issue(src, e):
                    wt = wbr.next()
                    stgs = []
                    for hf in range(2):
                        s_ = stgr.next()
                        dma("sp", s_[:, :, :], V(src.h[l, e, hf * 512:(hf + 1) * 512, :].rearrange("(k p) x -> p k x", p=128),
                                                 src.p((l, e), (l, e)).bufs))
                        stgs.append(s_)
                    return (wt, stgs)

                def loadw_cast(lw):
                    wt, stgs = lw
                    for hf, s_ in enumerate(stgs):
                        ci[0] += 1
                        cp(cengs[ci[0] % 2], V(wt.h[:, hf * 4:(hf + 1) * 4, :], wt[:, :, :].bufs), s_[:, :, :])
                    return wt

                def loadw(src, e):
                    return loadw_cast(loadw_issue(src, e))

                xgTr = sbring(st3, "b3_xgTr", [128, KC, CAP], BF16, 2)

                def stageA1(e):
                    pidx = accring.next()
                    mm(pidx[:, 0:SCH * 8], zeros_b[:, :], zeros_b[:, 0:SCH * 8], True, False, skip_group_check=True)
                    for b in range(NB):
                        oh = ohr.next()
                        ts("dve", oh[:, :], iota_f[:, 0:CAP], slotm[:, b, e:e + 1], ALU.is_equal)
                        for sc in range(SCH):
                            mm(pidx[:, sc * 8:sc * 8 + 5], oh[:, sc * 128:(sc + 1) * 128], tinfo[:, b, e, :], False, b == NB - 1,
                               skip_group_check=True)
                    pcs = pcsr.next()
                    cp("act", pcs[:, :], pidx[:, 0:SCH * 8])
                    pv = V(pcs.h[:, :].rearrange("p (s c) -> p s c", c=8), pcs[:, :].bufs)
                    xf = idxf.next(); xi = idxi.next(); vl = val.next()
                    stt("dve", xf[:, :], V(pv.ap[:, :, 0], pv.bufs), 64.0, V(pv.ap[:, :, 1], pv.bufs), ALU.mult, ALU.add)
                    cp("dve", xi[:, :], xf[:, :])
                    tt("dve", vl[:, :], V(pv.ap[:, :, 2], pv.bufs), V(pv.ap[:, :, 3], pv.bufs), ALU.add)
                    tt("dve", vl[:, :], vl[:, :], V(pv.ap[:, :, 4], pv.bufs), ALU.add)
                    xg = xgr.next()
                    for sc in range(SCH):
                        P.op("pool", "indirect_dma_start", dma=True, reads=[xi[:, :], h2_d.p(allb, (slice(None),))],
                             out=xg[:, sc, :], out_offset=None, in_=h2_d.h[:, :],
                             in_offset=bass.IndirectOffsetOnAxis(ap=xi.h[:, sc:sc + 1], axis=0))
                    return dict(xi=xi, vl=vl, xg=xg)

                def stageA2(sa):
                    xg = sa["xg"]
                    xgT_ = xgTr.next()
                    for sc in range(SCH):
                        pt = auxring.next()
                        ptv = pt.h[:, :].bitcast(BF16)
                        for kc in range(KC):
                            P.op("pe", "transpose", out=V(ptv[:, kc * 128:(kc + 1) * 128], pt[:, :].bufs),
                                 in_=xg[:, sc, kc * 128:(kc + 1) * 128], identity=ident_b[:, :])
                        cp("act" if sc % 2 else "dve", V(xgT_.h[:, :, sc * 128:(sc + 1) * 128], xgT_[:, :, :].bufs),
                           V(ptv.rearrange("p (k t) -> p k t", k=KC), pt[:, :].bufs))
                    sa["xgT"] = xgT_

                def stage_up(sa, Wg, Wu, mid=None):
                    xgT_ = sa["xgT"]
                    for fc in range(KC):
                        if fc == 4 and mid is not None:
                            mid()
                        psg = mmring.next()
                        for kc in range(KC):
                            mm(psg[:, 0:CAP], Wg[:, kc, fc * 128:(fc + 1) * 128], xgT_[:, kc, :], kc == 0, kc == KC - 1)
                        psu = mmring.next()
                        for kc in range(KC):
                            mm(psu[:, 0:CAP], Wu[:, kc, fc * 128:(fc + 1) * 128], xgT_[:, kc, :], kc == 0, kc == KC - 1)
                        sg = sgr.next()
                        act(sg[:, :], psg[:, 0:CAP], AF.Silu)
                        tt("dve", aT[:, fc, :], psu[:, 0:CAP], sg[:, :], ALU.mult)

                prev_sc = [[]]

                def stage_down(sa, Wd, mid=None):
                    xi, vl = sa["xi"], sa["vl"]
                    mine = []
                    for sc in range(SCH):
                        if sc == (SCH + 1) // 2 and mid is not None:
                            mid()
                            mid = None
                        ys = ysr.next()
                        for hf in range(2):
                            ps = mmring.next()
                            for fc in range(KC):
                                mm(ps[:, :], aT[:, fc, sc * 128:(sc + 1) * 128], Wd[:, fc, hf * 512:(hf + 1) * 512], fc == 0, fc == KC - 1)
                            stt("dve", ys[:, hf * 512:(hf + 1) * 512], ps[:, :], vl[:, sc:sc + 1], g2[:, hf * 512:(hf + 1) * 512],
                                ALU.mult, ALU.mult)
                        mine.append(P.op("pool", "indirect_dma_start", dma=True, reads=[xi[:, :], ys[:, :]], deps_extra=prev_sc[0],
                                         out=xm_d.h[:, :], out_offset=bass.IndirectOffsetOnAxis(ap=xi.h[:, sc:sc + 1], axis=0),
                                         in_=ys.h[:, :], in_offset=None, compute_op=ALU.add))
                    if mid is not None:
                        mid()
                    prev_sc[0] = mine

                sa = stageA1(0)
                Wg = loadw(wg_in, 0)
                Wu = loadw(wu_in, 0)
                Wd = loadw(wd_in, 0)
                stageA2(sa)
                for e in range(NE):
                    nxt = e + 1 < NE
                    if nxt:
                        sb_ = stageA1(e + 1)
                        lg = loadw_issue(wg_in, e + 1)
                        stage_up(sa, Wg, Wu, mid=lambda: loadw_cast(lg))
                        lu = loadw_issue(wu_in, e + 1)
                        stage_down(sa, Wd, mid=lambda: loadw_cast(lu))
                        Wd2 = loadw(wd_in, e + 1)
                        stageA2(sb_)
                        sa, Wg, Wu, Wd = sb_, lg[0], lu[0], Wd2
                    else:
                        stage_up(sa, Wg, Wu)
                        stage_down(sa, Wd)
        P.barrier()
        chk(8)

    except (_Stop, StopBuild):
        P.limit = None
    with ExitStack() as st:
        frow = sb(st, "f_row", [1, D], F32)
        fbc = sb(st, "f_bc", [128, D], F32)
        dma("sp", frow[:, :], fg_in[:, :])
        for hf in range(2):
            ps = auxring.next()
            mm(ps[:, :], ones_f[0:1, :], frow[0:1, hf * 512:(hf + 1) * 512], True, True)
            cp("dve", fbc[:, hf * 512:(hf + 1) * 512], ps[:, :])
        xr = sbring(st, "f_x", [128, D], F32, 3)
        sqj = sb(st, "f_sqj", [128, D], BF16)
        rs = sbring(st, "f_rs", [128, 2], F32, 3)
        yo = sbring(st, "f_y", [128, D], F32, 3)
        for b in range(NB):
            rows = slice(b * 128, b * 128 + 128)
            xt = xr.next(); r = rs.next(); y = yo.next()
            dma("sp", xt[:, :], xm_d.p(b, (rows, slice(None))))
            act(sqj[:, :], xt[:, :], AF.Square, accum_out=r[:, 0:1])
            rsqrt_from_ss(r[:, 1:2], r[:, 0:1], D)
            stt("dve", y[:, :], xt[:, :], r[:, 1:2], fbc[:, :], ALU.mult, ALU.mult)
            dma("pool", out_d.p(b, (rows, slice(None))), y[:, :])

    P.emit(top)
    return nc, top


def host_consts(S):
    NB = S // 128
    f = np.float32
    ident = np.eye(128, dtype=f)
    ltri = (np.arange(128)[:, None] < np.arange(128)[None, :]).astype(f)
    iota = np.tile(np.arange(512, dtype=f)[None, :], (128, 1))
    t = (np.arange(NB)[None, :] * 128 + np.arange(128)[:, None])
    tokhl = np.stack([(t // 64).astype(f), (t % 64).astype(f)], axis=-1)
    invc = np.zeros((128, 4, 16), f)
    for g in range(4):
        half = 1 << g
        for n in range(8):
            invc[:, g, n] = 1.0 / (min(n, half) + half)
            r = 7 - n
            invc[:, g, 8 + n] = 1.0 / (min(half - 1, r) + half + 1)
    ropec = np.zeros((96, 4), f)
    freqs = (10000.0 ** (-np.arange(0, 32, 2, dtype=np.float32) / np.float32(32))).astype(f)
    for i in range(16):
        for base in (64, 80):
            ropec[base + i, 0] = freqs[i]
            ropec[base + i, 1] = np.pi / 2
        ropec[64 + i, 2] = np.pi
        ropec[80 + i, 2] = 0.0
    return dict(ident=ident, ltri=ltri, iota=iota, tokhl=np.ascontiguousarray(tokhl), invc=invc, ropec=ropec)


def host_layout(inp, L):
    f = np.float32
    A = lambda a: np.ascontiguousarray(np.asarray(a), dtype=f)
    w_in = A(inp["w_in"])[:L]
    w_in_ext = np.concatenate([w_in, w_in[:, :, 576:640], w_in[:, :, 656:672], w_in[:, :, 640:656]], axis=2)
    w_uq = A(inp["w_uq"])[:L].reshape(L, 384, H, 96)
    wuqB = np.concatenate([w_uq[..., 0:64], w_uq[..., 80:96], w_uq[..., 64:80]], axis=-1)
    w_ukv = A(inp["w_ukv"])[:L].reshape(L, 256, H, 128)
    pp = lambda a, k: np.ascontiguousarray(A(a)[:L].reshape(L, k, 128).transpose(0, 2, 1))
    d = dict(
        w_mod=A(inp["w_mod"])[:L], b_mod=A(inp["b_mod"])[:L].reshape(L, 1, 6 * D),
        n1g=pp(inp["norm1_g"], KC), n2g=A(inp["norm2_g"])[:L].reshape(L, 1, D),
        w_in=np.ascontiguousarray(w_in_ext),
        bgate=pp(inp["b_gate"], 24), qng=pp(inp["q_norm_g"], 3), kvng=pp(inp["kv_norm_g"], 2),
        wuqA=np.ascontiguousarray(w_uq.reshape(L, 384, 768)), wuqB=np.ascontiguousarray(wuqB.reshape(L, 384, 768)),
        wk=np.ascontiguousarray(w_ukv[..., 0:64].reshape(L, 256, 512)),
        wv=np.ascontiguousarray(w_ukv[..., 64:128].reshape(L, 256, 512)),
        woa=np.ascontiguousarray(A(inp["w_oa"])[:L].reshape(L, H, 64, D).transpose(0, 2, 1, 3)),
        wpool=np.ascontiguousarray(A(inp["w_pool"])[:L].transpose(0, 2, 1, 3)),
        pscale=pp(inp["pool_scale"], KC),
        convw=np.ascontiguousarray(A(inp["conv_w"])[:L].reshape(L, 3, 4, 128).transpose(0, 3, 2, 1)),
        woc=A(inp["w_oc"])[:L], wout=A(inp["w_out"])[:L],
        wr=np.ascontiguousarray(A(inp["w_router"])[:L].reshape(L, KC, 128, NE).transpose(0, 2, 1, 3)),
        w_gate=A(inp["w_gate"])[:L], w_up=A(inp["w_up"])[:L], w_down=A(inp["w_down"])[:L],
        fg=A(inp["final_g"]).reshape(1, D),
    )
    return d


_CACHE = {}


def run(inp, S, L, debug=False, trace=False, stop=99):
    B = np.asarray(inp["x"]).shape[0]
    key = (S, L, debug, stop)
    if key not in _CACHE:
        _CACHE[key] = build(S, L, debug, stop)
    nc, _ = _CACHE[key]
    shared = host_layout(inp, L)
    shared.update(host_consts(S))
    x = np.ascontiguousarray(np.asarray(inp["x"]), dtype=np.float32)
    c = np.asarray(inp["c"], dtype=np.float32)
    pos = np.asarray(inp["positions"]).astype(np.int32)
    in_maps = []
    ncores = int(_os.environ.get('KCORES', '8'))
    for core in range(ncores):
        b = core % B
        m = dict(shared)
        m["x"] = x[b]
        m["cT"] = np.ascontiguousarray(c[b].reshape(KC, 128).T)
        m["pos"] = np.ascontiguousarray(pos[b].reshape(1, S))
        in_maps.append(m)
    res = run_bass_kernel_spmd(nc, in_maps, core_ids=list(range(ncores)), **({"trace": True} if trace else {}))
    return res


def kernel(**inputs):
    S = np.asarray(inputs["x"]).shape[1]
    B = np.asarray(inputs["x"]).shape[0]
    L = np.asarray(inputs["w_mod"]).shape[0]
    res = run(inputs, S, L)
    return np.stack([np.asarray(res.results[b]["out"], dtype=np.float32) for b in range(B)], axis=0)
```

```python
import math
import os as _os
from contextlib import ExitStack

import numpy as np
import concourse.bass as bass
import concourse.mybir as mybir
from concourse.bass_utils import run_bass_kernel_spmd

F32 = mybir.dt.float32
BF16 = mybir.dt.bfloat16
I32 = mybir.dt.int32
ALU = mybir.AluOpType
AF = mybir.ActivationFunctionType
AX = mybir.AxisListType

D = 1024
KC = 8
H = 8
NE = 16
EPS = 1e-6
SCALE = 96 ** -0.5
WIN_EXT = 5888


class Buf:
    __slots__ = ("last_w", "readers", "dma_readers")

    def __init__(self):
        self.last_w = None
        self.readers = {}
        self.dma_readers = []


class V:
    __slots__ = ("ap", "bufs")

    def __init__(self, ap, bufs):
        self.ap = ap
        self.bufs = bufs


class Tile:
    def __init__(self, handle):
        self.h = handle
        self.bufs = {}

    def buf(self, key):
        b = self.bufs.get(key)
        if b is None:
            b = self.bufs[key] = Buf()
        return b

    def __getitem__(self, idx):
        return V(self.h[idx], [self.buf(None)])

    def p(self, key, idx):
        keys = key if isinstance(key, (list, tuple)) else [key]
        return V(self.h[idx], [self.buf(k) for k in keys])


class Op:
    __slots__ = ("eng", "meth", "kw", "deps", "is_dma", "has_dep", "sig")


WRITE_KEYS = ("out", "accum_out", "ap")
ENGS = ("pe", "act", "dve", "pool", "sp")


class StopBuild(Exception):
    pass


class Prog:
    def __init__(self, nc):
        self.nc = nc
        self.ops = []
        self.last = {}
        self.recent_dma = {e: [] for e in ENGS}
        self.R = 8

    limit = None

    def op(self, eng, meth, reads=(), writes=(), dma=False, deps_extra=(), **kw):
        if self.limit is not None and len(self.ops) >= self.limit:
            self.limit = None
            raise StopBuild()
        rd, wr = [], []
        for k, v in list(kw.items()):
            if isinstance(v, V):
                (wr if k in WRITE_KEYS else rd).extend(v.bufs)
                kw[k] = v.ap
        for v in reads:
            rd.extend(v.bufs)
        for v in writes:
            wr.extend(v.bufs)
        o = Op()
        o.eng, o.meth, o.kw, o.is_dma, o.has_dep, o.sig = eng, meth, kw, dma, False, None
        deps = {}

        def add(d):
            if d is None or d is o:
                return
            if (not dma) and eng == "pe" and d.eng == "pe" and not d.is_dma:
                return
            deps[id(d)] = d

        for d_ in deps_extra:
            add(d_)
        for b in rd:
            add(b.last_w)
        for b in wr:
            add(b.last_w)
            for r in b.readers.values():
                add(r)
            for r in b.dma_readers:
                add(r)
        o.deps = list(deps.values())
        for d in o.deps:
            d.has_dep = True
        for b in rd:
            if dma:
                b.dma_readers.append(o)
                if len(b.dma_readers) > 64:
                    b.dma_readers = b.dma_readers[-64:]
            else:
                b.readers[eng] = o
        for b in wr:
            b.last_w = o
            b.readers = {}
            b.dma_readers = []
        self.ops.append(o)
        if dma:
            lst = self.recent_dma[eng]
            lst.append(o)
            if len(lst) > self.R:
                lst.pop(0)
        else:
            self.last[eng] = o
        return o

    def barrier(self):
        alld = [o for o in self.last.values()]
        for lst in self.recent_dma.values():
            alld.extend(lst)
        for d in alld:
            d.has_dep = True
        for e in ENGS:
            o = Op()
            o.eng, o.meth, o.kw, o.is_dma, o.has_dep, o.sig = e, None, {}, False, False, None
            o.deps = list(alld)
            self.ops.append(o)

    def emit(self, stack):
        nc = self.nc
        engobj = {"pe": nc.tensor, "act": nc.scalar, "dve": nc.vector, "pool": nc.gpsimd, "sp": nc.sync}
        nsem = [0]

        def new_sem(tag):
            nsem[0] += 1
            return stack.enter_context(nc.semaphore(f"s_{tag}_{nsem[0]}"))

        sem_state = {}
        waited = {e: {} for e in ENGS}
        dma_cnt = {e: 0 for e in ENGS}
        dma_sems = {}

        def wait(eng, sem, val):
            w = waited[eng]
            k = id(sem)
            if w.get(k, 0) >= val:
                return
            engobj[eng].wait_ge(sem, val)
            w[k] = val

        keep = []
        for o in self.ops:
            e = engobj[o.eng]
            for d in o.deps:
                wait(o.eng, d.sig[0], d.sig[1])
            if o.meth is None:
                continue
            if o.is_dma:
                if o.eng not in dma_sems:
                    dma_sems[o.eng] = [new_sem("d" + o.eng) for _ in range(self.R)]
                j = dma_cnt[o.eng]
                dma_cnt[o.eng] += 1
                sem = dma_sems[o.eng][j % self.R]
                val = 16 * (j // self.R + 1)
                if j >= self.R:
                    wait(o.eng, sem, val - 16)
                ins = getattr(e, o.meth)(**o.kw)
                ins.then_inc(sem, 16)
                o.sig = (sem, val)
            else:
                ins = getattr(e, o.meth)(**o.kw)
                if o.has_dep:
                    st = sem_state.get(o.eng)
                    if st is None or st[1] >= 30000:
                        st = sem_state[o.eng] = [new_sem(o.eng), 0]
                    st[1] += 1
                    ins.then_inc(st[0], 1)
                    o.sig = (st[0], st[1])
            o.kw = None
        for eng in ENGS:
            for q, sems in dma_sems.items():
                n = dma_cnt[q]
                for sl, sem in enumerate(sems):
                    cnt = (n - sl + self.R - 1) // self.R if n > sl else 0
                    if cnt > 0:
                        wait(eng, sem, 16 * cnt)
            for e2, st in sem_state.items():
                if st[1] > 0:
                    wait(eng, st[0], st[1])


class Ring:
    def __init__(self, tiles):
        self.t = tiles
        self.i = 0

    def next(self):
        t = self.t[self.i % len(self.t)]
        self.i += 1
        return t


def build(S, L, debug=False, stop=99):
    NT = S // 512
    NB = S // 128
    CAP = 2 * S // NE
    SCH = CAP // 128
    nc = bass.Bass("TRN2", target_bir_lowering=False)
    P = Prog(nc)
    if _os.environ.get('KLIMIT'):
        P.limit = int(_os.environ['KLIMIT'])
    top = ExitStack()

    def dram(name, shape, dt, kind):
        return Tile(nc.dram_tensor(name, list(shape), dt, kind=kind))

    uniq = [0]

    def sb(stack, name, shape, dt):
        uniq[0] += 1
        return Tile(stack.enter_context(nc.sbuf_tensor(f"sb{uniq[0]}_{name}", list(shape), dt)))

    def sbring(stack, name, shape, dt, n):
        return Ring([sb(stack, f"{name}{i}", shape, dt) for i in range(n)])

    EI = "ExternalInput"
    x_in = dram("x", [S, D], F32, EI)
    cT_in = dram("cT", [128, KC], F32, EI)
    pos_in = dram("pos", [1, S], I32, EI)
    w_mod = dram("w_mod", [L, D, 6 * D], F32, EI)
    b_mod = dram("b_mod", [L, 1, 6 * D], F32, EI)
    n1g_in = dram("n1g", [L, 128, KC], F32, EI)
    n2g_in = dram("n2g", [L, 1, D], F32, EI)
    w_in = dram("w_in", [L, D, WIN_EXT], F32, EI)
    bgate_in = dram("bgate", [L, 128, 24], F32, EI)
    qng_in = dram("qng", [L, 128, 3], F32, EI)
    kvng_in = dram("kvng", [L, 128, 2], F32, EI)
    wuqA_in = dram("wuqA", [L, 384, 768], F32, EI)
    wuqB_in = dram("wuqB", [L, 384, 768], F32, EI)
    wk_in = dram("wk", [L, 256, 512], F32, EI)
    wv_in = dram("wv", [L, 256, 512], F32, EI)
    woa_in = dram("woa", [L, 64, H, D], F32, EI)
    wpool_in = dram("wpool", [L, 128, 4, 256], F32, EI)
    pscale_in = dram("pscale", [L, 128, KC], F32, EI)
    convw_in = dram("convw", [L, 128, 4, 3], F32, EI)
    woc_in = dram("woc", [L, 512, D], F32, EI)
    wout_in = dram("wout", [L, D, D], F32, EI)
    wr_in = dram("wr", [L, 128, KC, NE], F32, EI)
    wg_in = dram("w_gate", [L, NE, D, D], F32, EI)
    wu_in = dram("w_up", [L, NE, D, D], F32, EI)
    wd_in = dram("w_down", [L, NE, D, D], F32, EI)
    fg_in = dram("fg", [1, D], F32, EI)
    ident_in = dram("ident", [128, 128], F32, EI)
    ltri_in = dram("ltri", [128, 128], F32, EI)
    iota_in = dram("iota", [128, 512], F32, EI)
    tokhl_in = dram("tokhl", [128, NB, 2], F32, EI)
    invc_in = dram("invc", [128, 4, 16], F32, EI)
    ropec_in = dram("ropec", [96, 4], F32, EI)
    out_d = dram("out", [S, D], F32, "ExternalOutput")

    IN = "Internal"
    xm_d = dram("xm_d", [S, D], F32, IN)
    cc_d = dram("cc_d", [32, S], F32, IN)
    ss_d = dram("ss_d", [32, S], F32, IN)
    mod_d = dram("mod_d", [1, 6 * D], F32, IN)
    wa1_d = dram("wa1_d", [128, KC, 1984], BF16, IN)
    wa2_d = dram("wa2_d", [128, KC, 896], BF16, IN)
    wgt_d = dram("wgt_d", [128, 8, KC, 384], BF16, IN)
    woa_d = dram("woa_d", [64, 8, H, 128], BF16, IN)
    woc_d = dram("woc_d", [128, 8, 4, 128], BF16, IN)
    wout_d = dram("wout_d", [128, KC, D], BF16, IN)
    kt_d = dram("kt_d", [96, H, S], BF16, IN)
    v_d = dram("v_d", [128, NB, H, 65], BF16, IN)
    ot_d = dram("ot_d", [64, H, S], BF16, IN)
    pu_d = dram("pu_d", [128, 4, S + 16], F32, IN)
    z_d = dram("z_d", [128, 4, S + 2], F32, IN)
    h2_d = dram("h2_d", [S, D], BF16, IN)
    dbg = {}
    if debug:
        dbg["dbg_x1"] = dram("dbg_x1", [S, D], F32, "ExternalOutput")

    ident_f = sb(top, "ident_f", [128, 128], F32)
    ident_b = sb(top, "ident_b", [128, 128], BF16)
    ones_b = sb(top, "ones_b", [128, 128], BF16)
    ones_f = sb(top, "ones_f", [128, 128], F32)
    zeros_b = sb(top, "zeros_b", [128, 128], BF16)
    sel_f = sb(top, "sel_f", [65, 64], F32)
    ltri_b = sb(top, "ltri_b", [128, 128], BF16)
    iota_f = sb(top, "iota_f", [128, 512], F32)
    tokhl_b = sb(top, "tokhl_b", [128, NB, 2], BF16)
    invc = sb(top, "invc", [128, 4, 16], F32)
    eps_t = sb(top, "eps_t", [128, 1], F32)
    chalf = sb(top, "chalf", [128, 8], F32)
    phalf = sb(top, "phalf", [128, 8], F32)
    cact = sb(top, "cact", [128, KC], F32)
    kmax = sb(top, "kmax", [128, H], F32)
    gm1T = sb(top, "gm1T", [128, KC], F32)
    sh1T = sb(top, "sh1T", [128, KC], F32)
    small = sb(top, "small", [128, 64], F32)

    mmall = Tile(top.enter_context(nc.psum_tensor("pmmall", [128, 2048], F32)))

    class Sub:
        def __init__(self, k0, nb):
            self.h = mmall.h[:, k0 * 512:(k0 + nb) * 512]
            self.bl = [mmall.buf(k0 + j) for j in range(nb)]

        def __getitem__(self, idx):
            return V(self.h[idx], self.bl)

    mmring = Ring([Sub(i, 1) for i in range(4)])
    mm2ring = Ring([Sub(0, 2), Sub(2, 2)])
    accring = Ring([Tile(top.enter_context(nc.psum_tensor(f"pacc{i}", [128, 512], F32))) for i in range(2)])
    auxring = Ring([Tile(top.enter_context(nc.psum_tensor(f"paux{i}", [128, 512], F32))) for i in range(2)])

    def dma(q, out, in_, **kw):
        return P.op(q, "dma_start", dma=True, out=out, in_=in_, **kw)

    def mm(out, lhsT, rhs, start, stop, **kw):
        return P.op("pe", "matmul", out=out, lhsT=lhsT, rhs=rhs, start=start, stop=stop, **kw)

    def act(out, in_, func, **kw):
        return P.op("act", "activation", out=out, in_=in_, func=func, **kw)

    def tt(eng, out, in0, in1, op):
        return P.op(eng, "tensor_tensor", out=out, in0=in0, in1=in1, op=op)

    def ts(eng, out, in0, s1, op0, s2=None, op1=None, **kw):
        if op1 is None:
            return P.op(eng, "tensor_scalar", out=out, in0=in0, scalar1=s1, scalar2=None, op0=op0, **kw)
        return P.op(eng, "tensor_scalar", out=out, in0=in0, scalar1=s1, scalar2=s2, op0=op0, op1=op1, **kw)

    def stt(eng, out, in0, scalar, in1, op0, op1):
        return P.op(eng, "scalar_tensor_tensor", out=out, in0=in0, scalar=scalar, in1=in1, op0=op0, op1=op1)

    def cp(eng, out, in_):
        if eng == "act":
            return act(out, in_, AF.Copy)
        return P.op(eng, "tensor_copy", out=out, in_=in_)

    def rsqrt_from_ss(dst, ss, n):
        np_ = dst.ap.shape[0]
        w = dst.ap.shape[1]
        if w <= 8:
            ts("dve", dst, ss, 1.0 / n, ALU.mult, EPS, ALU.add)
            P.op("pool", "tensor_tensor", out=dst, in0=dst, in1=chalf[0:np_, 0:w], op=ALU.pow)
        else:
            act(dst, ss, AF.Ln, scale=1.0 / n, bias=eps_t[0:np_, 0:1])
            act(dst, dst, AF.Exp, scale=-0.5)

    def bview(v, shape_mid):
        return V(v.ap.unsqueeze(2).to_broadcast(list(shape_mid)), v.bufs)

    with ExitStack() as st:
        stg = sb(st, "su_stg", [128, 1024], F32)
        dma("sp", ident_f[:, :], ident_in[:, :])
        cp("dve", ident_b[:, :], ident_f[:, :])
        dma("sp", stg[:, 0:128], ltri_in[:, :])
        cp("dve", ltri_b[:, :], stg[:, 0:128])
        dma("sp", iota_f[:, :], iota_in[:, :])
        dma("sp", invc[:, :, :], invc_in[:, :, :])
        stg2 = sb(st, "su_stg2", [128, NB, 2], F32)
        dma("sp", stg2[:, :, :], tokhl_in[:, :, :])
        cp("dve", tokhl_b[:, :, :], stg2[:, :, :])
        P.op("dve", "memset", ap=ones_b[:, :], constant=1.0)
        P.op("dve", "memset", ap=ones_f[:, :], constant=1.0)
        P.op("dve", "memset", ap=zeros_b[:, :], constant=0.0)
        P.op("dve", "memset", ap=sel_f[:, :], constant=0.0)
        P.op("dve", "memset", ap=sel_f[64:65, :], constant=1.0)
        P.op("dve", "memset", ap=eps_t[:, :], constant=EPS)
        P.op("dve", "memset", ap=chalf[:, :], constant=-0.5)
        P.op("dve", "memset", ap=phalf[:, :], constant=0.5)
        dma("sp", cact[:, :], cT_in[:, :])
        act(cact[:, :], cact[:, :], AF.Silu)
        P.op("dve", "memset", ap=stg[:, 0:64], constant=0.0)
        zv = V(stg.h[:, 0:32].rearrange("p (g c) -> p g c", g=4), stg[:, :].bufs)
        dma("pool", pu_d.p("pad", (slice(None), slice(None), slice(0, 8))), zv)
        dma("pool", pu_d.p("pad", (slice(None), slice(None), slice(S + 8, S + 16))), zv)
        zv1 = V(stg.h[:, 0:4].rearrange("p (g c) -> p g c", g=4), stg[:, :].bufs)
        dma("pool", z_d.p("pad", (slice(None), slice(None), slice(0, 1))), zv1, allow_slow_non_contiguous=True)
        dma("pool", z_d.p("pad", (slice(None), slice(None), slice(S + 1, S + 2))), zv1, allow_slow_non_contiguous=True)
        rc = sb(st, "su_rc", [96, 4], F32)
        dma("sp", rc[:, :], ropec_in[:, :])
        CH = min(S, 2048)
        posi = sb(st, "su_posi", [96, CH], I32)
        posf = sb(st, "su_posf", [96, CH], F32)
        a2 = sb(st, "su_a2", [96, CH], F32)
        ki = sb(st, "su_ki", [96, CH], I32)
        kf = sb(st, "su_kf", [96, CH], F32)
        r_ = sb(st, "su_r", [96, CH], F32)
        m_ = sb(st, "su_m", [96, CH], F32)
        PR = slice(64, 96)
        C1 = 6.28125
        C2 = 2.0 * math.pi - 6.28125
        for c0 in range(0, S, CH):
            dma("sp", posi[PR, :], V(pos_in.h[0:1, c0:c0 + CH].broadcast_to([32, CH]), pos_in[:, :].bufs))
            cp("dve", posf[PR, :], posi[PR, :])
            for which, dst in ((1, cc_d), (2, ss_d)):
                ts("dve", a2[PR, :], posf[PR, :], rc[PR, 0:1], ALU.mult, rc[PR, which:which + 1], ALU.add)
                ts("dve", m_[PR, :], a2[PR, :], 1.0 / (2.0 * math.pi), ALU.mult)
                cp("dve", ki[PR, :], m_[PR, :])
                cp("dve", kf[PR, :], ki[PR, :])
                stt("dve", r_[PR, :], kf[PR, :], -C1, a2[PR, :], ALU.mult, ALU.add)
                stt("dve", r_[PR, :], kf[PR, :], -C2, r_[PR, :], ALU.mult, ALU.add)
                ts("dve", m_[PR, :], r_[PR, :], math.pi, ALU.is_gt, -2.0 * math.pi, ALU.mult)
                tt("dve", r_[PR, :], r_[PR, :], m_[PR, :], ALU.add)
                ts("dve", m_[PR, :], r_[PR, :], -math.pi, ALU.is_lt, 2.0 * math.pi, ALU.mult)
                tt("dve", r_[PR, :], r_[PR, :], m_[PR, :], ALU.add)
                ts("dve", r_[PR, :], r_[PR, :], -3.141592, ALU.max, 3.141592, ALU.min)
                act(m_[PR, :], r_[PR, :], AF.Sin)
                dma("pool", dst.p("all", (slice(None), slice(c0, c0 + CH))), m_[PR, :])
    P.barrier()

    def make_norm(st, tag):
        xring = sbring(st, f"{tag}_x", [128, D], F32, 2)
        sqj = sb(st, f"{tag}_sqj", [128, D], BF16)
        xnring = sbring(st, f"{tag}_xn", [128, D], BF16, 2)
        rs = sbring(st, f"{tag}_rs", [128, 2], F32, 2)

        def norm_tile(xsrc, i, hT):
            for j in range(4):
                blk = i * 4 + j
                xt = xring.next()
                r = rs.next()
                dma("sp", xt[:, :], xsrc.p(blk, (slice(blk * 128, blk * 128 + 128), slice(None))))
                act(sqj[:, :], xt[:, :], AF.Square, accum_out=r[:, 0:1])
                rsqrt_from_ss(r[:, 1:2], r[:, 0:1], D)
                xn = xnring.next()
                act(xn[:, :], xt[:, :], AF.Copy, scale=r[:, 1:2])
                pt = auxring.next()
                ptv = pt.h[:, :].bitcast(BF16)
                for kc in range(KC):
                    P.op("pe", "transpose", out=V(ptv[:, kc * 128:(kc + 1) * 128], pt[:, :].bufs),
                         in_=xn[:, kc * 128:(kc + 1) * 128], identity=ident_b[:, :])
                pv = V(ptv.rearrange("p (k t) -> p k t", k=KC), pt[:, :].bufs)
                hv = V(hT.h[:, :, j * 128:(j + 1) * 128], hT[:, :, :].bufs)
                tt("dve", hv, pv, bview(gm1T[:, :], [128, KC, 128]), ALU.mult)
                tt("dve", hv, hv, bview(sh1T[:, :], [128, KC, 128]), ALU.add)

        return norm_tile

    def load_mod_T(l):
        with ExitStack() as st:
            row = sb(st, "lm_row", [1, 2 * D], F32)
            n1 = sb(st, "lm_n1", [128, KC], F32)
            dma("sp", row[:, :], mod_d.p("all", (slice(0, 1), slice(0, 2 * D))))
            dma("sp", n1[:, :], n1g_in.p(l, (l, slice(None), slice(None))))
            ps = auxring.next()
            for j in range(2 * KC):
                mm(ps[:, j:j + 1], row[0:1, j * 128:(j + 1) * 128], ones_f[0:1, 0:1], True, True)
            cp("dve", sh1T[:, :], ps[:, 0:KC])
            ts("dve", gm1T[:, :], ps[:, KC:2 * KC], 1.0, ALU.add)
            tt("dve", gm1T[:, :], gm1T[:, :], n1[:, :], ALU.mult)
        P.barrier()

    def bc_row(dst, off, stq, scratch_row):
        dma("sp", scratch_row[:, :], mod_d.p("all", (slice(0, 1), slice(off, off + D))))
        for hf in range(2):
            ps = auxring.next()
            mm(ps[:, :], ones_f[0:1, :], scratch_row[0:1, hf * 512:(hf + 1) * 512], True, True)
            cp("dve", dst[:, hf * 512:(hf + 1) * 512], ps[:, :])

    class _Stop(Exception):
        pass

    def chk(n):
        if _os.environ.get('KVERB'):
            print('chk', n, 'ops', len(P.ops))
        if stop <= n:
            raise _Stop()

    try:
      for l in range(L):
        chk(0)
        xsrc = x_in if l == 0 else xm_d

        with ExitStack() as st:
            wst = sbring(st, "m_w", [128, KC, 512], F32, 4)
            mrow = sb(st, "m_row", [1, 6 * D], F32)
            brow = sb(st, "m_brow", [1, 6 * D], F32)
            dma("sp", brow[:, :], b_mod.p(l, (l, slice(None), slice(None))))
            for g in range(12):
                w = wst.next()
                dma("sp", w[:, :, :], V(w_mod.h[l].rearrange("(kc p) c -> p kc c", p=128)[:, :, g * 512:(g + 1) * 512],
                                        w_mod.p(l, (l,)).bufs))
                ps = mmring.next()
                for kc in range(KC):
                    mm(ps[0:1, :], cact[:, kc:kc + 1], w[:, kc, :], kc == 0, kc == KC - 1)
                tt("dve", mrow[0:1, g * 512:(g + 1) * 512], ps[0:1, :], brow[0:1, g * 512:(g + 1) * 512], ALU.add)
            dma("pool", mod_d.p("all", (slice(None), slice(None))), mrow[:, :])
        P.barrier()
        chk(1)
        load_mod_T(l)

        with ExitStack() as st:
            stg = sbring(st, "c_stg", [128, 8192], F32, 3)
            ob = sbring(st, "c_ob", [128, 8192], BF16, 3)
            g1bc = sb(st, "c_g1bc", [128, D], F32)
            srow = sb(st, "c_srow", [1, D], F32)
            bc_row(g1bc, 2 * D, st, srow)
            engs = ["dve", "act", "pool"]
            ei = [0]

            def ce():
                ei[0] += 1
                return engs[ei[0] % 3]

            for kc in range(KC):
                s_ = stg.next()
                o_ = ob.next()
                dma("sp", s_[:, 0:WIN_EXT], w_in.p(l, (l, slice(kc * 128, kc * 128 + 128), slice(None))))
                for (a, b, o0) in ((384, 640, 0), (576, 672, 256), (5792, 5888, 352), (672, 1696, 448), (2208, 2720, 1472)):
                    cp(ce(), o_[:, o0:o0 + (b - a)], s_[:, a:b])
                cp(ce(), o_[:, 2048:2048 + 384], s_[:, 0:384])
                cp(ce(), o_[:, 2432:2432 + 512], s_[:, 1696:2208])
                gin = V(s_.h[:, 2720:5792].rearrange("p (j c i) -> p j c i", j=3, c=8), s_[:, :].bufs)
                gout = V(o_.h[:, 3072:3072 + 3072].rearrange("p (c j i) -> p j c i", j=3, c=8), o_[:, :].bufs)
                for j in range(3):
                    cp(ce(), V(gout.ap[:, j], gout.bufs), V(gin.ap[:, j], gin.bufs))
                dma("pool", wa1_d.p("all", (slice(None), kc, slice(None))), o_[:, 0:1984])
                dma("pool", wa2_d.p("all", (slice(None), kc, slice(None))), o_[:, 2048:2048 + 896])
                dma("pool", wgt_d.p("all", (slice(None), slice(None), kc, slice(None))),
                    V(o_.h[:, 3072:6144].rearrange("p (c x) -> p c x", c=8), o_[:, :].bufs))
            s_ = stg.next(); o_ = ob.next()
            dma("sp", V(s_.h[0:64, 0:8192].rearrange("p (h x) -> p h x", h=H), s_[:, :].bufs), woa_in.p(l, (l,)))
            for h in range(H):
                cp(ce(), V(o_.h[0:64, 0:8192].rearrange("p (c h i) -> p h c i", c=8, h=H)[:, h], o_[:, :].bufs),
                   V(s_.h[0:64, h * 1024:(h + 1) * 1024].rearrange("p (c i) -> p c i", c=8), s_[:, :].bufs))
            dma("pool", woa_d.p("all", (slice(None),)), V(o_.h[0:64, 0:8192].rearrange("p (c h i) -> p c h i", c=8, h=H), o_[:, :].bufs))
            s_ = stg.next(); o_ = ob.next()
            dma("sp", V(s_.h[:, 0:4096].rearrange("p (k x) -> p k x", k=4), s_[:, :].bufs),
                V(woc_in.h[l].rearrange("(k p) x -> p k x", p=128), woc_in.p(l, (l,)).bufs))
            for k in range(4):
                cp(ce(), V(o_.h[:, 0:4096].rearrange("p (c k i) -> p k c i", c=8, k=4)[:, k], o_[:, :].bufs),
                   V(s_.h[:, k * 1024:(k + 1) * 1024].rearrange("p (c i) -> p c i", c=8), s_[:, :].bufs))
            dma("pool", woc_d.p("all", (slice(None),)), V(o_.h[:, 0:4096].rearrange("p (c k i) -> p c k i", c=8, k=4), o_[:, :].bufs))
            s_ = stg.next(); o_ = ob.next()
            dma("sp", V(s_.h[:, 0:8192].rearrange("p (k x) -> p k x", k=KC), s_[:, :].bufs),
                V(wout_in.h[l].rearrange("(k p) x -> p k x", p=128), wout_in.p(l, (l,)).bufs))
            for k in range(KC):
                tt("dve" if k % 2 else "pool", o_[:, k * 1024:(k + 1) * 1024], s_[:, k * 1024:(k + 1) * 1024], g1bc[:, :], ALU.mult)
            dma("pool", wout_d.p("all", (slice(None),)), V(o_.h[:, 0:8192].rearrange("p (k x) -> p k x", k=KC), o_[:, :].bufs))
        P.barrier()
        chk(2)

        with ExitStack() as st:
            wa1 = sb(st, "a1_w", [128, KC, 1984], BF16)
            wk = sb(st, "a1_wk", [128, 2, 512], BF16)
            wv = sb(st, "a1_wv", [128, 2, 512], BF16)
            kvng = sb(st, "a1_kvng", [128, 2], F32)
            stg = sb(st, "a1_stg", [128, 2, 512], F32)
            dma("sp", wa1[:, :, :], wa1_d.p("all", (slice(None),)))
            dma("sp", kvng[:, :], kvng_in.p(l, (l,)))
            for src, dst in ((wk_in, wk), (wv_in, wv)):
                dma("sp", stg[:, :, :], V(src.h[l].rearrange("(k p) x -> p k x", p=128), src.p(l, (l,)).bufs))
                cp("dve", dst[:, :, :], stg[:, :, :])
            P.op("dve", "memset", ap=kmax[:, :], constant=0.0)
            norm_tile = make_norm(st, "a1n")
            hTr = sbring(st, "a1_hT", [128, KC, 512], BF16, 2)
            sq = sb(st, "a1_sq", [128, 2, 512], BF16)
            ckvg = sb(st, "a1_ckvg", [128, 2, 512], BF16)
            rstd = sb(st, "a1_rstd", [128, 512], F32)
            rtok = sb(st, "a1_rtok", [128, 4], F32)
            cct = sbring(st, "a1_cct", [96, 512], F32, 2)
            sst = sbring(st, "a1_sst", [96, 512], F32, 2)
            t1 = sb(st, "a1_t1", [96, 512], F32)
            t2 = sb(st, "a1_t2", [96, 512], F32)
            ktt = sbring(st, "a1_kt", [96, H, 512], BF16, 2)
            sqk = sb(st, "a1_sqk", [96, H, 512], BF16)
            vt = sbring(st, "a1_vt", [128, 4, H, 65], BF16, 2)
            f32r = sbring(st, "a1_f", [128, 512], F32, 4)
            kmt = sb(st, "a1_kmt", [128, H], F32)
            for i in range(NT):
                ts_ = slice(i * 512, (i + 1) * 512)
                hT = hTr.next()
                norm_tile(xsrc, i, hT)
                CC = cct.next(); SS = sst.next()
                dma("sp", CC[PR, :], cc_d.p("all", (slice(None), ts_)))
                dma("sp", SS[PR, :], ss_d.p("all", (slice(None), ts_)))
                chk(2.1)
                for m in range(2):
                    ps = mmring.next()
                    for kc in range(KC):
                        mm(ps[:, :], wa1[:, kc, m * 128:(m + 1) * 128], hT[:, kc, :], kc == 0, kc == KC - 1)
                    act(sq[:, m, :], ps[:, :], AF.Square)
                    act(ckvg[:, m, :], ps[:, :], AF.Copy, scale=kvng[:, m:m + 1])
                ps = auxring.next()
                for m in range(2):
                    mm(ps[:, :], ones_b[:, :], sq[:, m, :], m == 0, m == 1)
                rsqrt_from_ss(rstd[:, :], ps[:, :], 256)
                chk(2.2)
                ps = auxring.next()
                for j in range(4):
                    for m in range(2):
                        mm(ps[:, j:j + 1], sq[:, m, j * 128:(j + 1) * 128], ones_b[:, 0:1], m == 0, m == 1)
                rsqrt_from_ss(rtok[:, :], ps[:, 0:4], 256)
                chk(2.3)
                KT = ktt.next()
                for h in range(H):
                    ps = mmring.next()
                    for m in range(2):
                        mm(ps[0:64, :], wk[:, m, h * 64:(h + 1) * 64], ckvg[:, m, :], m == 0, m == 1)
                    tt("dve", KT[0:64, h, :], ps[0:64, :], rstd[0:64, :], ALU.mult)
                chk(2.4)
                psa = mmring.next()
                for kc in range(KC):
                    mm(psa[0:96, :], wa1[:, kc, 256:352], hT[:, kc, :], kc == 0, kc == KC - 1)
                psb = mmring.next()
                for kc in range(KC):
                    mm(psb[0:96, :], wa1[:, kc, 352:448], hT[:, kc, :], kc == 0, kc == KC - 1)
                tt("dve", t1[PR, :], psa[PR, :], CC[PR, :], ALU.mult)
                tt("dve", t2[PR, :], psb[PR, :], SS[PR, :], ALU.mult)
                for h in range(H):
                    tt("pool" if h % 2 else "dve", KT[PR, h, :], t1[PR, :], t2[PR, :], ALU.add)
                dma("pool", kt_d.p(i, (slice(None), slice(None), ts_)), KT[:, :, :])
                chk(2.6)
                VT = vt.next()
                P.op("pool", "memset", ap=VT[:, :, :, 64:65], constant=1.0)
                for j in range(4):
                    ps = mmring.next()
                    for m in range(2):
                        mm(ps[:, :], ckvg[:, m, j * 128:(j + 1) * 128], wv[:, m, :], m == 0, m == 1)
                    act(VT[:, j, :, 0:64], V(ps.h[:, :].rearrange("p (h d) -> p h d", h=H), ps[:, :].bufs),
                        AF.Copy, scale=rtok[:, j:j + 1])
                dma("pool", v_d.p(i, (slice(None), slice(i * 4, i * 4 + 4))), VT[:, :, :, :])
                chk(2.7)
                for g in range(4):
                    ps = mmring.next()
                    for kc in range(KC):
                        mm(ps[:, :], wa1[:, kc, 448 + g * 128:448 + (g + 1) * 128], hT[:, kc, :], kc == 0, kc == KC - 1)
                    f = f32r.next()
                    cp("act", f[:, :], ps[:, :])
                    dma("pool", pu_d.p(i, (slice(None), g, slice(8 + i * 512, 8 + (i + 1) * 512))), f[:, :])
                chk(2.8)
                for g in range(4):
                    ps = mmring.next()
                    for kc in range(KC):
                        mm(ps[:, :], wa1[:, kc, 960 + g * 128:960 + (g + 1) * 128], hT[:, kc, :], kc == 0, kc == KC - 1)
                    f = f32r.next()
                    cp("act", f[:, :], ps[:, :])
                    ps2 = mmring.next()
                    for kc in range(KC):
                        mm(ps2[:, :], wa1[:, kc, 1472 + g * 128:1472 + (g + 1) * 128], hT[:, kc, :], kc == 0, kc == KC - 1)
                    f2 = f32r.next()
                    tt("dve", f2[:, :], ps2[:, :], f[:, :], ALU.mult)
                    dma("pool", z_d.p(i, (slice(None), g, slice(1 + i * 512, 1 + (i + 1) * 512))), f2[:, :])
                chk(2.5)
                act(sqk[:, :, :], KT[:, :, :], AF.Square)
                for h in range(H):
                    ps = auxring.next()
                    mm(ps[:, :], ones_b[0:96, :], sqk[0:96, h, :], True, True)
                    P.op("dve", "tensor_reduce", out=kmt[:, h:h + 1], in_=ps[:, :], axis=AX.X, op=ALU.max)
                tt("dve", kmax[:, :], kmax[:, :], kmt[:, :], ALU.max)
        P.barrier()
        chk(3)

        with ExitStack() as st:
            KTs = sb(st, "at_KT", [96, H, S], BF16)
            Vs = sb(st, "at_V", [128, NB, H, 65], BF16)
            for i in range(NT):
                dma("sp", KTs.p(i, (slice(None), slice(None), slice(i * 512, (i + 1) * 512))),
                    kt_d.p(i, (slice(None), slice(None), slice(i * 512, (i + 1) * 512))))
                dma("sp", Vs.p(i, (slice(None), slice(i * 4, i * 4 + 4))), v_d.p(i, (slice(None), slice(i * 4, i * 4 + 4))))
            wcq = sb(st, "at_wcq", [128, KC, 384], BF16)
            dma("sp", wcq[:, :, :], wa2_d.p("all", (slice(None), slice(None), slice(0, 384))))
            wA = sb(st, "at_wA", [128, 3, 768], BF16)
            wB = sb(st, "at_wB", [128, 3, 768], BF16)
            qng = sb(st, "at_qng", [128, 3], F32)
            dma("sp", qng[:, :], qng_in.p(l, (l,)))
            with ExitStack() as stw:
                stg = sb(stw, "at_stg", [128, 3, 768], F32)
                for src, dst in ((wuqA_in, wA), (wuqB_in, wB)):
                    dma("sp", stg[:, :, :], V(src.h[l].rearrange("(k p) x -> p k x", p=128), src.p(l, (l,)).bufs))
                    cp("dve", dst[:, :, :], stg[:, :, :])
            P.barrier()
            norm_tile = make_norm(st, "atn")
            hTr = sbring(st, "at_hT", [128, KC, 512], BF16, 1)
            sq = sb(st, "at_sq", [128, 3, 512], BF16)
            cqg = sb(st, "at_cqg", [128, 3, 512], BF16)
            rstd = sb(st, "at_rstd", [128, 512], F32)
            cct = sbring(st, "at_cct", [96, 512], F32, 2)
            sst = sbring(st, "at_sst", [96, 512], F32, 2)
            ta = sbring(st, "at_ta", [96, 512], F32, 1)
            tb = sbring(st, "at_tb", [96, 512], F32, 1)
            qTr = sbring(st, "at_qT", [96, H, 512], BF16, 2)
            sqq = sbring(st, "at_sqq", [96, 512], BF16, 2)
            negb = sbring(st, "at_negb", [128, H], F32, 2)
            qm = sbring(st, "at_qm", [128, 2], F32, 4)
            ptr_ = sbring(st, "at_pt", [128, 1024], BF16, 3)
            osb = sbring(st, "at_osb", [65, 512], F32, 2)
            rec = sbring(st, "at_rec", [64, 512], F32, 2)
            oTr = sbring(st, "at_oT", [64, H, 512], BF16, 1)
            allk = list(range(NT))
            pending = [None]

            def prep(i):
                ts_ = slice(i * 512, (i + 1) * 512)
                hT = hTr.next()
                norm_tile(xsrc, i, hT)
                CC = cct.next(); SS = sst.next()
                dma("sp", CC[PR, :], cc_d.p("all", (slice(None), ts_)))
                dma("sp", SS[PR, :], ss_d.p("all", (slice(None), ts_)))
                for m in range(3):
                    ps = auxring.next()
                    for kc in range(KC):
                        mm(ps[:, :], wcq[:, kc, m * 128:(m + 1) * 128], hT[:, kc, :], kc == 0, kc == KC - 1)
                    act(sq[:, m, :], ps[:, :], AF.Square)
                    act(cqg[:, m, :], ps[:, :], AF.Copy, scale=qng[:, m:m + 1])
                ps = auxring.next()
                for m in range(3):
                    mm(ps[:, :], ones_b[:, :], sq[:, m, :], m == 0, m == 2)
                rsqrt_from_ss(rstd[:, :], ps[:, :], 384)
                tt("dve", CC[PR, :], CC[PR, :], rstd[PR, :], ALU.mult)
                tt("pool", SS[PR, :], SS[PR, :], rstd[PR, :], ALU.mult)
                return dict(CC=CC, SS=SS, qT=qTr.next(), nb=negb.next(), s2={})

            def q1(stt_, h):
                CC, SS, qT = stt_["CC"], stt_["SS"], stt_["qT"]
                psA = auxring.next()
                for m in range(3):
                    mm(psA[0:96, :], wA[:, m, h * 96:(h + 1) * 96], cqg[:, m, :], m == 0, m == 2)
                psB = auxring.next()
                for m in range(3):
                    mm(psB[0:96, :], wB[:, m, h * 96:(h + 1) * 96], cqg[:, m, :], m == 0, m == 2)
                tt("dve", qT[0:64, h, :], psA[0:64, :], rstd[0:64, :], ALU.mult)
                a_ = ta.next(); b_ = tb.next()
                tt("dve", a_[PR, :], psA[PR, :], CC[PR, :], ALU.mult)
                tt("dve", b_[PR, :], psB[PR, :], SS[PR, :], ALU.mult)
                tt("pool", qT[PR, h, :], a_[PR, :], b_[PR, :], ALU.add)
                s2 = sqq.next()
                tt("pool", s2[0:96, :], qT[0:96, h, :], qT[0:96, h, :], ALU.mult)
                stt_["s2"][h] = s2

            def q2(stt_, h):
                s2 = stt_["s2"].pop(h)
                nb_ = stt_["nb"]
                ps = auxring.next()
                mm(ps[:, :], ones_b[0:96, :], s2[0:96, :], True, True)
                q_ = qm.next()
                P.op("dve", "tensor_reduce", out=q_[:, 0:1], in_=ps[:, :], axis=AX.X, op=ALU.max)
                tt("dve", q_[:, 1:2], q_[:, 0:1], kmax[:, h:h + 1], ALU.mult)
                P.op("pool", "tensor_tensor", out=q_[:, 1:2], in0=q_[:, 1:2], in1=phalf[:, 0:1], op=ALU.pow)
                ts("dve", nb_[:, h:h + 1], q_[:, 1:2], -SCALE * 1.03, ALU.mult)

            def qorder(stt_):
                items = []
                for h in range(H):
                    items.append(lambda h=h: q1(stt_, h))
                    if h >= 1:
                        items.append(lambda h=h: q2(stt_, h - 1))
                items.append(lambda: q2(stt_, H - 1))
                return items

            cur = prep(0)
            for f_ in qorder(cur):
                f_()
            for i in range(NT):
                ts_ = slice(i * 512, (i + 1) * 512)
                qT = cur["qT"]
                nb_ = cur["nb"]
                nxt_state = None
                todo = []
                oT = oTr.next()
                for h in range(H):
                    acc = accring.next()
                    NP2 = NB // 2
                    pss = [None] * NP2

                    def qk2(kp_):
                        pss[kp_] = mm2ring.next()
                        for u in range(2):
                            kb_ = 2 * kp_ + u
                            mm(pss[kp_][:, u * 512:(u + 1) * 512], KTs.p(allk, (slice(0, 96), h, slice(kb_ * 128, (kb_ + 1) * 128))),
                               qT[0:96, h, :], True, True)

                    qk2(0)
                    for kp in range(NP2):
                        if kp + 1 < NP2:
                            qk2(kp + 1)
                        if kp == min(1, NP2 - 1) and pending[0] is not None:
                            pending[0]()
                            pending[0] = None
                        if i + 1 < NT:
                            if h == 0 and kp == min(2, NP2 - 1):
                                nxt_state = prep(i + 1)
                                todo = qorder(nxt_state)
                            elif h >= 1 and kp in (3, 8, 13) and todo:
                                todo.pop(0)()
                        pt = ptr_.next()
                        act(pt[:, :], pss[kp][:, :], AF.Exp, scale=SCALE, bias=nb_[:, h:h + 1])
                        for u in range(2):
                            kb = 2 * kp + u
                            mm(acc[0:65, :], Vs.p(allk, (slice(None), kb, h, slice(None))), pt[:, u * 512:(u + 1) * 512], kb == 0, kb == NB - 1)
                        pss[kp] = None

                    def fin(h=h, acc=acc, oT=oT):
                        o_ = osb.next()
                        cp("act", o_[0:65, :], acc[0:65, :])
                        den = auxring.next()
                        mm(den[0:64, :], sel_f[0:65, 0:64], o_[0:65, :], True, True)
                        rc_ = rec.next()
                        P.op("dve", "reciprocal", out=rc_[:, :], in_=den[0:64, :])
                        tt("pool", oT[0:64, h, :], o_[0:64, :], rc_[:, :], ALU.mult)

                    pending[0] = fin
                while todo:
                    todo.pop(0)()
                pending[0]()
                pending[0] = None
                dma("pool", ot_d.p(i, (slice(None), slice(None), ts_)), oT[:, :, :])
                cur = nxt_state
        P.barrier()
        chk(4)

        with ExitStack() as st:
            wcb = sb(st, "a2_wcb", [128, KC, 512], BF16)
            dma("sp", wcb[:, :, :], wa2_d.p("all", (slice(None), slice(None), slice(384, 896))))
            wout = sb(st, "a2_wout", [128, KC, D], BF16)
            dma("sp", wout[:, :, :], wout_d.p("all", (slice(None),)))
            wpool = sb(st, "a2_wpool", [128, 4, 256], BF16)
            stg = sb(st, "a2_stg", [128, 4, 256], F32)
            dma("sp", stg[:, :, :], wpool_in.p(l, (l,)))
            cp("dve", wpool[:, :, :], stg[:, :, :])
            bgate = sb(st, "a2_bgate", [128, 24], F32)
            pscale = sb(st, "a2_pscale", [128, KC], F32)
            convw = sb(st, "a2_convw", [128, 4, 3], F32)
            dma("sp", bgate[:, :], bgate_in.p(l, (l,)))
            dma("sp", pscale[:, :], pscale_in.p(l, (l,)))
            dma("sp", convw[:, :, :], convw_in.p(l, (l,)))
            norm_tile = make_norm(st, "a2n")
            hTr = sbring(st, "a2_hT", [128, KC, 512], BF16, 2)
            oTr = sbring(st, "a2_oT", [64, H, 512], BF16, 2)
            Ur = sbring(st, "a2_U", [128, 4, 528], F32, 2)
            Zr = sbring(st, "a2_Z", [128, 4, 514], F32, 2)
            pa = sb(st, "a2_pa", [128, 528], F32)
            pb = sb(st, "a2_pb", [128, 528], F32)
            t8 = sb(st, "a2_t8", [128, 8], F32)
            mixed = sb(st, "a2_mixed", [128, 4, 512], BF16)
            yc = sbring(st, "a2_yc", [128, 512], F32, 2)
            convo = sb(st, "a2_convo", [128, 4, 512], BF16)
            wgr = sbring(st, "a2_wg", [128, KC, 384], BF16, 2)
            woar = sbring(st, "a2_woa", [64, H, 128], BF16, 2)
            wocr = sbring(st, "a2_woc", [128, 4, 128], BF16, 2)
            gt = sbring(st, "a2_gt", [128, 3, 512], F32, 2)
            tmp = sbring(st, "a2_tmp", [128, 512], F32, 6)
            merged = sb(st, "a2_merged", [128, KC, 512], BF16)
            xr = sbring(st, "a2_xr", [128, D], F32, 2)
            xn_ = sbring(st, "a2_xnew", [128, D], F32, 2)
            for i in range(NT):
                ts_ = slice(i * 512, (i + 1) * 512)
                hT = hTr.next()
                norm_tile(xsrc, i, hT)
                oT = oTr.next()
                dma("sp", oT[:, :, :], ot_d.p(i, (slice(None), slice(None), ts_)))
                U = Ur.next(); Z = Zr.next()
                nbr = [k for k in (i - 1, i, i + 1) if 0 <= k < NT] + ["pad"]
                dma("sp", U[:, :, :], pu_d.p(nbr, (slice(None), slice(None), slice(i * 512, i * 512 + 528))))
                dma("sp", Z[:, :, :], z_d.p(nbr, (slice(None), slice(None), slice(i * 512, i * 512 + 514))))
                for g in range(4):
                    w = 2 << g
                    half = w // 2
                    cur, n = None, 528
                    bufs2 = [pa, pb]
                    src = V(U.h[:, g, :], U[:, :, :].bufs)
                    k = 1
                    bi = 0
                    while k < w:
                        dst = bufs2[bi % 2]; bi += 1
                        n2 = n - k
                        tt("dve" if g >= 2 else "pool", dst[:, 0:n2], V(src.ap[:, 0:n2], src.bufs), V(src.ap[:, k:k + n2], src.bufs), ALU.add)
                        src = V(dst.h[:, :], dst[:, :].bufs)
                        n = n2
                        k *= 2
                    sw = src
                    stt("dve", mixed[:, g, :], V(sw.ap[:, 8 - half:8 - half + 512], sw.bufs), 1.0 / w,
                        V(U.h[:, g, 8:520], U[:, :, :].bufs), ALU.mult, ALU.subtract)
                    if i == 0:
                        tt("dve", t8[:, :], V(sw.ap[:, 8 - half:16 - half], sw.bufs), invc[:, g, 0:8], ALU.mult)
                        tt("dve", mixed[:, g, 0:8], t8[:, :], V(U.h[:, g, 8:16], U[:, :, :].bufs), ALU.subtract)
                    if i == NT - 1:
                        tt("dve", t8[:, :], V(sw.ap[:, 512 - half:520 - half], sw.bufs), invc[:, g, 8:16], ALU.mult)
                        tt("dve", mixed[:, g, 504:512], t8[:, :], V(U.h[:, g, 512:520], U[:, :, :].bufs), ALU.subtract)
                for k4 in range(4):
                    y = yc.next()
                    ts("dve", y[:, :], Z[:, k4, 0:512], convw[:, k4, 0:1], ALU.mult)
                    stt("dve", y[:, :], Z[:, k4, 1:513], convw[:, k4, 1:2], y[:, :], ALU.mult, ALU.add)
                    stt("dve", y[:, :], Z[:, k4, 2:514], convw[:, k4, 2:3], y[:, :], ALU.mult, ALU.add)
                    ps = mmring.next()
                    for kc in range(KC):
                        mm(ps[:, :], wcb[:, kc, k4 * 128:(k4 + 1) * 128], hT[:, kc, :], kc == 0, kc == KC - 1)
                    tt("dve", convo[:, k4, :], ps[:, :], y[:, :], ALU.mult)
                for c in range(8):
                    wg = wgr.next(); woa = woar.next(); woc = wocr.next()
                    dma("sp", wg[:, :, :], wgt_d.p("all", (slice(None), c)))
                    dma("sp", woa[:, :, :], woa_d.p("all", (slice(None), c)))
                    dma("sp", woc[:, :, :], woc_d.p("all", (slice(None), c)))
                    G = gt.next()
                    for j in range(3):
                        ps = mmring.next()
                        for kc in range(KC):
                            mm(ps[:, :], wg[:, kc, j * 128:(j + 1) * 128], hT[:, kc, :], kc == 0, kc == KC - 1)
                        act(G[:, j, :], ps[:, :], AF.Sigmoid, bias=bgate[:, j * 8 + c:j * 8 + c + 1])
                    psa = accring.next()
                    for h in range(H):
                        mm(psa[:, :], woa[0:64, h, :], oT[0:64, h, :], h == 0, h == H - 1)
                    t0 = tmp.next()
                    tt("dve", t0[:, :], psa[:, :], G[:, 0, :], ALU.mult)
                    psb = accring.next()
                    mm(psb[:, :], wpool[:, c // 2, (c % 2) * 128:(c % 2) * 128 + 128], mixed[:, c // 2, :], True, True)
                    t1_ = tmp.next()
                    stt("dve", t1_[:, :], psb[:, :], pscale[:, c:c + 1], G[:, 1, :], ALU.mult, ALU.mult)
                    psc = auxring.next()
                    for k4 in range(4):
                        mm(psc[:, :], woc[:, k4, :], convo[:, k4, :], k4 == 0, k4 == 3)
                    t2_ = tmp.next()
                    tt("dve", t2_[:, :], psc[:, :], G[:, 2, :], ALU.mult)
                    tt("pool", t0[:, :], t0[:, :], t1_[:, :], ALU.add)
                    tt("pool", merged[:, c, :], t0[:, :], t2_[:, :], ALU.add)
                for j in range(4):
                    blk = i * 4 + j
                    xt = xr.next(); xo = xn_.next()
                    rows = slice(blk * 128, blk * 128 + 128)
                    dma("sp", xt[:, :], xsrc.p(blk, (rows, slice(None))))
                    for hf in range(2):
                        ps = mmring.next()
                        for kc in range(KC):
                            mm(ps[:, :], merged[:, kc, j * 128:(j + 1) * 128], wout[:, kc, hf * 512:(hf + 1) * 512], kc == 0, kc == KC - 1)
                        tt("dve", xo[:, hf * 512:(hf + 1) * 512], ps[:, :], xt[:, hf * 512:(hf + 1) * 512], ALU.add)
                    dma("pool", xm_d.p(blk, (rows, slice(None))), xo[:, :])
                    if debug and l == 0:
                        dma("pool", dbg["dbg_x1"].p(blk, (rows, slice(None))), xo[:, :])
        P.barrier()
        chk(5)

        with ExitStack() as st:
            gm2 = sb(st, "b_gm2", [128, D], F32)
            sh2 = sb(st, "b_sh2", [128, D], F32)
            g2 = sb(st, "b_g2", [128, D], F32)
            aff = sb(st, "b_aff", [128, NB, NE], F32)
            slotm = sb(st, "b_slotm", [128, NB, NE], F32)
            tinfo = sb(st, "b_tinfo", [128, NB, NE, 5], BF16)
            allb = list(range(NB))
            with ExitStack() as st1:
                srow = sb(st1, "b1_srow", [1, D], F32)
                n2row = sb(st1, "b1_n2row", [1, D], F32)
                n2bc = sb(st1, "b1_n2bc", [128, D], F32)
                bc_row(sh2, 3 * D, st1, srow)
                bc_row(gm2, 4 * D, st1, srow)
                bc_row(g2, 5 * D, st1, srow)
                dma("sp", n2row[:, :], n2g_in.p(l, (l,)))
                for hf in range(2):
                    ps = auxring.next()
                    mm(ps[:, :], ones_f[0:1, :], n2row[0:1, hf * 512:(hf + 1) * 512], True, True)
                    cp("dve", n2bc[:, hf * 512:(hf + 1) * 512], ps[:, :])
                ts("dve", gm2[:, :], gm2[:, :], 1.0, ALU.add)
                tt("dve", gm2[:, :], gm2[:, :], n2bc[:, :], ALU.mult)
                wr = sb(st1, "b1_wr", [128, KC, NE], F32)
                dma("sp", wr[:, :, :], wr_in.p(l, (l,)))
                xr = sbring(st1, "b1_x", [128, D], F32, 2)
                sqj = sb(st1, "b1_sqj", [128, D], BF16)
                rs = sbring(st1, "b1_rs", [128, 8], F32, 2)
                h2r = sbring(st1, "b1_h2", [128, D], F32, 2)
                h2br = sbring(st1, "b1_h2b", [128, D], BF16, 2)
                h2Tr = sbring(st1, "b1_h2T", [128, KC, 128], F32, 2)
                ex = sbring(st1, "b1_ex", [128, NE], F32, 2)
                for b in range(NB):
                    rows = slice(b * 128, b * 128 + 128)
                    xt = xr.next(); r = rs.next()
                    dma("sp", xt[:, :], xm_d.p(b, (rows, slice(None))))
                    act(sqj[:, :], xt[:, :], AF.Square, accum_out=r[:, 0:1])
                    rsqrt_from_ss(r[:, 1:2], r[:, 0:1], D)
                    h2 = h2r.next()
                    stt("dve", h2[:, :], xt[:, :], r[:, 1:2], gm2[:, :], ALU.mult, ALU.mult)
                    tt("pool", h2[:, :], h2[:, :], sh2[:, :], ALU.add)
                    h2b = h2br.next()
                    cp("act", h2b[:, :], h2[:, :])
                    dma("pool", h2_d.p(b, (rows, slice(None))), h2b[:, :])
                    h2T = h2Tr.next()
                    for hf in range(2):
                        ps = mmring.next()
                        for k4 in range(4):
                            kc = hf * 4 + k4
                            P.op("pe", "transpose", out=ps[:, k4 * 128:(k4 + 1) * 128], in_=h2[:, kc * 128:(kc + 1) * 128],
                                 identity=ident_f[:, :])
                        cp("dve" if hf else "act", V(h2T.h[:, hf * 4:(hf + 1) * 4, :], h2T[:, :, :].bufs),
                           V(ps.h[:, :].rearrange("p (k t) -> p k t", k=4), ps[:, :].bufs))
                    ps = auxring.next()
                    for kc in range(KC):
                        mm(ps[:, 0:NE], h2T[:, kc, :], wr[:, kc, :], kc == 0, kc == KC - 1)
                    P.op("dve", "tensor_reduce", out=r[:, 2:3], in_=ps[:, 0:NE], axis=AX.X, op=ALU.max)
                    ts("dve", r[:, 3:4], r[:, 2:3], -1.0, ALU.mult)
                    e_ = ex.next()
                    act(e_[:, :], ps[:, 0:NE], AF.Exp, bias=r[:, 3:4], accum_out=r[:, 4:5])
                    P.op("dve", "reciprocal", out=r[:, 5:6], in_=r[:, 4:5])
                    ts("dve", aff.p(b, (slice(None), b, slice(None))), e_[:, :], r[:, 5:6], ALU.mult)
            P.barrier()
            chk(6)
            with ExitStack() as st2:
                W = NB * NE
                lo = sb(st2, "b2_lo", [128, NE], F32)
                hi = sb(st2, "b2_hi", [128, NE], F32)
                mid = sb(st2, "b2_mid", [128, NE], F32)
                c16 = sb(st2, "b2_c16", [128, NE], F32)
                ge = sb(st2, "b2_ge", [128, NE], F32)
                t16 = sb(st2, "b2_t16", [128, NE], F32)
                cmpb = sb(st2, "b2_cmp", [128, NB, NE], BF16)
                maskf = sb(st2, "b2_maskf", [128, NB, NE], F32)
                offs = sb(st2, "b2_offs", [128, NB, NE], F32)
                r1 = sb(st2, "b2_r1", [128, NB, NE], F32)
                r2 = sb(st2, "b2_r2", [128, NB, NE], F32)
                affv = aff.p(allb, (slice(None),))
                P.op("dve", "memset", ap=lo[:, :], constant=0.0)

                def bcast_e(t):
                    return V(t.h[:, :].unsqueeze(1).to_broadcast([128, NB, NE]), t[:, :].bufs)

                for it in range(30):
                    hstep = 2.0 ** -(it + 1)
                    ts("dve", mid[:, :], lo[:, :], hstep, ALU.add)
                    tt("dve", cmpb[:, :, :], affv, bcast_e(mid), ALU.is_ge)
                    ps = auxring.next()
                    mm(ps[:, 0:W], ones_b[:, :], V(cmpb.h[:, :, :].rearrange("p b e -> p (b e)"), cmpb[:, :, :].bufs), True, True)
                    P.op("dve", "tensor_reduce", out=c16[:, :], in_=V(ps.h[:, 0:W].rearrange("p (b e) -> p e b", e=NE), ps[:, :].bufs),
                         axis=AX.X, op=ALU.add)
                    ts("dve", ge[:, :], c16[:, :], float(CAP) - 0.5, ALU.is_ge)
                    tt("dve", t16[:, :], mid[:, :], ge[:, :], ALU.mult)
                    tt("dve", lo[:, :], lo[:, :], t16[:, :], ALU.max)
                tt("dve", maskf[:, :, :], affv, bcast_e(lo), ALU.is_ge)
                cp("dve", cmpb[:, :, :], maskf[:, :, :])
                cmpflat = V(cmpb.h[:, :, :].rearrange("p b e -> p (b e)"), cmpb[:, :, :].bufs)
                psw = auxring.next()
                mm(psw[:, 0:W], ltri_b[:, :], cmpflat, True, True)
                pst = auxring.next()
                mm(pst[:, 0:W], ones_b[:, :], cmpflat, True, True)
                cp("dve", V(r1.h[:, :, :].rearrange("p b e -> p (b e)"), r1[:, :, :].bufs), pst[:, 0:W])
                P.op("dve", "memset", ap=offs[:, 0, :], constant=0.0)
                for b in range(1, NB):
                    tt("dve", offs[:, b, :], offs[:, b - 1, :], r1[:, b - 1, :], ALU.add)
                tt("dve", V(r2.h[:, :, :].rearrange("p b e -> p (b e)"), r2[:, :, :].bufs), psw[:, 0:W],
                   V(offs.h[:, :, :].rearrange("p b e -> p (b e)"), offs[:, :, :].bufs), ALU.add)
                stt("dve", r2[:, :, :], r2[:, :, :], 1.0, maskf[:, :, :], ALU.add, ALU.mult)
                ts("dve", slotm[:, :, :], r2[:, :, :], -1.0, ALU.add)
                for q in range(2):
                    cp("dve", V(tinfo.h[:, :, :, q], tinfo[:, :, :, :].bufs),
                       V(tokhl_b.h[:, :, q:q + 1].to_broadcast([128, NB, NE]), tokhl_b[:, :, :].bufs))
                a1v = V(tinfo.h[:, :, :, 2], tinfo[:, :, :, :].bufs)
                a2v = V(tinfo.h[:, :, :, 3], tinfo[:, :, :, :].bufs)
                a3v = V(tinfo.h[:, :, :, 4], tinfo[:, :, :, :].bufs)
                cp("dve", a1v, affv)
                tt("dve", r1[:, :, :], affv, a1v, ALU.subtract)
                cp("dve", a2v, r1[:, :, :])
                tt("dve", r2[:, :, :], r1[:, :, :], a2v, ALU.subtract)
                cp("dve", a3v, r2[:, :, :])
            P.barrier()
            chk(7)
            with ExitStack() as st3:
                stgr = sbring(st3, "b3_stg", [128, 4, D], F32, 4)
                wbr = sbring(st3, "b3_w", [128, KC, D], BF16, 4)
                ohr = sbring(st3, "b3_oh", [128, CAP], BF16, 3)
                idxf = sbring(st3, "b3_idxf", [128, SCH], F32, 2)
                pcsr = sbring(st3, "b3_pcs", [128, SCH * 8], F32, 2)
                idxi = sbring(st3, "b3_idxi", [128, SCH], I32, 2)
                val = sbring(st3, "b3_val", [128, SCH], F32, 2)
                xgr = sbring(st3, "b3_xg", [128, SCH, D], BF16, 2)
                sgr = sbring(st3, "b3_sg", [128, CAP], F32, 1)
                aT = sb(st3, "b3_aT", [128, KC, CAP], BF16)
                ysr = sbring(st3, "b3_ys", [128, D], F32, 2)
                cengs = ["dve", "act"]
                ci = [0]

                def loadw_issue(src, e):
                    wt = wbr.next()
                    stgs = []
                    for hf in range(2):
                        s_ = stgr.next()
                        dma("sp", s_[:, :, :], V(src.h[l, e, hf * 512:(hf + 1) * 512, :].rearrange("(k p) x -> p k x", p=128),
                                                 src.p((l, e), (l, e)).bufs))
                        stgs.append(s_)
                    return (wt, stgs)

                def loadw_cast(lw):
                    wt, stgs = lw
                    for hf, s_ in enumerate(stgs):
                        ci[0] += 1
                        cp(cengs[ci[0] % 2], V(wt.h[:, hf * 4:(hf + 1) * 4, :], wt[:, :, :].bufs), s_[:, :, :])
                    return wt

                def loadw(src, e):
                    return loadw_cast(loadw_issue(src, e))

                xgTr = sbring(st3, "b3_xgTr", [128, KC, CAP], BF16, 2)

                def stageA1(e):
                    pidx = accring.next()
                    mm(pidx[:, 0:SCH * 8], zeros_b[:, :], zeros_b[:, 0:SCH * 8], True, False, skip_group_check=True)
                    for b in range(NB):
                        oh = ohr.next()
                        ts("dve", oh[:, :], iota_f[:, 0:CAP], slotm[:, b, e:e + 1], ALU.is_equal)
                        for sc in range(SCH):
                            mm(pidx[:, sc * 8:sc * 8 + 5], oh[:, sc * 128:(sc + 1) * 128], tinfo[:, b, e, :], False, b == NB - 1,
                               skip_group_check=True)
                    pcs = pcsr.next()
                    cp("act", pcs[:, :], pidx[:, 0:SCH * 8])
                    pv = V(pcs.h[:, :].rearrange("p (s c) -> p s c", c=8), pcs[:, :].bufs)
                    xf = idxf.next(); xi = idxi.next(); vl = val.next()
                    stt("dve", xf[:, :], V(pv.ap[:, :, 0], pv.bufs), 64.0, V(pv.ap[:, :, 1], pv.bufs), ALU.mult, ALU.add)
                    cp("dve", xi[:, :], xf[:, :])
                    tt("dve", vl[:, :], V(pv.ap[:, :, 2], pv.bufs), V(pv.ap[:, :, 3], pv.bufs), ALU.add)
                    tt("dve", vl[:, :], vl[:, :], V(pv.ap[:, :, 4], pv.bufs), ALU.add)
                    xg = xgr.next()
                    for sc in range(SCH):
                        P.op("pool", "indirect_dma_start", dma=True, reads=[xi[:, :], h2_d.p(allb, (slice(None),))],
                             out=xg[:, sc, :], out_offset=None, in_=h2_d.h[:, :],
                             in_offset=bass.IndirectOffsetOnAxis(ap=xi.h[:, sc:sc + 1], axis=0))
                    return dict(xi=xi, vl=vl, xg=xg)

                def stageA2(sa):
                    xg = sa["xg"]
                    xgT_ = xgTr.next()
                    for sc in range(SCH):
                        pt = auxring.next()
                        ptv = pt.h[:, :].bitcast(BF16)
                        for kc in range(KC):
                            P.op("pe", "transpose", out=V(ptv[:, kc * 128:(kc + 1) * 128], pt[:, :].bufs),
                                 in_=xg[:, sc, kc * 128:(kc + 1) * 128], identity=ident_b[:, :])
                        cp("act" if sc % 2 else "dve", V(xgT_.h[:, :, sc * 128:(sc + 1) * 128], xgT_[:, :, :].bufs),
                           V(ptv.rearrange("p (k t) -> p k t", k=KC), pt[:, :].bufs))
                    sa["xgT"] = xgT_

                def stage_up(sa, Wg, Wu, mid=None):
                    xgT_ = sa["xgT"]
                    for fc in range(KC):
                        if fc == 4 and mid is not None:
                            mid()
                        psg = mmring.next()
                        for kc in range(KC):
                            mm(psg[:, 0:CAP], Wg[:, kc, fc * 128:(fc + 1) * 128], xgT_[:, kc, :], kc == 0, kc == KC - 1)
                        psu = mmring.next()
                        for kc in range(KC):
                            mm(psu[:, 0:CAP], Wu[:, kc, fc * 128:(fc + 1) * 128], xgT_[:, kc, :], kc == 0, kc == KC - 1)
                        sg = sgr.next()
                        act(sg[:, :], psg[:, 0:CAP], AF.Silu)
                        tt("dve", aT[:, fc, :], psu[:, 0:CAP], sg[:, :], ALU.mult)

                prev_sc = [[]]

                def stage_down(sa, Wd, mid=None):
                    xi, vl = sa["xi"], sa["vl"]
                    mine = []
                    for sc in range(SCH):
                        if sc == (SCH + 1) // 2 and mid is not None:
                            mid()
                            mid = None
                        ys = ysr.next()
                        for hf in range(2):
                            ps = mmring.next()
                            for fc in range(KC):
                                mm(ps[:, :], aT[:, fc, sc * 128:(sc + 1) * 128], Wd[:, fc, hf * 512:(hf + 1) * 512], fc == 0, fc == KC - 1)
                            stt("dve", ys[:, hf * 512:(hf + 1) * 512], ps[:, :], vl[:, sc:sc + 1], g2[:, hf * 512:(hf + 1) * 512],
                                ALU.mult, ALU.mult)
                        mine.append(P.op("pool", "indirect_dma_start", dma=True, reads=[xi[:, :], ys[:, :]], deps_extra=prev_sc[0],
                                         out=xm_d.h[:, :], out_offset=bass.IndirectOffsetOnAxis(ap=xi.h[:, sc:sc + 1], axis=0),
                                         in_=ys.h[:, :], in_offset=None, compute_op=ALU.add))
                    if mid is not None:
                        mid()
                    prev_sc[0] = mine

                sa = stageA1(0)
                Wg = loadw(wg_in, 0)
                Wu = loadw(wu_in, 0)
                Wd = loadw(wd_in, 0)
                stageA2(sa)
                for e in range(NE):
                    nxt = e + 1 < NE
                    if nxt:
                        sb_ = stageA1(e + 1)
                        lg = loadw_issue(wg_in, e + 1)
                        stage_up(sa, Wg, Wu, mid=lambda: loadw_cast(lg))
                        lu = loadw_issue(wu_in, e + 1)
                        stage_down(sa, Wd, mid=lambda: loadw_cast(lu))
                        Wd2 = loadw(wd_in, e + 1)
                        stageA2(sb_)
                        sa, Wg, Wu, Wd = sb_, lg[0], lu[0], Wd2
                    else:
                        stage_up(sa, Wg, Wu)
                        stage_down(sa, Wd)
        P.barrier()
        chk(8)

    except (_Stop, StopBuild):
        P.limit = None
    with ExitStack() as st:
        frow = sb(st, "f_row", [1, D], F32)
        fbc = sb(st, "f_bc", [128, D], F32)
        dma("sp", frow[:, :], fg_in[:, :])
        for hf in range(2):
            ps = auxring.next()
            mm(ps[:, :], ones_f[0:1, :], frow[0:1, hf * 512:(hf + 1) * 512], True, True)
            cp("dve", fbc[:, hf * 512:(hf + 1) * 512], ps[:, :])
        xr = sbring(st, "f_x", [128, D], F32, 3)
        sqj = sb(st, "f_sqj", [128, D], BF16)
        rs = sbring(st, "f_rs", [128, 2], F32, 3)
        yo = sbring(st, "f_y", [128, D], F32, 3)
        for b in range(NB):
            rows = slice(b * 128, b * 128 + 128)
            xt = xr.next(); r = rs.next(); y = yo.next()
            dma("sp", xt[:, :], xm_d.p(b, (rows, slice(None))))
            act(sqj[:, :], xt[:, :], AF.Square, accum_out=r[:, 0:1])
            rsqrt_from_ss(r[:, 1:2], r[:, 0:1], D)
            stt("dve", y[:, :], xt[:, :], r[:, 1:2], fbc[:, :], ALU.mult, ALU.mult)
            dma("pool", out_d.p(b, (rows, slice(None))), y[:, :])

    P.emit(top)
    return nc, top


def host_consts(S):
    NB = S // 128
    f = np.float32
    ident = np.eye(128, dtype=f)
    ltri = (np.arange(128)[:, None] < np.arange(128)[None, :]).astype(f)
    iota = np.tile(np.arange(512, dtype=f)[None, :], (128, 1))
    t = (np.arange(NB)[None, :] * 128 + np.arange(128)[:, None])
    tokhl = np.stack([(t // 64).astype(f), (t % 64).astype(f)], axis=-1)
    invc = np.zeros((128, 4, 16), f)
    for g in range(4):
        half = 1 << g
        for n in range(8):
            invc[:, g, n] = 1.0 / (min(n, half) + half)
            r = 7 - n
            invc[:, g, 8 + n] = 1.0 / (min(half - 1, r) + half + 1)
    ropec = np.zeros((96, 4), f)
    freqs = (10000.0 ** (-np.arange(0, 32, 2, dtype=np.float32) / np.float32(32))).astype(f)
    for i in range(16):
        for base in (64, 80):
            ropec[base + i, 0] = freqs[i]
            ropec[base + i, 1] = np.pi / 2
        ropec[64 + i, 2] = np.pi
        ropec[80 + i, 2] = 0.0
    return dict(ident=ident, ltri=ltri, iota=iota, tokhl=np.ascontiguousarray(tokhl), invc=invc, ropec=ropec)


def host_layout(inp, L):
    f = np.float32
    A = lambda a: np.ascontiguousarray(np.asarray(a), dtype=f)
    w_in = A(inp["w_in"])[:L]
    w_in_ext = np.concatenate([w_in, w_in[:, :, 576:640], w_in[:, :, 656:672], w_in[:, :, 640:656]], axis=2)
    w_uq = A(inp["w_uq"])[:L].reshape(L, 384, H, 96)
    wuqB = np.concatenate([w_uq[..., 0:64], w_uq[..., 80:96], w_uq[..., 64:80]], axis=-1)
    w_ukv = A(inp["w_ukv"])[:L].reshape(L, 256, H, 128)
    pp = lambda a, k: np.ascontiguousarray(A(a)[:L].reshape(L, k, 128).transpose(0, 2, 1))
    d = dict(
        w_mod=A(inp["w_mod"])[:L], b_mod=A(inp["b_mod"])[:L].reshape(L, 1, 6 * D),
        n1g=pp(inp["norm1_g"], KC), n2g=A(inp["norm2_g"])[:L].reshape(L, 1, D),
        w_in=np.ascontiguousarray(w_in_ext),
        bgate=pp(inp["b_gate"], 24), qng=pp(inp["q_norm_g"], 3), kvng=pp(inp["kv_norm_g"], 2),
        wuqA=np.ascontiguousarray(w_uq.reshape(L, 384, 768)), wuqB=np.ascontiguousarray(wuqB.reshape(L, 384, 768)),
        wk=np.ascontiguousarray(w_ukv[..., 0:64].reshape(L, 256, 512)),
        wv=np.ascontiguousarray(w_ukv[..., 64:128].reshape(L, 256, 512)),
        woa=np.ascontiguousarray(A(inp["w_oa"])[:L].reshape(L, H, 64, D).transpose(0, 2, 1, 3)),
        wpool=np.ascontiguousarray(A(inp["w_pool"])[:L].transpose(0, 2, 1, 3)),
        pscale=pp(inp["pool_scale"], KC),
        convw=np.ascontiguousarray(A(inp["conv_w"])[:L].reshape(L, 3, 4, 128).transpose(0, 3, 2, 1)),
        woc=A(inp["w_oc"])[:L], wout=A(inp["w_out"])[:L],
        wr=np.ascontiguousarray(A(inp["w_router"])[:L].reshape(L, KC, 128, NE).transpose(0, 2, 1, 3)),
        w_gate=A(inp["w_gate"])[:L], w_up=A(inp["w_up"])[:L], w_down=A(inp["w_down"])[:L],
        fg=A(inp["final_g"]).reshape(1, D),
    )
    return d


_CACHE = {}


def run(inp, S, L, debug=False, trace=False, stop=99):
    B = np.asarray(inp["x"]).shape[0]
    key = (S, L, debug, stop)
    if key not in _CACHE:
        _CACHE[key] = build(S, L, debug, stop)
    nc, _ = _CACHE[key]
    shared = host_layout(inp, L)
    shared.update(host_consts(S))
    x = np.ascontiguousarray(np.asarray(inp["x"]), dtype=np.float32)
    c = np.asarray(inp["c"], dtype=np.float32)
    pos = np.asarray(inp["positions"]).astype(np.int32)
    in_maps = []
    ncores = int(_os.environ.get('KCORES', '8'))
    for core in range(ncores):
        b = core % B
        m = dict(shared)
        m["x"] = x[b]
        m["cT"] = np.ascontiguousarray(c[b].reshape(KC, 128).T)
        m["pos"] = np.ascontiguousarray(pos[b].reshape(1, S))
        in_maps.append(m)
    res = run_bass_kernel_spmd(nc, in_maps, core_ids=list(range(ncores)), **({"trace": True} if trace else {}))
    return res


def kernel(**inputs):
    S = np.asarray(inputs["x"]).shape[1]
    B = np.asarray(inputs["x"]).shape[0]
    L = np.asarray(inputs["w_mod"]).shape[0]
    res = run(inputs, S, L)
    return np.stack([np.asarray(res.results[b]["out"], dtype=np.float32) for b in range(B)], axis=0)
```
